# Optimizing a Trainium2 kernel written in Bass

```python
import math
import jax
import jax.numpy as jnp
from jax import lax
import numpy as np


D_MODEL = 1024
BATCH = 16
SEQ = 2048
DEPTH = 1

CTX_LEN = 256
GRID_W = 64
EPS = 1e-6

N_ATTN_HEADS = 8
ATTN_DH = 64
ATTN_DV = 2 * ATTN_DH
ATTN_QK_DIM = N_ATTN_HEADS * 2 * ATTN_DH
ATTN_V_DIM = N_ATTN_HEADS * ATTN_DV
ATTN_SCALE = ATTN_DH ** -0.5
ATTN_QBLOCK = 128
ROPE_THETA = 10000.0
ROPE_AXIS_DIM = ATTN_DH // 2

D_SSD = D_MODEL
SSD_HEADDIM = 64
SSD_HEADS = D_SSD // SSD_HEADDIM
SSD_GROUPS = 4
SSD_HPG = SSD_HEADS // SSD_GROUPS
SSD_STATE = 128
SSD_CONV_W = 3
SSD_CHUNK = 64
XBC_DIM = D_SSD + 2 * SSD_GROUPS * SSD_STATE

IN_SPLITS = (ATTN_QK_DIM, 2 * ATTN_QK_DIM, 2 * ATTN_QK_DIM + ATTN_V_DIM, 2 * ATTN_QK_DIM + ATTN_V_DIM + D_SSD, 2 * ATTN_QK_DIM + ATTN_V_DIM + D_SSD + XBC_DIM, 2 * ATTN_QK_DIM + ATTN_V_DIM + D_SSD + XBC_DIM + 2 * SSD_HEADS)
IN_COLS = IN_SPLITS[-1] + 2 * D_MODEL

N_EXPERTS = 64
EXPERT_FF = 256
SHARED_FF = 256
TOP_K = 8
N_EXPERT_GROUPS = 8
TOPK_GROUPS = 4
ROUTED_SCALE = 2.5
MOE_BLOCK = 128

kernel_name = 'hybrid_diffattn_ssd_moe_dit_layer'


def rmsnorm(u, g):
    uf = u.astype(jnp.float32)
    y = uf * lax.rsqrt(jnp.mean(uf * uf, axis=-1, keepdims=True) + EPS)
    return (y * g.astype(jnp.float32)).astype(u.dtype)


def modulate(u, shift, scale):
    return u * (1 + scale) + shift


def rev(t):
    return jnp.flip(t, axis=1)


def axial_rope_tables(n_tok):
    rows = n_tok // GRID_W
    row = jnp.repeat(jnp.arange(rows, dtype=jnp.float32), GRID_W)
    col = jnp.broadcast_to(jnp.arange(GRID_W, dtype=jnp.float32)[None, :], (rows, GRID_W)).reshape(-1)
    inv_freq = ROPE_THETA ** (-jnp.arange(0, ROPE_AXIS_DIM, 2, dtype=jnp.float32) / ROPE_AXIS_DIM)
    ang = jnp.concatenate([row[:, None] * inv_freq, col[:, None] * inv_freq], axis=-1)
    ang = jnp.concatenate([ang, ang], axis=-1)
    return jnp.cos(ang), jnp.sin(ang)


def apply_rope(u, cos, sin):
    half = ATTN_DH // 2
    uf = u.astype(jnp.float32)
    rot = jnp.concatenate([-uf[..., half:], uf[..., :half]], axis=-1)
    return (uf * cos[:, None, None, :] + rot * sin[:, None, None, :]).astype(u.dtype)


def diff_attend(q, k, v, lam, g_subln, lam_init):
    s = jnp.einsum('bqhcd,bkhcd->bchqk', q.astype(jnp.float32), k.astype(jnp.float32)) * ATTN_SCALE
    p = jax.nn.softmax(s, axis=-1)
    w = p[:, 0] - lam * p[:, 1]
    o = jnp.einsum('bhqk,bkhd->bqhd', w, v.astype(jnp.float32))
    o = o * lax.rsqrt(jnp.mean(o * o, axis=-1, keepdims=True) + EPS) * g_subln.astype(jnp.float32)
    return (o * (1.0 - lam_init)).astype(v.dtype)


def centred_dwconv(u, w, b):
    pad = SSD_CONV_W // 2
    n = u.shape[1]
    up = jnp.pad(u, ((0, 0), (pad, pad), (0, 0)))
    out = b
    for i in range(SSD_CONV_W):
        out = out + up[:, i:i + n] * w[i]
    return out


def ssd_inputs(xbc, dt_raw, conv_w, conv_b, dt_bias):
    nb, nl, _ = xbc.shape
    u = jax.nn.silu(centred_dwconv(xbc, conv_w, conv_b)).astype(jnp.float32)
    xs, bm, cm = jnp.split(u, (D_SSD, D_SSD + SSD_GROUPS * SSD_STATE), axis=-1)
    xh = xs.reshape(nb, nl, SSD_GROUPS, SSD_HPG, SSD_HEADDIM)
    bm = bm.reshape(nb, nl, SSD_GROUPS, SSD_STATE)
    cm = cm.reshape(nb, nl, SSD_GROUPS, SSD_STATE)
    dt = jax.nn.softplus(dt_raw.astype(jnp.float32).reshape(nb, nl, 2, SSD_GROUPS, SSD_HPG) + dt_bias.astype(jnp.float32).reshape(2, SSD_GROUPS, SSD_HPG))
    return xh, bm, cm, dt[:, :, 0], dt[:, :, 1]


def ssd_chunked(xh, dt, a, bm, cm, h0, with_output):
    nb, nl, ng, nr, npd = xh.shape
    nn = bm.shape[-1]
    nc = nl // SSD_CHUNK
    xc = xh.reshape(nb, nc, SSD_CHUNK, ng, nr, npd)
    dtc = dt.reshape(nb, nc, SSD_CHUNK, ng, nr)
    bc = bm.reshape(nb, nc, SSD_CHUNK, ng, nn)
    acum = jnp.cumsum(dtc * a, axis=2)
    to_end = jnp.exp(acum[:, :, -1:] - acum)
    states = jnp.einsum('bcqgn,bcqgr,bcqgrp->bcgrpn', bc, to_end * dtc, xc)
    chunk_decay = jnp.exp(acum[:, :, -1])

    def step(h, inp):
        st, dec = inp
        return h * dec[..., None, None] + st, (h if with_output else None)

    h_last, h_prev = lax.scan(step, h0, (jnp.moveaxis(states, 1, 0), jnp.moveaxis(chunk_decay, 1, 0)))
    if not with_output:
        return None, h_last
    cc = cm.reshape(nb, nc, SSD_CHUNK, ng, nn)
    seg = acum[:, :, :, None] - acum[:, :, None]
    lower = (jnp.arange(SSD_CHUNK)[:, None] >= jnp.arange(SSD_CHUNK)[None, :])[:, :, None, None]
    decay = jnp.where(lower, jnp.exp(jnp.where(lower, seg, 0.0)), 0.0)
    cb = jnp.einsum('bcign,bcjgn->bcijg', cc, bc)
    y_diag = jnp.einsum('bcijg,bcijgr,bcjgr,bcjgrp->bcigrp', cb, decay, dtc, xc)
    y_off = jnp.einsum('bcign,cbgrpn,bcigr->bcigrp', cc, h_prev, jnp.exp(acum))
    y = (y_diag + y_off).reshape(nb, nl, ng, nr, npd)
    return y, h_last


def merge_branches(o_attn, y_ssd, z, gates, g_ssd_norm, w_branch_attn, w_branch_ssd, w_out):
    out_dtype = z.dtype
    y = y_ssd * jax.nn.silu(z.astype(jnp.float32))
    shp = y.shape
    yg = y.reshape(shp[:-1] + (SSD_GROUPS, D_SSD // SSD_GROUPS))
    yg = yg * lax.rsqrt(jnp.mean(yg * yg, axis=-1, keepdims=True) + EPS)
    y = (yg.reshape(shp) * g_ssd_norm.astype(jnp.float32)).astype(out_dtype)
    ya = o_attn @ w_branch_attn
    ys = y @ w_branch_ssd
    g_a, g_s = jnp.split(jax.nn.sigmoid(gates), 2, axis=-1)
    return (g_a * ya + g_s * ys) @ w_out


def token_mixer(h, hc, update_ctx, lam_init, w_in, lam_q1, lam_k1, lam_q2, lam_k2, g_attn_subln, conv_w, conv_b, dt_bias, a_log, d_skip, g_ssd_norm, w_branch_attn, w_branch_ssd, w_out):
    nb, ns, _ = h.shape
    nl = hc.shape[1]
    w_q, w_k, w_v, w_z, w_xbc, w_dt, w_g = jnp.split(w_in, IN_SPLITS, axis=1)
    q, k, v, z, xbc, dt_raw, gates = jnp.split(h @ w_in, IN_SPLITS, axis=-1)

    lam = (jnp.exp(jnp.sum(lam_q1.astype(jnp.float32) * lam_k1.astype(jnp.float32)))
           - jnp.exp(jnp.sum(lam_q2.astype(jnp.float32) * lam_k2.astype(jnp.float32))) + lam_init)
    cos, sin = axial_rope_tables(ns)
    q = apply_rope(q.reshape(nb, ns, N_ATTN_HEADS, 2, ATTN_DH), cos, sin)
    k = apply_rope(k.reshape(nb, ns, N_ATTN_HEADS, 2, ATTN_DH), cos, sin)
    v = v.reshape(nb, ns, N_ATTN_HEADS, ATTN_DV)
    kc = (hc @ w_k).reshape(nb, nl, N_ATTN_HEADS, 2, ATTN_DH)
    vc = (hc @ w_v).reshape(nb, nl, N_ATTN_HEADS, ATTN_DV)
    k_all = jnp.concatenate([kc, k], axis=1)
    v_all = jnp.concatenate([vc, v], axis=1)
    n_qb = ns // ATTN_QBLOCK
    q_blocks = jnp.swapaxes(q.reshape(nb, n_qb, ATTN_QBLOCK, N_ATTN_HEADS, 2, ATTN_DH), 0, 1)
    o = lax.map(lambda qb: diff_attend(qb, k_all, v_all, lam, g_attn_subln, lam_init), q_blocks)
    o_lat = jnp.swapaxes(o, 0, 1).reshape(nb, ns, ATTN_V_DIM)

    a = -jnp.exp(a_log.astype(jnp.float32)).reshape(2, SSD_GROUPS, SSD_HPG)
    d = d_skip.astype(jnp.float32).reshape(SSD_GROUPS, SSD_HPG, 1)
    xh_c, bm_c, cm_c, dtf_c, dtb_c = ssd_inputs(hc @ w_xbc, hc @ w_dt, conv_w, conv_b, dt_bias)
    h0 = jnp.zeros((nb, SSD_GROUPS, SSD_HPG, SSD_HEADDIM, SSD_STATE), jnp.float32)
    yf_c, hf = ssd_chunked(xh_c, dtf_c, a[0], bm_c, cm_c, h0, update_ctx)
    yb_c, hb = ssd_chunked(rev(xh_c), rev(dtb_c), a[1], rev(bm_c), rev(cm_c), h0, update_ctx)
    xh, bm, cm, dtf, dtb = ssd_inputs(xbc, dt_raw, conv_w, conv_b, dt_bias)
    yf, _ = ssd_chunked(xh, dtf, a[0], bm, cm, hf, True)
    yb, _ = ssd_chunked(rev(xh), rev(dtb), a[1], rev(bm), rev(cm), hb, True)
    y_lat = (yf + rev(yb) + d * xh).reshape(nb, ns, D_SSD)

    out_lat = merge_branches(o_lat, y_lat, z, gates, g_ssd_norm, w_branch_attn, w_branch_ssd, w_out)
    if not update_ctx:
        return out_lat, None
    qc = (hc @ w_q).reshape(nb, nl, N_ATTN_HEADS, 2, ATTN_DH)
    o_ctx = diff_attend(qc, kc, vc, lam, g_attn_subln, lam_init).reshape(nb, nl, ATTN_V_DIM)
    y_ctx = (yf_c + rev(yb_c) + d * xh_c).reshape(nb, nl, D_SSD)
    out_ctx = merge_branches(o_ctx, y_ctx, hc @ w_z, hc @ w_g, g_ssd_norm, w_branch_attn, w_branch_ssd, w_out)
    return out_lat, out_ctx


def moe_ffn(h, w_router, router_bias, w_e_gate, w_e_up, w_e_down, w_sh_gate, w_sh_up, w_sh_down):
    n_tok, d = h.shape
    scores = jax.nn.sigmoid((h @ w_router).astype(jnp.float32))
    sel = scores + router_bias.astype(jnp.float32)
    grp = sel.reshape(n_tok, N_EXPERT_GROUPS, N_EXPERTS // N_EXPERT_GROUPS)
    grp_score = lax.top_k(grp, 2)[0].sum(-1)
    top_g = lax.top_k(grp_score, TOPK_GROUPS)[1]
    gmask = jax.nn.one_hot(top_g, N_EXPERT_GROUPS, dtype=jnp.float32).sum(1) > 0
    emask = jnp.repeat(gmask, N_EXPERTS // N_EXPERT_GROUPS, axis=1)
    top_e = lax.top_k(jnp.where(emask, sel, -jnp.inf), TOP_K)[1]
    wts = jnp.take_along_axis(scores, top_e, axis=1)
    wts = wts / jnp.sum(wts, axis=-1, keepdims=True) * ROUTED_SCALE

    n_assign = n_tok * TOP_K
    n_blocks = -(-n_assign // MOE_BLOCK) + N_EXPERTS
    n_pad = n_blocks * MOE_BLOCK
    e_flat = top_e.reshape(-1)
    order = jnp.argsort(e_flat)
    e_sorted = e_flat[order]
    tok_sorted = (jnp.arange(n_assign, dtype=jnp.int32) // TOP_K)[order]
    w_sorted = wts.reshape(-1)[order]
    counts = jnp.bincount(e_flat, length=N_EXPERTS)
    start = jnp.cumsum(counts) - counts
    padded = (counts + MOE_BLOCK - 1) // MOE_BLOCK * MOE_BLOCK
    pend = jnp.cumsum(padded)
    pstart = pend - padded
    dest = pstart[e_sorted] + jnp.arange(n_assign, dtype=jnp.int32) - start[e_sorted]
    buf_tok = jnp.zeros((n_pad,), jnp.int32).at[dest].set(tok_sorted)
    buf_w = jnp.zeros((n_pad,), jnp.float32).at[dest].set(w_sorted)
    block_e = jnp.minimum(jnp.searchsorted(pend, jnp.arange(n_blocks, dtype=jnp.int32) * MOE_BLOCK, side='right'), N_EXPERTS - 1)

    def expert_block(args):
        tok_b, w_b, e = args
        xb = h[tok_b]
        hid = jax.nn.silu(xb @ w_e_gate[e]) * (xb @ w_e_up[e])
        return (hid @ w_e_down[e]) * w_b[:, None].astype(h.dtype)

    out = lax.map(expert_block, (buf_tok.reshape(n_blocks, MOE_BLOCK), buf_w.reshape(n_blocks, MOE_BLOCK), block_e))
    routed = jax.ops.segment_sum(out.reshape(n_pad, d), buf_tok, num_segments=n_tok)
    shared = (jax.nn.silu(h @ w_sh_gate) * (h @ w_sh_up)) @ w_sh_down
    return routed + shared


def setup_inputs(seed: int = 0) -> dict:
    key = jax.random.key(seed)
    ks = jax.random.split(key, 40)
    f32 = jnp.float32

    def nrm(k, shape, scale):
        return jax.random.normal(k, shape, f32) * scale

    def gain(k, shape):
        return 1.0 + 0.05 * jax.random.normal(k, shape, f32)

    u_dt = jax.random.uniform(ks[18], (DEPTH, 2, SSD_HEADS), f32)
    dt0 = jnp.exp(u_dt * (math.log(0.1) - math.log(0.001)) + math.log(0.001))
    return {
        'x': nrm(ks[0], (BATCH, SEQ, D_MODEL), 1.0),
        'c': nrm(ks[1], (BATCH, D_MODEL), 1.0),
        'ctx': nrm(ks[2], (BATCH, CTX_LEN, D_MODEL), 1.0),
        'c_ctx': nrm(ks[3], (D_MODEL,), 1.0),
        'w_ada': nrm(ks[4], (DEPTH, D_MODEL, 6 * D_MODEL), 0.5 * D_MODEL ** -0.5),
        'b_ada': nrm(ks[5], (DEPTH, 6 * D_MODEL), 0.02),
        'g_pre_mix': gain(ks[6], (DEPTH, D_MODEL)),
        'g_post_mix': gain(ks[7], (DEPTH, D_MODEL)),
        'g_pre_ffn': gain(ks[8], (DEPTH, D_MODEL)),
        'g_post_ffn': gain(ks[9], (DEPTH, D_MODEL)),
        'w_in': nrm(ks[10], (DEPTH, D_MODEL, IN_COLS), D_MODEL ** -0.5),
        'lam_q1': nrm(ks[11], (DEPTH, ATTN_DH), 0.1),
        'lam_k1': nrm(ks[12], (DEPTH, ATTN_DH), 0.1),
        'lam_q2': nrm(ks[13], (DEPTH, ATTN_DH), 0.1),
        'lam_k2': nrm(ks[14], (DEPTH, ATTN_DH), 0.1),
        'g_attn_subln': gain(ks[15], (DEPTH, ATTN_DV)),
        'conv_w': nrm(ks[16], (DEPTH, SSD_CONV_W, XBC_DIM), SSD_CONV_W ** -0.5),
        'conv_b': nrm(ks[17], (DEPTH, XBC_DIM), 0.02),
        'dt_bias': dt0 + jnp.log(-jnp.expm1(-dt0)),
        'a_log': jnp.log(jax.random.uniform(ks[19], (DEPTH, 2, SSD_HEADS), f32, 1.0, 16.0)),
        'd_skip': gain(ks[20], (DEPTH, SSD_HEADS)),
        'g_ssd_norm': gain(ks[21], (DEPTH, D_SSD)),
        'w_branch_attn': nrm(ks[22], (DEPTH, ATTN_V_DIM, D_MODEL), ATTN_V_DIM ** -0.5),
        'w_branch_ssd': nrm(ks[23], (DEPTH, D_SSD, D_MODEL), D_SSD ** -0.5),
        'w_out': nrm(ks[24], (DEPTH, D_MODEL, D_MODEL), D_MODEL ** -0.5),
        'w_router': nrm(ks[25], (DEPTH, D_MODEL, N_EXPERTS), D_MODEL ** -0.5),
        'router_bias': nrm(ks[26], (DEPTH, N_EXPERTS), 0.01),
        'w_e_gate': nrm(ks[27], (DEPTH, N_EXPERTS, D_MODEL, EXPERT_FF), D_MODEL ** -0.5),
        'w_e_up': nrm(ks[28], (DEPTH, N_EXPERTS, D_MODEL, EXPERT_FF), D_MODEL ** -0.5),
        'w_e_down': nrm(ks[29], (DEPTH, N_EXPERTS, EXPERT_FF, D_MODEL), EXPERT_FF ** -0.5),
        'w_sh_gate': nrm(ks[30], (DEPTH, D_MODEL, SHARED_FF), D_MODEL ** -0.5),
        'w_sh_up': nrm(ks[31], (DEPTH, D_MODEL, SHARED_FF), D_MODEL ** -0.5),
        'w_sh_down': nrm(ks[32], (DEPTH, SHARED_FF, D_MODEL), SHARED_FF ** -0.5),
    }


def reference(x, c, ctx, c_ctx, w_ada, b_ada, g_pre_mix, g_post_mix, g_pre_ffn, g_post_ffn, w_in, lam_q1, lam_k1, lam_q2, lam_k2, g_attn_subln, conv_w, conv_b, dt_bias, a_log, d_skip, g_ssd_norm, w_branch_attn, w_branch_ssd, w_out, w_router, router_bias, w_e_gate, w_e_up, w_e_down, w_sh_gate, w_sh_up, w_sh_down):
    nb, ns, d = x.shape
    for li in range(DEPTH):
        update_ctx = li < DEPTH - 1
        lam_init = 0.8 - 0.6 * math.exp(-0.3 * li)
        mod = jax.nn.silu(c) @ w_ada[li] + b_ada[li]
        sh1, sc1, gt1, sh2, sc2, gt2 = jnp.split(mod[:, None, :], 6, axis=-1)
        mod_c = jax.nn.silu(c_ctx) @ w_ada[li] + b_ada[li]
        sh1c, sc1c, gt1c, sh2c, sc2c, gt2c = jnp.split(mod_c, 6)

        h = modulate(rmsnorm(x, g_pre_mix[li]), sh1, sc1)
        hc = modulate(rmsnorm(ctx, g_pre_mix[li]), sh1c, sc1c)
        mix, mix_c = token_mixer(h, hc, update_ctx, lam_init, w_in[li], lam_q1[li], lam_k1[li], lam_q2[li], lam_k2[li], g_attn_subln[li], conv_w[li], conv_b[li], dt_bias[li], a_log[li], d_skip[li], g_ssd_norm[li], w_branch_attn[li], w_branch_ssd[li], w_out[li])
        x = x + gt1 * rmsnorm(mix, g_post_mix[li])

        h = modulate(rmsnorm(x, g_pre_ffn[li]), sh2, sc2)
        moe_w = (w_router[li], router_bias[li], w_e_gate[li], w_e_up[li], w_e_down[li], w_sh_gate[li], w_sh_up[li], w_sh_down[li])
        if update_ctx:
            ctx = ctx + gt1c * rmsnorm(mix_c, g_post_mix[li])
            hc = modulate(rmsnorm(ctx, g_pre_ffn[li]), sh2c, sc2c)
            f = moe_ffn(jnp.concatenate([h.reshape(-1, d), hc.reshape(-1, d)], axis=0), *moe_w)
            f_lat = f[: nb * ns]
            ctx = ctx + gt2c * rmsnorm(f[nb * ns:].reshape(ctx.shape), g_post_ffn[li])
        else:
            f_lat = moe_ffn(h.reshape(-1, d), *moe_w)
        x = x + gt2 * rmsnorm(f_lat.reshape(x.shape), g_post_ffn[li])
    return x
```

```python
from contextlib import ExitStack
import numpy as np
import concourse.bass as bass
import concourse.mybir as mybir
from concourse.bass_utils import run_bass_kernel_spmd

F32 = mybir.dt.float32
BF16 = mybir.dt.bfloat16
AF = mybir.ActivationFunctionType
ALU = mybir.AluOpType
AX = mybir.AxisListType

S_LEN = 2048
L_CTX = 256
D = 1024
NB = 2
NT = S_LEN // 128
NEXP = 64
BIG = 30000.0
SPARSE = True
BLK = 256
NBLK = (S_LEN * 8) // BLK + NEXP
NSLOT = NBLK * BLK
I32 = mybir.dt.int32
U32 = mybir.dt.uint32


class Res:
    __slots__ = ("w", "r")

    def __init__(self):
        self.w = None
        self.r = []


def RL(n):
    return [Res() for _ in range(n)]


ENGS = ("pe", "act", "dve", "pool", "sp")
NDMA = 48


class Sched:
    def __init__(self, nc, stack):
        self.nc = nc
        self.esem = {e: stack.enter_context(nc.semaphore("s_" + e)) for e in ENGS if e != "sp"}
        self.dsem = [stack.enter_context(nc.semaphore("d%d" % i)) for i in range(NDMA)]
        self.dcnt = [0] * NDMA
        self.dnext = 0
        self.ops = {e: [] for e in ENGS}
        self.cnt = {e: 0 for e in ENGS}
        self.seen = {e: {} for e in ENGS}

    def _deps(self, eng, reads, writes):
        deps = {}

        def add(tok, kind):
            if tok is None:
                return
            key, val = tok
            if key == eng and (eng == "pe" or kind != "raw"):
                return
            if deps.get(key, 0) < val:
                deps[key] = val

        for r in reads:
            add(r.w, "raw")
        for w in writes:
            add(w.w, "waw")
            for t in w.r:
                add(t, "war")
        waits = []
        for key, val in deps.items():
            if self.seen[eng].get(key, 0) >= val:
                continue
            self.seen[eng][key] = val
            waits.append((key, val))
        return waits

    def _mark(self, tok, reads, writes):
        for r in reads:
            r.r.append(tok)
            if len(r.r) > 48:
                best = {}
                for k, v in r.r:
                    if best.get(k, 0) < v:
                        best[k] = v
                r.r = list(best.items())
        for w in writes:
            w.w = tok
            w.r = []

    def op(self, eng, fn, reads=(), writes=(), inc=True):
        waits = self._deps(eng, reads, writes)
        if inc:
            self.cnt[eng] += 1
            tok = (eng, self.cnt[eng])
        else:
            tok = (eng, self.cnt[eng] + 1)
        self.ops[eng].append((waits, fn, ("e", inc)))
        self._mark(tok, reads, writes)

    def dma(self, q, out, in_, reads=(), writes=(), **kw):
        return self.dma_fn(q, (lambda e: e.dma_start(out=out, in_=in_, **kw)), reads, writes)

    def dma_fn(self, q, fn, reads=(), writes=()):
        k = self.dnext
        self.dnext = (self.dnext + 1) % NDMA
        waits = self._deps(q, reads, writes)
        key = "d%d" % k
        prev = 16 * self.dcnt[k]
        if prev and self.seen[q].get(key, 0) < prev:
            self.seen[q][key] = prev
            waits.append((key, prev))
        self.dcnt[k] += 1
        tok = (key, 16 * self.dcnt[k])
        self.ops[q].append((waits, fn, ("d", k)))
        self._mark(tok, reads, writes)
        return tok

    def barrier(self):
        toks = [(e, self.cnt[e]) for e in ("pe", "act", "dve", "pool") if self.cnt[e]]
        toks += [("d%d" % k, 16 * self.dcnt[k]) for k in range(NDMA) if self.dcnt[k]]
        for e in ENGS:
            waits = []
            for key, val in toks:
                if key == e or self.seen[e].get(key, 0) >= val:
                    continue
                self.seen[e][key] = val
                waits.append((key, val))
            self.ops[e].append((waits, None, None))

    def _sem(self, key):
        if key in self.esem:
            return self.esem[key]
        return self.dsem[int(key[1:])]

    def emit(self):
        nc = self.nc
        finals = [("d%d" % k, 16 * self.dcnt[k]) for k in range(NDMA) if self.dcnt[k]]
        self.ops["sp"].append((finals, None, None))

        def runner(ename):
            def run(e):
                for waits, fn, kind in self.ops[ename]:
                    for key, val in waits:
                        e.wait_ge(self._sem(key), val)
                    if fn is None:
                        continue
                    ins = fn(e)
                    if kind[0] == "e":
                        if kind[1]:
                            ins.then_inc(self.esem[ename], 1)
                    else:
                        ins.then_inc(self.dsem[kind[1]], 16)
            return run

        with nc.Block() as block:
            block.sync(runner("sp"))
            block.tensor(runner("pe"))
            block.scalar(runner("act"))
            block.vector(runner("dve"))
            block.gpsimd(runner("pool"))


class Arena:
    def __init__(self, nc, st, nbytes):
        self.t = st.enter_context(nc.sbuf_tensor("arena", [128, nbytes // 4], F32))
        self.v = {F32: self.t, BF16: self.t.bitcast(BF16)}
        self.off = 0
        self.cap = nbytes

    def alloc(self, free, dt):
        sz = 4 if dt == F32 else 2
        n = int(np.prod(free))
        off = self.off
        self.off += (n * sz + 63) // 64 * 64
        assert self.off <= self.cap, ("arena overflow", self.off, self.cap)
        v = self.v[dt][:, off // sz: off // sz + n]
        if len(free) == 2:
            v = v.rearrange("p (a b) -> p a b", a=free[0], b=free[1])
        elif len(free) == 3:
            v = v.rearrange("p (a b c) -> p a b c", a=free[0], b=free[1], c=free[2])
        return v


def host_consts():
    c = {}
    c["ident"] = np.eye(128, dtype=np.float32)
    k = np.arange(128)
    c["trif"] = (k[:, None] <= k[None, :]).astype(np.float32)
    c["trib"] = (k[:, None] >= k[None, :]).astype(np.float32)
    c["negf"] = np.where(k[:, None] <= k[None, :], 0.0, -BIG).astype(np.float32)
    c["negb"] = np.where(k[:, None] >= k[None, :], 0.0, -BIG).astype(np.float32)
    c["ones"] = np.ones((128, 128), np.float32)
    c["tris"] = (k[:, None] < k[None, :]).astype(np.float32)
    c["pswap"] = (k[:, None] == (k[None, :] ^ 32)).astype(np.float32)
    c["pidx"] = np.tile(np.arange(128, dtype=np.float32)[:, None], (1, 128))
    c["jtab"] = np.tile((np.arange(128, dtype=np.float32) * BLK)[None, :], (128, 1))
    sel = np.zeros((3, 3, 128), np.float32)
    for b in range(3):
        sel[b, b, :] = 1.0
    c["sel3"] = sel
    t = np.arange(S_LEN)
    row = (t // 64).astype(np.float32)
    col = (t % 64).astype(np.float32)
    inv = (np.float32(10000.0) ** (-np.arange(0, 32, 2, dtype=np.float32) / np.float32(32))).astype(np.float32)
    ang = np.concatenate([row[:, None] * inv[None, :], col[:, None] * inv[None, :]], axis=-1)
    ang = np.concatenate([ang, ang], axis=-1).astype(np.float32)
    cos = np.cos(ang).astype(np.float32).T
    sin = np.sin(ang).astype(np.float32).T
    sign = np.where(np.arange(64) < 32, -1.0, 1.0).astype(np.float32)[:, None]
    c["cosT"] = np.concatenate([cos, cos], axis=0)
    c["sinT"] = np.concatenate([sin * sign, sin * sign], axis=0)
    return c


CONST_SHAPES = {"ident": [128, 128], "trif": [128, 128], "trib": [128, 128], "negf": [128, 128],
                "negb": [128, 128], "ones": [128, 128], "tris": [128, 128], "jtab": [128, 128], "pidx": [128, 128], "pswap": [128, 128], "sel3": [3, 3, 128],
                "cosT": [128, S_LEN], "sinT": [128, S_LEN]}

WSHAPES = {
    "w_ada": [D, 6 * D], "b_ada": [6 * D], "g_pre_mix": [D], "g_post_mix": [D], "g_pre_ffn": [D],
    "g_post_ffn": [D], "w_in": [D, 8224], "lam_q1": [64], "lam_k1": [64], "lam_q2": [64], "lam_k2": [64],
    "g_attn_subln": [128], "conv_w": [3, 2048], "conv_b": [2048], "dt_bias": [32], "a_log": [32],
    "d_skip": [16], "g_ssd_norm": [D], "w_branch_attn": [D, D], "w_branch_ssd": [D, D], "w_out": [D, D],
    "w_router": [D, 64], "router_bias": [64], "w_e_gate": [NEXP, D, 256], "w_e_up": [NEXP, D, 256],
    "w_e_down": [NEXP, 256, D], "w_sh_gate": [D, 256], "w_sh_up": [D, 256], "w_sh_down": [256, D],
}


def build(dbg=None, stop=None, nseq=NB):
    nc = bass.Bass("TRN2", target_bir_lowering=False)
    di = lambda name, shape: nc.dram_tensor(name, shape, F32, kind="ExternalInput").ap()
    x_d = di("x", [NB, S_LEN, D])
    c3_d = di("c3", [3, D])
    ctx_d = di("ctx", [NB, L_CTX, D])
    Wd = {k: di(k, v) for k, v in WSHAPES.items()}
    Cd = {k: di("k_" + k, v) for k, v in CONST_SHAPES.items()}
    out_d = nc.dram_tensor("out", [NB, S_LEN, D], F32, kind="ExternalOutput").ap()
    dbg_d = {}
    if dbg:
        for k, shp in dbg.items():
            dbg_d[k] = nc.dram_tensor("dbg_" + k, shp, F32, kind="ExternalOutput").ap()

    wgb = nc.dram_tensor("wgb", [NEXP * 128, 2048], BF16, kind="Internal").ap()
    wub = nc.dram_tensor("wub", [NEXP * 128, 2048], BF16, kind="Internal").ap()
    wdb = nc.dram_tensor("wdb", [NEXP * 128, 2048], BF16, kind="Internal").ap()
    xbuf = nc.dram_tensor("xbuf", [NSLOT, D], BF16, kind="Internal").ap()
    ybuf = nc.dram_tensor("ybuf", [NSLOT, D], BF16, kind="Internal").ap()
    st = ExitStack()
    with st:
        S = Sched(nc, st)
        r_wconv = Res()
        conv_list = []
        for e_ in range(NEXP):
            rows_ = slice(e_ * 128, (e_ + 1) * 128)
            conv_list.append((wgb[rows_, :].rearrange("p (k f) -> p k f", k=8), Wd["w_e_gate"][e_].rearrange("(k p) f -> p k f", p=128)))
            conv_list.append((wub[rows_, :].rearrange("p (k f) -> p k f", k=8), Wd["w_e_up"][e_].rearrange("(k p) f -> p k f", p=128)))
            conv_list.append((wdb[rows_, :].rearrange("p (c n) -> p c n", c=2), Wd["w_e_down"][e_].rearrange("(c p) n -> p c n", p=128)))

        def conv_some(n):
            for _ in range(n):
                if conv_list and SPARSE:
                    d_, s_ = conv_list.pop(0)
                    S.dma("pool", d_, s_, writes=[r_wconv])
        sbt = lambda name, shape, dt=F32: st.enter_context(nc.sbuf_tensor(name, shape, dt))
        PSALL = st.enter_context(nc.psum_tensor("psall", [128, 4096], F32))
        PSALLB = PSALL.bitcast(BF16)
        PS = [PSALL[:, i * 1024:(i + 1) * 1024] for i in range(4)]
        PSB = [PSALLB[:, i * 2048:(i + 1) * 2048] for i in range(4)]
        pb = RL(8)

        def bank(b):
            return PS[b // 2][:, (b % 2) * 512:(b % 2) * 512 + 512]

        def bankb(b):
            return PSB[b // 2][:, (b % 2) * 1024:(b % 2) * 1024 + 1024]

        def mm(out, lhsT, rhs, start, stop_, R, W, inc=None):
            if inc is None:
                inc = stop_
            S.op("pe", lambda e: e.matmul(out, lhsT, rhs, start=start, stop=stop_), reads=R, writes=W, inc=inc)

        def tr(out, in_, idn, R, W, inc=True):
            S.op("pe", lambda e: e.transpose(out, in_, idn), reads=R, writes=W, inc=inc)

        def act(out, in_, func, R, W, **kw):
            S.op("act", lambda e: e.activation(out, in_, func, **kw), reads=R, writes=W)

        def tt(eng, out, a, b, op, R, W):
            S.op(eng, lambda e: e.tensor_tensor(out, a, b, op), reads=R, writes=W)

        def ts(eng, out, a, s1, s2, op0, op1, R, W):
            if s2 is None:
                S.op(eng, lambda e: e.tensor_scalar(out, a, s1, None, op0), reads=R, writes=W)
            else:
                S.op(eng, lambda e: e.tensor_scalar(out, a, s1, s2, op0, op1), reads=R, writes=W)

        def stt(out, in0, sc, in1, op0, op1, R, W):
            S.op("dve", lambda e: e.scalar_tensor_tensor(out, in0, sc, in1, op0, op1), reads=R, writes=W)

        def cp(eng, out, in_, R, W):
            if eng == "act":
                S.op("act", lambda e: e.copy(out, in_), reads=R, writes=W)
            else:
                S.op(eng, lambda e: e.tensor_copy(out, in_), reads=R, writes=W)

        def red(out, in_, op, R, W, axis=AX.X):
            S.op("dve", lambda e: e.tensor_reduce(out, in_, axis, op), reads=R, writes=W)

        def recip(out, in_, R, W):
            S.op("dve", lambda e: e.reciprocal(out, in_), reads=R, writes=W)

        def rstd_from_ssq(dst, ssq, n, scratch, R):
            ts("dve", scratch, ssq, 1.0 / n, 1e-6, ALU.mult, ALU.add, [R], [R])
            act(scratch, scratch, AF.Ln, [R], [R])
            act(dst, scratch, AF.Exp, [R], [R], scale=-0.5)

        r_c = Res()
        cst = {}
        for k in ("ident", "trif", "trib", "negf", "negb", "ones", "tris", "jtab", "pidx", "pswap"):
            cst[k] = sbt("c_" + k, [128, 128])
            S.dma("sp", cst[k][:], Cd[k], writes=[r_c])
        ident = cst["ident"]
        identb = sbt("identb", [128, 128], BF16)
        trisb = sbt("trisb", [128, 128], BF16)
        onesbp = sbt("onesbp", [128, 128], BF16)
        cp("dve", trisb[:], cst["tris"][:], [r_c], [r_c])
        cp("dve", onesbp[:], cst["ones"][:], [r_c], [r_c])
        pswapb = sbt("pswapb", [128, 128], BF16)
        cp("dve", pswapb[:], cst["pswap"][:], [r_c], [r_c])
        mhalf = sbt("mhalf", [128, 1])
        S.op("pool", lambda e: e.memset(mhalf[:], -0.5), writes=[r_c])
        cp("dve", identb[:], ident[:], [r_c], [r_c])
        sel3 = sbt("sel3", [3, 3, 128])
        S.dma("sp", sel3[:], Cd["sel3"], writes=[r_c])

        def bcast_load(name, n, parts=128):
            t = sbt("b_" + name, [parts, n])
            S.dma("sp", t[:], Wd[name].partition_broadcast(parts), writes=[r_c])
            return t

        rbiasB = bcast_load("router_bias", 64)
        dtbB = bcast_load("dt_bias", 32)
        alogB = bcast_load("a_log", 32)
        dskB = bcast_load("d_skip", 16)
        lamv = sbt("lamv", [128, 4, 64])
        for i, nm in enumerate(("lam_q1", "lam_k1", "lam_q2", "lam_k2")):
            S.dma("sp", lamv[:, i, :], Wd[nm].partition_broadcast(128), writes=[r_c])
        gpmT = sbt("gpmT", [128, 8])
        gpfT = sbt("gpfT", [128, 8])
        cwT = sbt("cwT", [128, 16, 3])
        cbT = sbt("cbT", [128, 16])
        S.dma("sp", gpmT[:], Wd["g_pre_mix"].rearrange("(k p) -> p k", p=128), writes=[r_c], allow_slow_non_contiguous=True)
        S.dma("sp", gpfT[:], Wd["g_pre_ffn"].rearrange("(k p) -> p k", p=128), writes=[r_c], allow_slow_non_contiguous=True)
        S.dma("sp", cbT[:], Wd["conv_b"].rearrange("(k p) -> p k", p=128), writes=[r_c], allow_slow_non_contiguous=True)
        for i in range(3):
            S.dma("sp", cwT[:, :, i], Wd["conv_w"][i].rearrange("(k p) -> p k", p=128), writes=[r_c], allow_slow_non_contiguous=True)
        wrt = sbt("wrt", [128, 8, 64])
        S.dma("sp", wrt[:], Wd["w_router"].rearrange("(k p) n -> p k n", p=128), writes=[r_c])

        sm = sbt("sm", [128, 64])
        lamt = sbt("lamt", [128, 2, 64])
        tt("dve", lamt[:, 0, :], lamv[:, 0, :], lamv[:, 1, :], ALU.mult, [r_c], [r_c])
        tt("dve", lamt[:, 1, :], lamv[:, 2, :], lamv[:, 3, :], ALU.mult, [r_c], [r_c])
        red(sm[:, 0:2], lamt[:], ALU.add, [r_c], [r_c])
        act(sm[:, 2:4], sm[:, 0:2], AF.Exp, [r_c], [r_c])
        tt("dve", sm[:, 4:5], sm[:, 3:4], sm[:, 2:3], ALU.subtract, [r_c], [r_c])
        ts("dve", sm[:, 5:6], sm[:, 4:5], -0.2, None, ALU.add, None, [r_c], [r_c])
        nlam = sm[:, 5:6]
        aB = sbt("aB", [128, 32])
        act(aB[:], alogB[:], AF.Exp, [r_c], [r_c])
        ts("dve", aB[:], aB[:], -1.0, None, ALU.mult, None, [r_c], [r_c])

        st1 = sbt("st1", [128, 4])
        gsubT = sbt("gsubT", [128, 1])
        sc4 = [sbt("sc4_%d" % d_, [128, 8, 4]) for d_ in range(2)]
        Wr_all = sbt("Wr_all", [128, NT, 64])
        st4 = sbt("st4", [128, 8])
        rt = sbt("rt", [128, 6, 64])
        rt8 = sbt("rt8", [128, 6, 8])
        st6 = sbt("st6", [128, 4])
        dest8u = sbt("dest8u", [128, NT, 8], U32)
        w8 = sbt("w8", [128, NT, 8])
        eb_i = sbt("eb_i", [128, NBLK], U32)
        csum = sbt("csum", [128, 64])
        sp64 = sbt("sp64", [128, 6, 64])
        sp64i = sbt("sp64i", [128, 64], I32)
        d8f = sbt("d8f", [128, 8])
        rem = int(nc.sbuf_bytes_remaining) - 18 * 1024
        arena = Arena(nc, st, rem // 256 * 256)
        a0 = arena.off
        gpostmixB = arena.alloc((D,), F32)
        gpostffnB = arena.alloc((D,), F32)
        S.dma("sp", gpostmixB, Wd["g_post_mix"].partition_broadcast(128), writes=[r_c])
        S.dma("sp", gpostffnB, Wd["g_post_ffn"].partition_broadcast(128), writes=[r_c])
        c3s = arena.alloc((D,), F32)
        mod_rm = arena.alloc((6 * D,), F32)
        bada3 = arena.alloc((6 * D,), F32)
        wab = [arena.alloc((8, 512), F32) for _ in range(2)]
        r_wab = RL(2)
        r_m = Res()
        S.dma("sp", c3s[0:3, :], c3_d, writes=[r_m])
        S.dma("sp", bada3[0:3, :], Wd["b_ada"].partition_broadcast(3), writes=[r_m])
        act(c3s[0:3, :], c3s[0:3, :], AF.Silu, [r_m], [r_m])
        cT = sbt("cT", [128, 8, 3])
        for kc in range(8):
            tr(bank(0)[:, kc * 3:kc * 3 + 3], c3s[0:3, kc * 128:(kc + 1) * 128], ident[0:3, 0:3], [r_m, r_c], [pb[0]])
        cp("dve", cT[:].rearrange("p a b -> p (a b)"), bank(0)[:, 0:24], [pb[0]], [r_m])
        wa_v = Wd["w_ada"].rearrange("(k p) n -> p k n", p=128)
        for nb_ in range(12):
            bi = nb_ % 2
            S.dma("sp", wab[bi], wa_v[:, :, nb_ * 512:(nb_ + 1) * 512], writes=[r_wab[bi]])
            pbk = 2 + bi
            for kc in range(8):
                mm(bank(pbk)[0:3, :], cT[:, kc, :], wab[bi][:, kc, :], kc == 0, kc == 7, [r_m, r_wab[bi]], [pb[pbk]])
            tt("dve", mod_rm[0:3, nb_ * 512:(nb_ + 1) * 512], bank(pbk)[0:3, :], bada3[0:3, nb_ * 512:(nb_ + 1) * 512],
               ALU.add, [pb[pbk], r_m], [r_m])
        modT = sbt("modT", [128, 48, 3])
        for j in range(48):
            tr(bank(0)[:, j * 3:j * 3 + 3], mod_rm[0:3, j * 128:(j + 1) * 128], ident[0:3, 0:3], [r_m, r_c], [pb[0]])
        cp("dve", modT[:].rearrange("p a b -> p (a b)"), bank(0)[:, 0:144], [pb[0]], [r_m])
        AB = sbt("AB", [128, 3, 4, 8])
        for b in range(3):
            ts("dve", AB[:, b, 0, :], modT[:, 8:16, b], 1.0, None, ALU.add, None, [r_m], [r_m])
            tt("dve", AB[:, b, 0, :], AB[:, b, 0, :], gpmT[:], ALU.mult, [r_m, r_c], [r_m])
            cp("dve", AB[:, b, 1, :], modT[:, 0:8, b], [r_m], [r_m])
            ts("dve", AB[:, b, 2, :], modT[:, 32:40, b], 1.0, None, ALU.add, None, [r_m], [r_m])
            tt("dve", AB[:, b, 2, :], AB[:, b, 2, :], gpfT[:], ALU.mult, [r_m, r_c], [r_m])
            cp("dve", AB[:, b, 3, :], modT[:, 24:32, b], [r_m], [r_m])
        G1 = sbt("G1", [128, NB, D])
        G2 = sbt("G2", [128, NB, D])
        for b in range(NB):
            for (G, coff, gB) in ((G1, 2 * D, gpostmixB), (G2, 5 * D, gpostffnB)):
                for hf in range(2):
                    mm(bank(4 + hf)[:, :], sel3[0:3, b, :], mod_rm[0:3, coff + hf * 512:coff + hf * 512 + 512], True, True,
                       [r_m, r_c], [pb[4 + hf]])
                    tt("dve", G[:, b, hf * 512:(hf + 1) * 512], bank(4 + hf)[:, :], gB[:, hf * 512:(hf + 1) * 512], ALU.mult,
                       [pb[4 + hf], r_c], [r_m])
        S.barrier()
        arena.off = a0

        win_v = Wd["w_in"].rearrange("(k p) n -> p k n", p=128)

        def ldw(dst, src, r):
            S.dma("pool", dst, src, writes=[r])

        for b in range(nseq):
            arena.off = a0
            hT = arena.alloc((8, S_LEN), BF16)
            r_hT = RL(NT)
            hcT = arena.alloc((8, L_CTX), BF16)
            r_hcT = RL(2)
            yT = arena.alloc((8, S_LEN), BF16)
            r_yT = RL(NT)
            a1 = arena.off

            xt = [arena.alloc((D,), F32) for _ in range(2)]
            r_xt = RL(2)
            xn = arena.alloc((D,), F32)
            r_xn = Res()
            junk = arena.alloc((D,), F32)
            r_junk = Res()
            r_st1 = Res()

            def norm_to_T(src_ap, bi, dstT, tcol, r_dst, ab_idx, abrow):
                act(junk, src_ap, AF.Square, [r_xt[bi]], [r_junk, r_st1], accum_out=st1[:, 0:1])
                rstd_from_ssq(st1[:, 2:3], st1[:, 0:1], D, st1[:, 1:2], r_st1)
                ts("dve", xn, src_ap, st1[:, 2:3], None, ALU.mult, None, [r_xt[bi], r_st1], [r_xn])
                for kc in range(8):
                    bk = 0 + kc // 4
                    tr(bank(bk)[:, (kc % 4) * 128:(kc % 4) * 128 + 128], xn[:, kc * 128:(kc + 1) * 128], ident[:],
                       [r_xn, r_c], [pb[bk]])
                for kc in range(8):
                    bk = 0 + kc // 4
                    act(dstT[:, kc, tcol:tcol + 128], bank(bk)[:, (kc % 4) * 128:(kc % 4) * 128 + 128], AF.Identity,
                        [pb[bk], r_m], [r_dst], bias=AB[:, abrow, ab_idx + 1, kc:kc + 1], scale=AB[:, abrow, ab_idx, kc:kc + 1])

            tiles = [("c", i) for i in range(2)] + [("x", i) for i in range(NT)]
            for n, (kind, i) in enumerate(tiles):
                bi = n % 2
                src = ctx_d[b, i * 128:(i + 1) * 128, :] if kind == "c" else x_d[b, i * 128:(i + 1) * 128, :]
                S.dma("sp", xt[bi], src, writes=[r_xt[bi]])
                if kind == "c":
                    norm_to_T(xt[bi], bi, hcT, i * 128, r_hcT[i], 0, 2)
                else:
                    norm_to_T(xt[bi], bi, hT, i * 128, r_hT[i], 0, b)
            if dbg and "hT" in dbg and b == 0:
                dtmp = arena.alloc((2, S_LEN), F32)
                r_d = Res()
                for kk in range(4):
                    cp("dve", dtmp, hT[:, kk * 2:kk * 2 + 2, :], r_hT, [r_d])
                    S.dma("sp", dbg_d["hT"][kk * 256:(kk + 1) * 256, :].rearrange("(k p) t -> p k t", p=128), dtmp, reads=[r_d])
            if stop == "p1":
                break
            conv_some(8)
            S.barrier()
            arena.off = a1

            gssdB = arena.alloc((D,), F32)
            r_g3 = Res()
            S.dma("sp", gssdB, Wd["g_ssd_norm"].partition_broadcast(128), writes=[r_g3])
            Wz = arena.alloc((8, 256), BF16)
            r_Wz = Res()
            Wdt = arena.alloc((8, 32), BF16)
            ldw(Wdt, win_v[:, :, 6144:6176], r_g3)
            dt_all = arena.alloc((18, 32), F32)
            dta_all = arena.alloc((18, 32), F32)
            r_dt = Res()
            for tl in range(18):
                bk = 6 + tl % 2
                for kc in range(8):
                    lhs = hcT[:, kc, tl * 128:(tl + 1) * 128] if tl < 2 else hT[:, kc, (tl - 2) * 128:(tl - 1) * 128]
                    rr = [r_hcT[tl]] if tl < 2 else [r_hT[tl - 2]]
                    mm(bank(bk)[:, 0:32], lhs, Wdt[:, kc, :], kc == 0, kc == 7, [r_g3] + rr, [pb[bk]])
                tt("dve", dt_all[:, tl, :], bank(bk)[:, 0:32], dtbB[:, :], ALU.add, [pb[bk], r_c], [r_dt])
            act(dt_all, dt_all, AF.Exp, [r_dt], [r_dt])
            act(dt_all, dt_all, AF.Ln, [r_dt], [r_dt], bias=1.0)
            tt("dve", dta_all, dt_all, aB[:, :].unsqueeze(1).to_broadcast([128, 18, 32]), ALU.mult, [r_dt, r_c], [r_dt])
            NTOK = L_CTX + S_LEN
            Wg4 = [arena.alloc((4, 8, 128), BF16)] * 2
            r_Wg4 = [Res()] * 2
            upad = arena.alloc((NTOK + 4,), F32)
            r_up = Res()
            cacc = PSALL[:, 0:NTOK + 4]
            r_ca = Res()
            xTf = arena.alloc((2, NTOK), BF16)
            BTf = arena.alloc((NTOK,), BF16)
            CTf = arena.alloc((NTOK,), BF16)
            r_xx = Res()
            r_bc = Res()
            x_tok = arena.alloc((18, 256), BF16)
            B_tok = arena.alloc((18, 128), BF16)
            r_tok = Res()
            ydir = [xTf.rearrange("p a b -> p (a b)")[:, 0:16 * 256].rearrange("p (a b) -> p a b", a=16, b=256), arena.alloc((16, 256), BF16)]
            r_yd = [RL(16), RL(16)]
            r_sc4 = RL(2)
            rb_off = arena.off
            Rb = [arena.alloc((4, 128), F32) for _ in range(2)] * 2
            R2b = [arena.alloc((4, 128), F32) for _ in range(2)] * 2
            Eb = [arena.alloc((4, 128), BF16) for _ in range(4)]
            MTb = [arena.alloc((4, 128), BF16) for _ in range(4)]
            xdt = [arena.alloc((4, 64), BF16) for _ in range(4)]
            xdt2 = [arena.alloc((4, 64), BF16) for _ in range(4)]
            r_R, r_R2, r_Eb, r_MT, r_x1_, r_x2 = RL(2) * 2, RL(2) * 2, RL(4), RL(4), RL(4), RL(4)
            ytmp = [arena.alloc((4, 64), F32) for _ in range(2)]
            r_yt = RL(2)
            Sst = [arena.alloc((256,), F32) for _ in range(2)]
            Sbf = [arena.alloc((256,), BF16) for _ in range(2)]
            r_S = RL(2)
            r_Sb = RL(2)
            nac_all = arena.alloc((18, 2, 4), F32)
            ea_all = arena.alloc((18, 2, 4), F32)
            dec_all = arena.alloc((18, 2, 4), F32)
            wst_all = arena.alloc((18, 2, 4), F32)
            pbh = [RL(2) for _ in range(8)]
            _fl = lambda a: a.rearrange("p a b -> p (a b)")
            _sz = 4 * (NTOK + 4)
            assert 4 * 2048 + 4 * 1024 >= _sz
            _o0 = arena.off
            arena.off = rb_off
            upad2 = arena.alloc((NTOK + 4,), F32)
            arena.off = _o0
            upads = [upad, upad2]
            r_ups = [r_up, Res()]
            S.op("pool", lambda e: e.memset(upad2, 0.0), writes=[r_ups[1]])
            fz_zs = [_fl(Rb[0])[:, 0:256], _fl(Rb[1])[:, 0:256], _fl(R2b[0])[:, 0:256]]
            fz_y = [_fl(Rb[0])[:, 256:512], _fl(Rb[1])[:, 256:512], _fl(R2b[0])[:, 256:512]]
            fz_yb = [_fl(Eb[0])[:, 0:256], _fl(Eb[1])[:, 0:256], _fl(Eb[2])[:, 0:256]]
            fz_jk = _fl(Eb[3])[:, 0:256]
            r_fzs = RL(3)
            r_fjk = Res()
            S.op("pool", lambda e: e.memset(upad, 0.0), writes=[r_up])

            def load_g_w(g, slot):
                xo = 4096 + g * 256
                for i, c0 in enumerate((xo, xo + 128, 4096 + 1024 + g * 128, 4096 + 1536 + g * 128)):
                    ldw(Wg4[slot][:, i, :, :], win_v[:, :, c0:c0 + 128], r_Wg4[slot])

            load_g_w(0, 0)
            for g in range(4):
                slot = 0
                Wg = Wg4[slot]
                ccs = (2 * g, 2 * g + 1, 8 + g, 12 + g)
                if g > 0:
                    S.op("pool", lambda e: e.memset(upad2[:, 0:1], 0.0), writes=[r_ups[1]])
                    S.op("pool", lambda e: e.memset(upad2[:, 257:259], 0.0), writes=[r_ups[1]])
                    S.op("pool", lambda e: e.memset(upad2[:, NTOK + 3:NTOK + 4], 0.0), writes=[r_ups[1]])
                for i in range(4):
                    cc = ccs[i]
                    up_ = upads[i % 2]
                    rup_ = r_ups[i % 2]
                    for kc in range(8):
                        mm(bank(6)[:, 0:L_CTX], Wg[:, i, kc, :], hcT[:, kc, :], kc == 0, kc == 7, [r_Wg4[slot]] + r_hcT, [pb[6]])
                    cp("act", up_[:, 1:1 + L_CTX], bank(6)[:, 0:L_CTX], [pb[6]], [rup_])
                    for t4 in range(4):
                        bk = 6 + (t4 + 1) % 2
                        for kc in range(8):
                            mm(bank(bk)[:, :], Wg[:, i, kc, :], hT[:, kc, t4 * 512:(t4 + 1) * 512], kc == 0, kc == 7,
                               [r_Wg4[slot]] + r_hT[t4 * 4:t4 * 4 + 4], [pb[bk]])
                        cp("act", up_[:, 259 + t4 * 512:259 + (t4 + 1) * 512], bank(bk)[:, :], [pb[bk]], [rup_])
                    n_ = NTOK + 2
                    ts("dve", cacc[:, 1:1 + n_], up_[:, 1:1 + n_], cwT[:, cc, 1:2], cbT[:, cc:cc + 1], ALU.mult, ALU.add, [rup_, r_c], [r_ca] + pb[0:5])
                    stt(cacc[:, 1:1 + n_], up_[:, 0:n_], cwT[:, cc, 0:1], cacc[:, 1:1 + n_], ALU.mult, ALU.add, [rup_, r_c, r_ca], [r_ca] + pb[0:5])
                    stt(cacc[:, 1:1 + n_], up_[:, 2:2 + n_], cwT[:, cc, 2:3], cacc[:, 1:1 + n_], ALU.mult, ALU.add, [rup_, r_c, r_ca], [r_ca] + pb[0:5])
                    dst = xTf[:, i, :] if i < 2 else (BTf if i == 2 else CTf)
                    rdst = r_xx if i < 2 else r_bc
                    act(dst[:, 0:L_CTX], cacc[:, 1:1 + L_CTX], AF.Silu, [r_ca] + pb[0:5], [rdst])
                    act(dst[:, L_CTX:NTOK], cacc[:, 259:259 + S_LEN], AF.Silu, [r_ca] + pb[0:5], [rdst])
                if g + 1 < 4:
                    load_g_w(g + 1, 0)
                for tl in range(18):
                    bk = (0, 1, 2, 3, 6, 7)[tl % 6]
                    cs = slice(tl * 128, (tl + 1) * 128)
                    tr(bankb(bk)[:, 0:128], xTf[:, 0, cs], identb[:], [r_xx, r_c], [pb[bk]], inc=False)
                    tr(bankb(bk)[:, 128:256], xTf[:, 1, cs], identb[:], [r_xx, r_c], [pb[bk]], inc=False)
                    tr(bankb(bk)[:, 256:384], BTf[:, cs], identb[:], [r_bc, r_c], [pb[bk]], inc=True)
                    cp("act", x_tok[:, tl, :], bankb(bk)[:, 0:256], [pb[bk]], [r_tok])
                    cp("dve", B_tok[:, tl, :], bankb(bk)[:, 256:384], [pb[bk]], [r_tok])

                ldw(Wz, win_v[:, :, 3 * D + g * 256:3 * D + (g + 1) * 256], r_Wz)
                for d_ in range(2):
                    S.op("pool", lambda e, d_=d_: e.memset(Sst[d_], 0.0), writes=[r_S[d_]])
                    S.op("pool", lambda e, d_=d_: e.memset(Sbf[d_], 0.0), writes=[r_S[d_]])
                dtg = dt_all.rearrange("p t (d h) -> p t d h", d=2, h=16)[:, :, :, g * 4:(g + 1) * 4]
                dtag = dta_all.rearrange("p t (d h) -> p t d h", d=2, h=16)[:, :, :, g * 4:(g + 1) * 4]
                for tl in range(18):
                    for d_ in range(2):
                        tri = cst["trif"] if d_ == 0 else cst["trib"]
                        mm(bank(0)[:, tl * 8 + d_ * 4:tl * 8 + d_ * 4 + 4], tri[:], dtag[:, tl, d_, :], True, True, [r_c, r_dt], [pb[0]], inc=False)
                        mm(bank(1)[:, tl * 8 + d_ * 4:tl * 8 + d_ * 4 + 4], cst["ones"][:], dtag[:, tl, d_, :], True, True, [r_c, r_dt], [pb[1]],
                           inc=(tl == 17 and d_ == 1))
                pa = bank(0)[:, 0:144].rearrange("p (t d h) -> p t d h", t=18, d=2, h=4)
                pt_ = bank(1)[:, 0:144].rearrange("p (t d h) -> p t d h", t=18, d=2, h=4)
                r_sm = Res()
                ts("dve", nac_all, pa, -1.0, None, ALU.mult, None, [pb[0]], [r_sm])
                act(ea_all, pa, AF.Exp, [pb[0]], [r_sm])
                act(dec_all, pt_, AF.Exp, [pb[1]], [r_sm])
                tt("dve", wst_all, pt_, nac_all, ALU.add, [pb[1], r_sm], [r_sm])
                act(wst_all, wst_all, AF.Exp, [r_sm], [r_sm])
                tt("dve", wst_all, wst_all, dtg, ALU.mult, [r_sm, r_dt], [r_sm])
                order = [[0, 1] + list(range(2, 18)), [1, 0] + list(range(17, 1, -1))]
                S.barrier()

                def pre(step, d_):
                    tl = order[d_][step]
                    lat = tl >= 2
                    par = step % 2
                    k_ = d_ * 2 + par
                    cs = slice(tl * 128, (tl + 1) * 128)
                    tri = cst["trif"] if d_ == 0 else cst["trib"]
                    neg = cst["negf"] if d_ == 0 else cst["negb"]
                    bA = 2 * k_
                    bB = 2 * k_ + 1
                    xv = x_tok[:, tl, :].rearrange("p (a b) -> p a b", a=4, b=64)
                    tt("dve", xdt2[k_], xv, wst_all[:, tl, d_, :].unsqueeze(2).to_broadcast([128, 4, 64]), ALU.mult, [r_tok, r_sm], [r_x2[k_]])
                    yield
                    mm(bank(bB)[:, 256:512], B_tok[:, tl, :], xdt2[k_].rearrange("p a b -> p (a b)"), True, True, [r_tok, r_x2[k_]], [pbh[bB][1]])
                    yield
                    if lat:
                        tt("pool", Rb[d_], tri[:, :].unsqueeze(1).to_broadcast([128, 4, 128]),
                           dtag[:, tl, d_, :].unsqueeze(2).to_broadcast([128, 4, 128]), ALU.mult, [r_c, r_dt], [r_R[d_]])
                        yield
                        tt("pool", R2b[d_], neg[:, :].unsqueeze(1).to_broadcast([128, 4, 128]),
                           nac_all[:, tl, d_, :].unsqueeze(2).to_broadcast([128, 4, 128]), ALU.add, [r_c, r_sm], [r_R2[d_]])
                        yield
                        mm(bank(bA)[:, :], cst["ones"][:], Rb[d_].rearrange("p a b -> p (a b)"), True, False, [r_c, r_R[d_]], pbh[bA], inc=False)
                        mm(bank(bA)[:, :], ident[:], R2b[d_].rearrange("p a b -> p (a b)"), False, True, [r_c, r_R2[d_]], pbh[bA])
                        yield
                        mm(bank(bB)[:, 0:128], BTf[:, cs], CTf[:, cs], True, True, [r_bc], [pbh[bB][0]])
                        yield
                        act(Eb[k_].rearrange("p a b -> p (a b)"), bank(bA)[:, :], AF.Exp, pbh[bA], [r_Eb[k_]])
                        yield
                        tt("dve", MTb[k_], Eb[k_], bank(bB)[:, 0:128].unsqueeze(1).to_broadcast([128, 4, 128]), ALU.mult, [r_Eb[k_], pbh[bB][0]], [r_MT[k_]])
                        yield
                        tt("dve", xdt[k_], xv, dtg[:, tl, d_, :].unsqueeze(2).to_broadcast([128, 4, 64]), ALU.mult, [r_tok, r_dt], [r_x1_[k_]])
                        yield
                        for h in range(4):
                            mm(bank(bA)[:, h * 64:(h + 1) * 64], MTb[k_][:, h, :], xdt[k_][:, h, :], True, True, [r_MT[k_], r_x1_[k_]], [pbh[bA][0]], inc=(h == 3))
                        yield

                def dep(step, d_):
                    tl = order[d_][step]
                    lat = tl >= 2
                    par = step % 2
                    k_ = d_ * 2 + par
                    cs = slice(tl * 128, (tl + 1) * 128)
                    bA = 2 * k_
                    bB = 2 * k_ + 1
                    if lat:
                        t = tl - 2
                        mm(bank(bA)[:, 256:512], CTf[:, cs], Sbf[d_], True, True, [r_bc, r_Sb[d_]], [pbh[bA][1]])
                        yield
                    Sv = Sst[d_].rearrange("p (a b) -> p a b", a=4, b=64)
                    tt("dve", Sv, Sv, dec_all[:, tl, d_, :].unsqueeze(2).to_broadcast([128, 4, 64]), ALU.mult, [r_S[d_], r_sm], [r_S[d_]])
                    yield
                    tt("dve", Sst[d_], Sst[d_], bank(bB)[:, 256:512], ALU.add, [r_S[d_], pbh[bB][1]], [r_S[d_]])
                    yield
                    cp("act", Sbf[d_], Sst[d_], [r_S[d_]], [r_Sb[d_]])
                    yield
                    if lat:
                        tt("dve", ytmp[d_], bank(bA)[:, 256:512].rearrange("p (a b) -> p a b", a=4, b=64),
                           ea_all[:, tl, d_, :].unsqueeze(2).to_broadcast([128, 4, 64]), ALU.mult, [pbh[bA][1], r_sm], [r_yt[d_]])
                        yield
                        tt("dve", ydir[d_][:, t, :], ytmp[d_].rearrange("p a b -> p (a b)"), bank(bA)[:, 0:256], ALU.add,
                           [r_yt[d_], pbh[bA][0]], [r_yd[d_][t]] + ([r_xx] if d_ == 0 else []))
                        yield

                def rr(gens):
                    while gens:
                        nxt = []
                        for g_ in gens:
                            try:
                                next(g_)
                                nxt.append(g_)
                            except StopIteration:
                                pass
                        gens = nxt

                rr([pre(0, 0), pre(0, 1)])
                for step in range(18):
                    gl = [dep(step, 0), dep(step, 1)]
                    if step + 1 < 18:
                        gl = [pre(step + 1, 0), pre(step + 1, 1)] + gl
                    rr(gl)
                    conv_some(1)
                S.barrier()
                pass
                def fin(t):
                    i = t % 3
                    cs = slice(t * 128, (t + 1) * 128)
                    zs_, yf_, ybf_, jk_ = fz_zs[i], fz_y[i], fz_yb[i], fz_jk
                    rz = r_fzs[i]
                    scl = sc4[0][:, 4 + i, :]
                    for kc in range(8):
                        mm(bank(i)[:, 0:256], hT[:, kc, cs], Wz[:, kc, :], kc == 0, kc == 7, [r_Wz, r_hT[t]], [pb[i]])
                    yield
                    act(zs_, bank(i)[:, 0:256], AF.Silu, [pb[i]], [rz])
                    yield
                    xv = x_tok[:, t + 2, :].rearrange("p (a b) -> p a b", a=4, b=64)
                    tt("pool", yf_.rearrange("p (a b) -> p a b", a=4, b=64), xv, dskB[:, g * 4:(g + 1) * 4].unsqueeze(2).to_broadcast([128, 4, 64]),
                       ALU.mult, [r_tok, r_c], [rz])
                    yield
                    tt("dve", yf_, yf_, ydir[0][:, t, :], ALU.add, [rz, r_yd[0][t], r_xx], [rz])
                    yield
                    tt("dve", yf_, yf_, ydir[1][:, t, :], ALU.add, [rz, r_yd[1][t]], [rz])
                    yield
                    tt("dve", yf_, yf_, zs_, ALU.mult, [rz], [rz])
                    yield
                    act(jk_, yf_, AF.Square, [rz], [r_fjk, rz], accum_out=scl[:, 0:1])
                    yield
                    ts("dve", scl[:, 1:2], scl[:, 0:1], 1.0 / 256, 1e-6, ALU.mult, ALU.add, [rz], [rz])
                    yield
                    act(scl[:, 1:2], scl[:, 1:2], AF.Ln, [rz], [rz])
                    yield
                    act(scl[:, 2:3], scl[:, 1:2], AF.Exp, [rz], [rz], scale=-0.5)
                    yield
                    stt(ybf_, yf_, scl[:, 2:3], gssdB[:, g * 256:(g + 1) * 256], ALU.mult, ALU.mult, [rz, r_g3], [rz])
                    yield
                    tr(bankb(3 + i)[:, 0:128], ybf_[:, 0:128], identb[:], [rz, r_c], [pb[3 + i]], inc=False)
                    tr(bankb(3 + i)[:, 128:256], ybf_[:, 128:256], identb[:], [rz, r_c], [pb[3 + i]], inc=True)
                    yield
                    cp("act", yT[:, 2 * g:2 * g + 2, cs], bankb(3 + i)[:, 0:256].rearrange("p (a b) -> p a b", a=2, b=128), [pb[3 + i]], [r_yT[t]])
                    yield

                active = []
                nxt_ = 0
                while nxt_ < NT or active:
                    if len(active) < 3 and nxt_ < NT:
                        active.append(fin(nxt_))
                        nxt_ += 1
                    new_ = []
                    for g_ in active:
                        try:
                            next(g_)
                            new_.append(g_)
                        except StopIteration:
                            pass
                    active = new_
                S.barrier()
            if dbg and "yT" in dbg and b == 0:
                S.barrier()
                arena.off = a1
                dtmp = arena.alloc((2, S_LEN), F32)
                r_d = Res()
                for kk in range(4):
                    cp("dve", dtmp, yT[:, kk * 2:kk * 2 + 2, :], r_yT, [r_d])
                    S.dma("sp", dbg_d["yT"][kk * 256:(kk + 1) * 256, :].rearrange("(k p) t -> p k t", p=128), dtmp, reads=[r_d])
            if stop == "p3":
                break
            S.barrier()
            arena.off = a1
            oaT = arena.alloc((8, S_LEN), BF16)
            r_oaT = RL(4)
            a2 = arena.off
            cosT = arena.alloc((S_LEN,), F32)
            sinT = arena.alloc((S_LEN,), F32)
            r_cs = Res()
            S.dma("sp", cosT, Cd["cosT"], writes=[r_cs])
            S.dma("sp", sinT, Cd["sinT"], writes=[r_cs])
            wq = [arena.alloc((3, 8, 128), BF16) for _ in range(2)]
            qpre = arena.alloc((512,), BF16)
            r_qpre = Res()
            tmpk1 = arena.alloc((512,), F32)
            tmpk2 = arena.alloc((512,), F32)
            qprek = arena.alloc((512,), BF16)
            r_tk = RL(3)
            r_wq = RL(2)
            qT = arena.alloc((2, S_LEN), BF16)
            r_qT = RL(4)
            S.op("pool", lambda e: e.memset(qT, 0.0), writes=r_qT)
            kT = arena.alloc((L_CTX + S_LEN,), BF16)
            r_kT = RL(5)
            va = arena.alloc((18, 128), BF16)
            r_va = Res()
            onesb = arena.alloc((128,), BF16)
            cp("dve", onesb, cst["ones"][:], [r_c], [r_va])
            Et = [arena.alloc((512,), BF16) for _ in range(3)]
            r_E = RL(3)
            fo = arena.alloc((512,), F32)
            ft = arena.alloc((512,), F32)
            fr = arena.alloc((512,), F32)
            ftb = arena.alloc((512,), BF16)
            r_f = Res()
            tmp1, tmp2 = fr, ft
            r_tmp = [r_f, r_f]
            S.dma("sp", gsubT[:], Wd["g_attn_subln"].rearrange("(p o) -> p o", o=1), writes=[r_f])
            ts("dve", gsubT[:], gsubT[:], 0.8, None, ALU.mult, None, [r_f], [r_f])

            wstg = [arena.alloc((8, 128), F32)] * 2
            r_wstg = [Res()] * 2
            stg_i = [0]

            def load_head_w(hd, slot):
                c0 = hd * 128
                for (wi, base) in ((0, c0), (1, D + c0), (2, 2 * D + c0)):
                    si = stg_i[0] % 2
                    stg_i[0] += 1
                    S.dma("sp", wstg[si], win_v[:, :, base:base + 128], writes=[r_wstg[si]])
                    cp("pool", wq[slot][:, wi, :, :], wstg[si], [r_wstg[si]], [r_wq[slot]])

            load_head_w(0, 0)
            for hd in range(8):
                slot = hd % 2
                if hd + 1 < 8:
                    load_head_w(hd + 1, 1 - slot)
                W = wq[slot]
                rW = r_wq[slot]
                def gen_qk(is_q):
                    wi = 0 if is_q else 1
                    bA, bB = (6, 7) if is_q else (0, 1)
                    t1_, t2_, qp_ = (tmp1, tmp2, qpre) if is_q else (tmpk1, tmpk2, qprek)
                    rt1, rt2, rqp = (r_tmp[0], r_tmp[1], r_qpre) if is_q else (r_tk[0], r_tk[1], r_tk[2])
                    if not is_q:
                        for kc in range(8):
                            mm(bank(bA)[:, 0:L_CTX], W[:, 1, kc, :], hcT[:, kc, :], kc == 0, kc == 7, [rW] + r_hcT, [pb[bA]])
                        yield
                        cp("act", kT[:, 0:L_CTX], bank(bA)[:, 0:L_CTX], [pb[bA]], [r_kT[0]])
                        yield
                    for t4 in range(4):
                        cols = slice(t4 * 512, (t4 + 1) * 512)
                        for kc in range(8):
                            mm(bank(bA)[:, :], W[:, wi, kc, :], hT[:, kc, cols], kc == 0, kc == 7, [rW] + r_hT[t4 * 4:t4 * 4 + 4], [pb[bA]])
                        yield
                        cp("dve", qp_, bank(bA)[:, :], [pb[bA]], [rqp])
                        yield
                        mm(bank(bB)[:, :], pswapb[:], qp_, True, True, [r_c, rqp], [pb[bB]])
                        yield
                        tt("dve", t1_, bank(bA)[:, :], cosT[:, cols], ALU.mult, [pb[bA], r_cs], [rt1])
                        yield
                        tt("dve", t2_, bank(bB)[:, :], sinT[:, cols], ALU.mult, [pb[bB], r_cs], [rt2])
                        yield
                        if is_q:
                            tt("pool", qT[0:64, 0, cols], t1_[0:64, :], t2_[0:64, :], ALU.add, [rt1, rt2], [r_qT[t4]])
                            tt("dve", qT[64:128, 1, cols], t1_[64:128, :], t2_[64:128, :], ALU.add, [rt1, rt2], [r_qT[t4]])
                        else:
                            tt("pool", kT[:, L_CTX + t4 * 512:L_CTX + (t4 + 1) * 512], t1_, t2_, ALU.add, [rt1, rt2], [r_kT[1 + t4]])
                        yield

                def gen_v():
                    for vt in range(18):
                        bk = 2 + (vt % 4)
                        for kc in range(8):
                            lhs = hcT[:, kc, vt * 128:(vt + 1) * 128] if vt < 2 else hT[:, kc, (vt - 2) * 128:(vt - 1) * 128]
                            rr_ = [r_hcT[vt]] if vt < 2 else [r_hT[vt - 2]]
                            mm(bank(bk)[:, 0:128], lhs, W[:, 2, kc, :], kc == 0, kc == 7, [rW] + rr_, [pb[bk]])
                        yield
                        cp("act", va[:, vt, :], bank(bk)[:, 0:128], [pb[bk]], [r_va])
                        yield

                gens_ = [gen_qk(True), gen_qk(False), gen_v()]
                while gens_:
                    nx_ = []
                    for g_ in gens_:
                        try:
                            next(g_)
                            nx_.append(g_)
                        except StopIteration:
                            pass
                    gens_ = nx_
                SB_ = (0, 1, 7)
                LA = 2
                its = [(qt, c_, kc) for qt in range(4) for c_ in range(2) for kc in range(18)]

                def emit_S(i):
                    qt, c_, kc = its[i]
                    prt = slice(c_ * 64, c_ * 64 + 64)
                    sb_ = SB_[i % 3]
                    kr = [r_kT[0]] if kc < 2 else [r_kT[1 + (kc - 2) // 4]]
                    mm(bank(sb_)[:, :], kT[:, kc * 128:(kc + 1) * 128], qT[:, c_, qt * 512:(qt + 1) * 512], True, True,
                       kr + [r_qT[qt]], [pb[sb_]])
                    act(Et[i % 3], bank(sb_)[:, :], AF.Exp, [pb[sb_]], [r_E[i % 3]], scale=0.125)

                pending = []
                for i in range(LA):
                    emit_S(i)
                for i in range(len(its)):
                    if i + LA < len(its):
                        emit_S(i + LA)
                    qt, c_, kc = its[i]
                    qcols = slice(qt * 512, (qt + 1) * 512)
                    bo = 2 + 2 * c_
                    E = Et[i % 3]
                    rE = r_E[i % 3]
                    mm(bank(bo)[:, :], va[:, kc, :], E, kc == 0, kc == 17, [rE, r_va], [pb[bo]])
                    mm(bank(bo + 1)[:, :], onesb, E, kc == 0, kc == 17, [rE, r_va], [pb[bo + 1]])
                    if kc == 8:
                        conv_some(1)
                    if kc == 17 and c_ == 0:
                        recip(fr, bank(3)[:, :], [pb[3]], [r_f])
                        tt("dve", fo, bank(2)[:, :], fr, ALU.mult, [pb[2], r_f], [r_f])
                    if kc == 17 and c_ == 1:
                        recip(fr, bank(5)[:, :], [pb[5]], [r_f])
                        tt("dve", ft, bank(4)[:, :], fr, ALU.mult, [pb[4], r_f], [r_f])
                        stt(fo, ft, nlam, fo, ALU.mult, ALU.add, [r_f, r_c], [r_f])
                        tt("pool", ftb, fo, fo, ALU.mult, [r_f], [r_f])

                        def partB(qcols=qcols, qt=qt):
                            mm(bank(6)[:, :], onesb, ftb, True, True, [r_f, r_va], [pb[6]])
                            ts("dve", fr, bank(6)[:, :], 1.0 / 128, 1e-6, ALU.mult, ALU.add, [pb[6]], [r_f])
                            act(fr, fr, AF.Ln, [r_f], [r_f])
                            act(fr, fr, AF.Exp, [r_f], [r_f], scale=-0.5)
                            tt("dve", fo, fo, fr, ALU.mult, [r_f], [r_f])
                            ts("dve", oaT[:, hd, qcols], fo, gsubT[:, 0:1], None, ALU.mult, None, [r_f], [r_oaT[qt]])
                        pending.append((i + 8, partB))
                    while pending and (pending[0][0] <= i or i == len(its) - 1):
                        pending.pop(0)[1]()
            if dbg and "oaT" in dbg and b == 0:
                S.barrier()
                arena.off = a2
                dtmp = arena.alloc((2, S_LEN), F32)
                r_d = Res()
                for kk in range(4):
                    cp("dve", dtmp, oaT[:, kk * 2:kk * 2 + 2, :], r_oaT, [r_d])
                    S.dma("sp", dbg_d["oaT"][kk * 256:(kk + 1) * 256, :].rearrange("(k p) t -> p k t", p=128), dtmp, reads=[r_d])
            if stop == "p2":
                break
            S.barrier()
            arena.off = a2


            mT = arena.alloc((8, S_LEN), BF16)
            r_mT = RL(4)
            a3 = arena.off
            Wm = [arena.alloc((4, 8, 128), BF16) for _ in range(2)]
            r_Wm = RL(2)
            sg = [arena.alloc((512,), F32) for _ in range(2)]
            t12 = [arena.alloc((512,), F32) for _ in range(2)]
            r_sg = RL(2)
            r_t12 = RL(2)
            wba_v = Wd["w_branch_attn"].rearrange("(k p) n -> p k n", p=128)
            wbs_v = Wd["w_branch_ssd"].rearrange("(k p) n -> p k n", p=128)
            wout_v = Wd["w_out"].rearrange("(k p) n -> p k n", p=128)

            mstg = [arena.alloc((8, 128), F32) for _ in range(2)]
            r_mstg = RL(2)
            mstg_i = [0]

            def load_m_w(m, slot):
                cs_ = slice(m * 128, (m + 1) * 128)
                srcs_ = (wba_v[:, :, cs_], wbs_v[:, :, cs_], win_v[:, :, 6176 + m * 128:6176 + (m + 1) * 128],
                         win_v[:, :, 6176 + D + m * 128:6176 + D + (m + 1) * 128])
                for i_, src_ in enumerate(srcs_):
                    si = mstg_i[0] % 2
                    mstg_i[0] += 1
                    S.dma("sp", mstg[si], src_, writes=[r_mstg[si]])
                    cp("act" if i_ % 2 == 0 else "dve", Wm[slot][:, i_, :, :], mstg[si], [r_mstg[si]], [r_Wm[slot]])

            load_m_w(0, 0)
            it = 0
            for m in range(8):
                slot = m % 2
                if m + 1 < 8:
                    load_m_w(m + 1, 1 - slot)
                for t4 in range(4):
                    cols = slice(t4 * 512, (t4 + 1) * 512)
                    bb = 4 * (it % 2)
                    it += 1
                    srcs = ((oaT, [r_oaT[t4]]), (yT, r_yT[t4 * 4:t4 * 4 + 4]), (hT, r_hT[t4 * 4:t4 * 4 + 4]), (hT, r_hT[t4 * 4:t4 * 4 + 4]))
                    for i in range(4):
                        for kc in range(8):
                            mm(bank(bb + i)[:, :], Wm[slot][:, i, kc, :], srcs[i][0][:, kc, cols], kc == 0, kc == 7,
                               [r_Wm[slot]] + srcs[i][1], [pb[bb + i]])
                    for i in range(2):
                        act(sg[i], bank(bb + 2 + i)[:, :], AF.Sigmoid, [pb[bb + 2 + i]], [r_sg[i]])
                        tt("dve", t12[i], bank(bb + i)[:, :], sg[i], ALU.mult, [pb[bb + i], r_sg[i]], [r_t12[i]])
                    tt("dve", mT[:, m, cols], t12[0], t12[1], ALU.add, r_t12, [r_mT[t4]])
                    conv_some(2)
            S.barrier()
            arena.off = a1
            Wout = arena.alloc((8, D), BF16)
            r_Wout = Res()
            for hf in range(2):
                ldw(Wout[:, :, hf * 512:(hf + 1) * 512], wout_v[:, :, hf * 512:(hf + 1) * 512], r_Wout)
            xt4 = [arena.alloc((D,), F32) for _ in range(2)]
            r_xt4 = RL(2)
            x1t = [arena.alloc((D,), F32) for _ in range(2)]
            r_x1t = RL(2)
            assert arena.off <= a2
            arena.off = a3
            tmpfs = [arena.alloc((D,), F32) for _ in range(2)]
            r_tmpfs = RL(2)
            h2fs = [arena.alloc((8, 128), F32) for _ in range(2)]
            r_h2fs = RL(2)
            st4b = arena.alloc((8,), F32)
            st4s = [st4, st4b]
            r_st4s = RL(2)
            r_Wr = RL(NT)
            r_st4 = Res()
            r_rt = Res()
            r_x1 = RL(NT)
            if SPARSE:
                h2tfs = [arena.alloc((D,), F32)] * 2
                r_h2tfs = [Res()] * 2
                A2R = arena.alloc((D,), F32)
                B2R = arena.alloc((D,), F32)
                r_AR = Res()
                diag = arena.alloc((128,), F32)
                r_diag = Res()
                rank_all = arena.alloc((NT, 64), F32)
                r_rank = Res()
                maskb = arena.alloc((64,), BF16)
                r_mk = Res()
                a4 = arena.off
                arena.off = a1 - 8 * S_LEN * 2
                h2tok = arena.alloc((NT, D), BF16)
                arena.off = a4
                r_h2tok = RL(NT)
                for (dstR, idx_) in ((A2R, 2), (B2R, 3)):
                    for kc in range(8):
                        ts("dve", diag, ident[:], AB[:, b, idx_, kc:kc + 1], None, ALU.mult, None, [r_c, r_m], [r_diag])
                        bk = 6 + kc // 4
                        mm(bank(bk)[:, (kc % 4) * 128:(kc % 4) * 128 + 128], cst["ones"][:], diag, True, True, [r_c, r_diag], [pb[bk]])
                    cp("dve", dstR[:, 0:512], bank(6)[:, :], [pb[6]], [r_AR])
                    cp("act", dstR[:, 512:1024], bank(7)[:, :], [pb[7]], [r_AR])
                S.op("pool", lambda e: e.memset(csum[:], 0.0), writes=[r_rank])
            A2v = AB[:, b, 2, :]
            B2v = AB[:, b, 3, :]
            def partA(t):
                bi = t % 2
                sl = t % 2
                bo_ = 0 if sl == 0 else 4
                rb_ = bo_
                tmpf, h2f, h2tf = tmpfs[sl], h2fs[sl], h2tfs[sl]
                r_tmpf, r_h2f, r_h2tf = r_tmpfs[sl], r_h2fs[sl], r_h2tfs[sl]
                st4 = st4s[sl]
                r_st4 = r_st4s[sl]
                cs = slice(t * 128, (t + 1) * 128)
                S.dma("sp", xt4[bi], x_d[b, cs, :], writes=[r_xt4[bi]])
                for hf in range(2):
                    for kc in range(8):
                        mm(bank(bo_ + hf)[:, :], mT[:, kc, cs], Wout[:, kc, hf * 512:(hf + 1) * 512], kc == 0, kc == 7,
                           [r_mT[t // 4], r_Wout], [pb[bo_ + hf]])
                yield
                for hf in range(2):
                    act(tmpf[:, hf * 512:(hf + 1) * 512], bank(bo_ + hf)[:, :], AF.Square, [pb[bo_ + hf]], [r_tmpf, r_st4], accum_out=st4[:, hf:hf + 1])
                yield
                tt("dve", st4[:, 2:3], st4[:, 0:1], st4[:, 1:2], ALU.add, [r_st4], [r_st4])
                yield
                ts("dve", st4[:, 3:4], st4[:, 2:3], 1.0 / D, 1e-6, ALU.mult, ALU.add, [r_st4], [r_st4])
                yield
                act(st4[:, 3:4], st4[:, 3:4], AF.Ln, [r_st4], [r_st4])
                yield
                act(st4[:, 4:5], st4[:, 3:4], AF.Exp, [r_st4], [r_st4], scale=-0.5)
                yield
                for hf in range(2):
                    hs_ = slice(hf * 512, (hf + 1) * 512)
                    stt(tmpf[:, hs_], bank(bo_ + hf)[:, :], st4[:, 4:5], G1[:, b, hs_], ALU.mult, ALU.mult, [pb[bo_ + hf], r_st4, r_m], [r_tmpf])
                    yield
                tt("dve", x1t[bi], tmpf, xt4[bi], ALU.add, [r_tmpf, r_xt4[bi]], [r_x1t[bi]])
                yield
                S.dma("sp", out_d[b, cs, :], x1t[bi], reads=[r_x1t[bi]], writes=[r_x1[t]])
                act(tmpf, x1t[bi], AF.Square, [r_x1t[bi]], [r_tmpf, r_st4], accum_out=st4[:, 5:6])
                yield
                ts("dve", st4[:, 6:7], st4[:, 5:6], 1.0 / D, 1e-6, ALU.mult, ALU.add, [r_st4], [r_st4])
                yield
                act(st4[:, 6:7], st4[:, 6:7], AF.Ln, [r_st4], [r_st4])
                yield
                act(st4[:, 7:8], st4[:, 6:7], AF.Exp, [r_st4], [r_st4], scale=-0.5)
                yield
                ts("dve", tmpf, x1t[bi], st4[:, 7:8], None, ALU.mult, None, [r_x1t[bi], r_st4], [r_tmpf])
                yield
                if SPARSE:
                    tt("dve", h2tf, tmpf, A2R, ALU.mult, [r_tmpf, r_AR], [r_h2tf])
                    tt("pool", h2tok[:, t, :], h2tf, B2R, ALU.add, [r_h2tf, r_AR], [r_h2tok[t]])
                    yield
                for kc in range(8):
                    bk = bo_ + 2 + kc // 4
                    tr(bank(bk)[:, (kc % 4) * 128:(kc % 4) * 128 + 128], tmpf[:, kc * 128:(kc + 1) * 128], ident[:],
                       [r_tmpf, r_c], [pb[bk]], inc=(kc % 4 == 3))
                yield
                pv = PS[bo_ // 2 + 1][:, :].rearrange("p (a b) -> p a b", a=8, b=128)
                tt("dve", h2f, pv, A2v.unsqueeze(2).to_broadcast([128, 8, 128]), ALU.mult, [pb[bo_ + 2], pb[bo_ + 3], r_m], [r_h2f])
                yield
                tt("dve", h2f, h2f, B2v.unsqueeze(2).to_broadcast([128, 8, 128]), ALU.add, [r_h2f, r_m], [r_h2f])
                yield
                cp("act", hT[:, :, cs], h2f, [r_h2f], [r_hT[t]])
                for kc in range(8):
                    mm(bank(rb_)[:, 0:64], h2f[:, kc, :], wrt[:, kc, :], kc == 0, kc == 7, [r_h2f, r_c], [pb[rb_]])
                yield

            def partB(t):
                rb_ = 0 if t % 2 == 0 else 4
                kb_ = 1 if t % 2 == 0 else 5
                sc_ = rt[:, 0, :]
                sel_ = rt[:, 1, :]
                act(sc_, bank(rb_)[:, 0:64], AF.Exp, [pb[rb_]], [r_rt], scale=-1.0)
                ts("dve", sc_, sc_, 1.0, None, ALU.add, None, [r_rt], [r_rt])
                recip(sc_, sc_, [r_rt], [r_rt])
                tt("dve", sel_, sc_, rbiasB[:, :], ALU.add, [r_rt, r_c], [r_rt])
                sel3v = sel_.rearrange("p (a b) -> p a b", a=8, b=8)
                red(rt8[:, 0, :], sel3v, ALU.max, [r_rt], [r_rt])
                tt("dve", rt[:, 2, :].rearrange("p (a b) -> p a b", a=8, b=8), sel3v,
                   rt8[:, 0, :].unsqueeze(2).to_broadcast([128, 8, 8]), ALU.is_equal, [r_rt], [r_rt])
                stt(rt[:, 2, :], rt[:, 2, :], -BIG, sel_, ALU.mult, ALU.add, [r_rt], [r_rt])
                red(rt8[:, 1, :], rt[:, 2, :].rearrange("p (a b) -> p a b", a=8, b=8), ALU.max, [r_rt], [r_rt])
                tt("dve", rt8[:, 2, :], rt8[:, 0, :], rt8[:, 1, :], ALU.add, [r_rt], [r_rt])
                S.op("dve", lambda e: e.max(rt8[:, 3, :], rt8[:, 2, :]), reads=[r_rt], writes=[r_rt])
                ts("dve", rt8[:, 4, :], rt8[:, 2, :], rt8[:, 3, 3:4], None, ALU.is_ge, None, [r_rt], [r_rt])
                ts("dve", rt8[:, 4, :], rt8[:, 4, :], -1.0, BIG, ALU.add, ALU.mult, [r_rt], [r_rt])
                tt("dve", rt[:, 3, :].rearrange("p (a b) -> p a b", a=8, b=8), sel3v,
                   rt8[:, 4, :].unsqueeze(2).to_broadcast([128, 8, 8]), ALU.add, [r_rt], [r_rt])
                S.op("dve", lambda e: e.max(rt8[:, 5, :], rt[:, 3, :]), reads=[r_rt], writes=[r_rt])
                ts("dve", rt[:, 4, :], rt[:, 3, :], rt8[:, 5, 7:8], None, ALU.is_ge, None, [r_rt], [r_rt])
                tt("dve", rt[:, 4, :], rt[:, 4, :], sc_, ALU.mult, [r_rt], [r_rt])
                red(sm[:, 8:9], rt[:, 4, :], ALU.add, [r_rt], [r_rt])
                recip(sm[:, 9:10], sm[:, 8:9], [r_rt], [r_rt])
                ts("dve", Wr_all[:, t, :], rt[:, 4, :], sm[:, 9:10], 2.5, ALU.mult, ALU.mult, [r_rt], [r_Wr[t]])
                if SPARSE:
                    ts("dve", maskb, Wr_all[:, t, :], 0.0, None, ALU.is_gt, None, [r_Wr[t]], [r_mk])
                    mm(bank(kb_)[:, 0:64], trisb[:], maskb, True, True, [r_c, r_mk], [pb[kb_]], inc=False)
                    mm(bank(kb_)[:, 64:128], onesbp[:], maskb, True, True, [r_c, r_mk], [pb[kb_]])
                    tt("dve", rank_all[:, t, :], bank(kb_)[:, 0:64], csum[:], ALU.add, [pb[kb_], r_rank], [r_rank])
                    tt("dve", csum[:], csum[:], bank(kb_)[:, 64:128], ALU.add, [pb[kb_], r_rank], [r_rank])
            act_ = []
            nt_ = 0
            while nt_ < NT or act_:
                while len(act_) < 2 and nt_ < NT:
                    act_.append((nt_, partA(nt_)))
                    nt_ += 1
                nw_ = []
                for (t_, g_) in act_:
                    try:
                        next(g_)
                        nw_.append((t_, g_))
                    except StopIteration:
                        partB(t_)
                act_ = nw_
            h2T = hT
            r_h2T = r_hT
            if dbg and "Wr" in dbg and b == 0:
                S.dma("sp", dbg_d["Wr"].rearrange("(t p) e -> p t e", p=128), Wr_all[:], reads=r_Wr)
            if dbg and "h2T" in dbg and b == 0:
                S.barrier()
                arena.off = a1
                dtmp = arena.alloc((2, S_LEN), F32)
                r_d = Res()
                for kk in range(4):
                    cp("dve", dtmp, h2T[:, kk * 2:kk * 2 + 2, :], r_h2T, [r_d])
                    S.dma("sp", dbg_d["h2T"][kk * 256:(kk + 1) * 256, :].rearrange("(k p) t -> p k t", p=128), dtmp, reads=[r_d])
            if stop == "p4":
                break
            if SPARSE:
                r_sp = Res()
                r_d8 = Res()
                r_w8 = Res()
                r_xbuf = Res()
                r_ybuf = Res()
                r_eb = Res()
                ones64 = cst["ones"][:, 0:64]
                S.barrier()
                arena.off = a1
                ts("dve", sp64[:, 0, :], csum[:], 255.0, None, ALU.add, None, [r_rank], [r_sp])
                cp("dve", sp64i[:], sp64[:, 0, :], [r_sp], [r_sp])
                ts("dve", sp64i[:], sp64i[:], 8, None, ALU.arith_shift_right, None, [r_sp], [r_sp])
                ts("dve", sp64i[:], sp64i[:], 8, None, ALU.logical_shift_left, None, [r_sp], [r_sp])
                cp("dve", sp64[:, 1, :], sp64i[:], [r_sp], [r_sp])
                S.op("dve", lambda e: e.tensor_tensor_scan(sp64[:, 2, :], ones64, sp64[:, 1, :], 0.0, ALU.mult, ALU.add),
                     reads=[r_sp, r_c], writes=[r_sp])
                tt("dve", sp64[:, 3, :], sp64[:, 2, :], sp64[:, 1, :], ALU.subtract, [r_sp], [r_sp])
                cmpb = arena.alloc((32, 64), F32)
                ebf = arena.alloc((NBLK,), F32)
                for ch in range(NBLK // 32):
                    tt("dve", cmpb, sp64[:, 2, :].unsqueeze(1).to_broadcast([128, 32, 64]),
                       cst["jtab"][:, ch * 32:(ch + 1) * 32].unsqueeze(2).to_broadcast([128, 32, 64]), ALU.is_le, [r_sp, r_c], [r_sp])
                    red(ebf[:, ch * 32:(ch + 1) * 32], cmpb, ALU.add, [r_sp], [r_sp])
                ts("dve", ebf, ebf, 63.0, None, ALU.min, None, [r_sp], [r_sp])
                ts("dve", ebf, ebf, 128.0, cst["pidx"][:, 0:1], ALU.mult, ALU.add, [r_sp, r_c], [r_sp])
                cp("dve", eb_i[:], ebf, [r_sp], [r_eb])
                dm = arena.alloc((NT, 64), F32)
                mk = arena.alloc((NT, 64), F32)
                d8a = arena.alloc((NT, 8), F32)
                tt("dve", dm, rank_all, sp64[:, 3, :].unsqueeze(1).to_broadcast([128, NT, 64]), ALU.add, [r_rank, r_sp], [r_sp])
                ts("dve", mk, Wr_all[:], 0.0, None, ALU.is_gt, None, r_Wr, [r_sp])
                stt(dm.rearrange("p a b -> p (a b)"), dm.rearrange("p a b -> p (a b)"), 1.0, mk.rearrange("p a b -> p (a b)"), ALU.add, ALU.mult, [r_sp], [r_sp])
                ts("dve", dm, dm, -1.0, None, ALU.add, None, [r_sp], [r_sp])
                for t in range(NT):
                    S.op("dve", lambda e, t=t: e.max(d8a[:, t, :], dm[:, t, :]), reads=[r_sp], writes=[r_sp])
                cp("dve", dest8u[:], d8a, [r_sp], [r_d8])
                for t in range(NT):
                    for j in range(8):
                        S.dma_fn("pool", (lambda e, t=t, j=j: e.indirect_dma_start(
                            out=xbuf, out_offset=bass.IndirectOffsetOnAxis(ap=dest8u[:, t, j:j + 1], axis=0),
                            in_=h2tok[:, t, :], in_offset=None)), reads=[r_d8, r_h2tok[t]], writes=[Res()])
                for j in range(8):
                    tt("dve", mk, dm, d8a[:, :, j:j + 1].to_broadcast([128, NT, 64]), ALU.is_equal, [r_sp], [r_sp])
                    tt("dve", mk, mk, Wr_all[:], ALU.mult, [r_sp] + r_Wr, [r_sp])
                    red(w8[:, :, j:j + 1].rearrange("p a b -> p (a b)"), mk, ALU.add, [r_sp], [r_w8])
                conv_some(1000)
                S.barrier()
                arena.off = a1

                NWS = 4
                Wblk = [(arena.alloc((8, 256), BF16), arena.alloc((8, 256), BF16), arena.alloc((2, D), BF16)) for _ in range(NWS)]
                r_Wb = [RL(3) for _ in range(NWS)]
                xtok = [arena.alloc((2, D), BF16) for _ in range(NWS)]
                r_xtok = RL(NWS)
                xTb = [arena.alloc((8, 256), BF16) for _ in range(2)]
                r_xTb = RL(2)
                sgb = arena.alloc((512,), F32)
                r_sgb = Res()
                hidb = [arena.alloc((2, 256), BF16) for _ in range(2)]
                r_hidb = RL(2)
                ysb = [arena.alloc((2, D), BF16) for _ in range(2)]
                r_ysb = RL(2)
                def load_blk(j, slot):
                    for wi_, src_t in enumerate((wgb, wub, wdb)):
                        dst = Wblk[slot][wi_]
                        dst2 = dst.rearrange("p a b -> p (a b)")
                        S.dma_fn("pool", (lambda e, dst2=dst2, src_t=src_t, j=j: e.indirect_dma_start(
                            out=dst2, out_offset=None, in_=src_t,
                            in_offset=bass.IndirectOffsetOnAxis(ap=eb_i[:, j:j + 1], axis=0))), reads=[r_eb, r_wconv], writes=[r_Wb[slot][wi_]])
                    S.dma("sp", xtok[slot], xbuf[j * BLK:(j + 1) * BLK, :].rearrange("(s p) n -> p s n", p=128), reads=[r_xbuf], writes=[r_xtok[slot]])

                def emit_T(j):
                    slot = j % 2
                    ws = j % NWS
                    for s_ in range(2):
                        for kc in range(8):
                            bk = kc // 4
                            o_ = (kc % 4) * 256 + s_ * 128
                            tr(bankb(bk)[:, o_:o_ + 128], xtok[ws][:, s_, kc * 128:(kc + 1) * 128], identb[:], [r_xtok[ws], r_c], [pb[bk]],
                               inc=(s_ == 1 and kc % 4 == 3))
                    cp("act", xTb[slot][:, 0:4, :].rearrange("p a b -> p (a b)"), bankb(0)[:, 0:1024], [pb[0]], [r_xTb[slot]])
                    cp("dve", xTb[slot][:, 4:8, :].rearrange("p a b -> p (a b)"), bankb(1)[:, 0:1024], [pb[1]], [r_xTb[slot]])

                def emit_GU(j):
                    slot = j % 2
                    ws = j % NWS
                    Wg_, Wu_, _ = Wblk[ws]
                    for (Wx, bk, wi_) in ((Wg_, 2, 0), (Wu_, 3, 1)):
                        for ffc in range(2):
                            for kc in range(8):
                                mm(bank(bk)[:, ffc * 256:(ffc + 1) * 256], Wx[:, kc, ffc * 128:(ffc + 1) * 128], xTb[slot][:, kc, :], kc == 0, kc == 7,
                                   [r_Wb[ws][wi_], r_xTb[slot]], [pb[bk]])
                    act(sgb, bank(2)[:, :], AF.Silu, [pb[2]], [r_sgb])
                    tt("dve", hidb[slot].rearrange("p a b -> p (a b)"), bank(3)[:, :], sgb, ALU.mult, [pb[3], r_sgb], [r_hidb[slot]])

                def emit_D(j):
                    slot = j % 2
                    ws = j % NWS
                    Wdn = Wblk[ws][2]
                    for s_ in range(2):
                        for hf in range(2):
                            bk = 4 + s_ * 2 + hf
                            for ffc in range(2):
                                mm(bank(bk)[:, :], hidb[slot][:, ffc, s_ * 128:(s_ + 1) * 128], Wdn[:, ffc, hf * 512:(hf + 1) * 512], ffc == 0, ffc == 1,
                                   [r_hidb[slot], r_Wb[ws][2]], [pb[bk]])
                            cp("act" if hf == 0 else "dve", ysb[slot][:, s_, hf * 512:(hf + 1) * 512], bank(bk)[:, :], [pb[bk]], [r_ysb[slot]])
                    S.dma("sp", ybuf[j * BLK:(j + 1) * BLK, :].rearrange("(s p) n -> p s n", p=128), ysb[slot], reads=[r_ysb[slot]], writes=[Res()])

                for j0_ in range(NWS - 1):
                    load_blk(j0_, j0_)
                emit_T(0)
                for j in range(NBLK):
                    if j + NWS - 1 < NBLK:
                        load_blk(j + NWS - 1, (j + NWS - 1) % NWS)
                    emit_GU(j)
                    if j + 1 < NBLK:
                        emit_T(j + 1)
                    emit_D(j)
                S.barrier()
                arena.off = a1

                Wsh = (arena.alloc((8, 256), BF16), arena.alloc((8, 256), BF16), arena.alloc((2, D), BF16))
                r_Wsh = Res()
                ldw(Wsh[0], Wd["w_sh_gate"].rearrange("(k p) f -> p k f", p=128), r_Wsh)
                ldw(Wsh[1], Wd["w_sh_up"].rearrange("(k p) f -> p k f", p=128), r_Wsh)
                ldw(Wsh[2], Wd["w_sh_down"].rearrange("(c p) n -> p c n", p=128), r_Wsh)
                sgs = arena.alloc((512,), F32)
                r_sgs = Res()
                hsh = [arena.alloc((512,), BF16) for _ in range(4)]
                r_hsh = RL(4)
                NYG = 16
                NDG = 16
                dgs = [arena.alloc((128,), BF16) for _ in range(NDG)]
                r_dg = RL(NDG)
                dgi = [0]
                yg = [arena.alloc((D,), BF16) for _ in range(NYG)]
                r_yg = RL(NYG)
                facc = [arena.alloc((D,), F32) for _ in range(2)]
                r_facc = RL(2)
                xo = [arena.alloc((D,), F32) for _ in range(2)]
                r_xo = RL(2)
                x1r = [arena.alloc((D,), F32) for _ in range(2)]
                r_x1r = RL(2)
                junk6 = arena.alloc((D,), F32)
                r_j6 = Res()
                r_st6 = Res()
                gi = 0
                for t4 in range(4):
                    cols = slice(t4 * 512, (t4 + 1) * 512)
                    rh = r_hT[t4 * 4:t4 * 4 + 4]
                    hh = []
                    for ffc in range(2):
                        for kc in range(8):
                            mm(bank(ffc)[:, :], Wsh[0][:, kc, ffc * 128:(ffc + 1) * 128], hT[:, kc, cols], kc == 0, kc == 7, [r_Wsh] + rh, [pb[ffc]])
                        for kc in range(8):
                            mm(bank(2 + ffc)[:, :], Wsh[1][:, kc, ffc * 128:(ffc + 1) * 128], hT[:, kc, cols], kc == 0, kc == 7, [r_Wsh] + rh, [pb[2 + ffc]])
                    for ffc in range(2):
                        hx = (t4 * 2 + ffc) % 4
                        act(sgs, bank(ffc)[:, :], AF.Silu, [pb[ffc]], [r_sgs])
                        tt("dve", hsh[hx], bank(2 + ffc)[:, :], sgs, ALU.mult, [pb[2 + ffc], r_sgs], [r_hsh[hx]])
                        hh.append(hx)
                    for sub in range(4):
                        t = t4 * 4 + sub
                        bi = t % 2
                        cs = slice(t * 128, (t + 1) * 128)
                        S.dma("sp", x1r[bi], out_d[b, cs, :], reads=[r_x1[t]], writes=[r_x1r[bi]])
                        gl_ = []
                        for j in range(8):
                            g_ = gi % NYG
                            gi += 1
                            S.dma_fn("pool", (lambda e, t=t, j=j, g_=g_: e.indirect_dma_start(
                                out=yg[g_], out_offset=None, in_=ybuf,
                                in_offset=bass.IndirectOffsetOnAxis(ap=dest8u[:, t, j:j + 1], axis=0))), reads=[r_d8], writes=[r_yg[g_]])
                            dj = dgi[0] % NDG
                            dgi[0] += 1
                            ts("dve", dgs[dj], identb[:], w8[:, t, j:j + 1], None, ALU.mult, None, [r_c, r_d8, r_w8], [r_dg[dj]])
                            gl_.append((g_, dj))
                        for hf in range(2):
                            bk = 4 + 2 * bi + hf
                            for ffc in range(2):
                                mm(bank(bk)[:, :], hsh[hh[ffc]][:, sub * 128:(sub + 1) * 128], Wsh[2][:, ffc, hf * 512:(hf + 1) * 512],
                                   ffc == 0, False, [r_hsh[hh[ffc]], r_Wsh], [pb[bk]], inc=False)
                            for j, (g_, dj) in enumerate(gl_):
                                mm(bank(bk)[:, :], dgs[dj], yg[g_][:, hf * 512:(hf + 1) * 512], False, j == 7, [r_dg[dj], r_yg[g_]], [pb[bk]])
                            cp("act", facc[bi][:, hf * 512:(hf + 1) * 512], bank(bk)[:, :], [pb[bk]], [r_facc[bi]])
                        if dbg and "acc" in dbg and b == 0:
                            S.dma("sp", dbg_d["acc"][cs, :], facc[bi], reads=[r_facc[bi]])
                        act(junk6, facc[bi], AF.Square, [r_facc[bi]], [r_j6, r_st6], accum_out=st6[:, 0:1])
                        rstd_from_ssq(st6[:, 2:3], st6[:, 0:1], D, st6[:, 1:2], r_st6)
                        stt(xo[bi], facc[bi], st6[:, 2:3], G2[:, b, :], ALU.mult, ALU.mult, [r_facc[bi], r_st6, r_m], [r_xo[bi]])
                        tt("dve", xo[bi], xo[bi], x1r[bi], ALU.add, [r_xo[bi], r_x1r[bi]], [r_xo[bi]])
                        S.dma("sp", out_d[b, cs, :], xo[bi], reads=[r_xo[bi]], writes=[r_x1[t]])
                S.barrier()
                continue
            S.barrier()
            arena.off = a1

            acc = arena.alloc((NT, D), F32)
            r_acc = RL(NT)
            a5 = arena.off
            We = [(arena.alloc((8, 256), BF16), arena.alloc((8, 256), BF16), arena.alloc((2, D), BF16)) for _ in range(2)]
            r_We = RL(2)
            sgm = [arena.alloc((512,), F32) for _ in range(2)]
            r_sgm = RL(2)
            hid = [arena.alloc((512,), BF16) for _ in range(4)]
            r_hid = RL(4)
            ones1 = cst["ones"][:, 0:1]

            def load_e_w(e_, slot):
                if e_ < NEXP:
                    g_, u_, d__ = Wd["w_e_gate"][e_], Wd["w_e_up"][e_], Wd["w_e_down"][e_]
                else:
                    g_, u_, d__ = Wd["w_sh_gate"], Wd["w_sh_up"], Wd["w_sh_down"]
                ldw(We[slot][0], g_.rearrange("(k p) f -> p k f", p=128), r_We[slot])
                ldw(We[slot][1], u_.rearrange("(k p) f -> p k f", p=128), r_We[slot])
                ldw(We[slot][2], d__.rearrange("(c p) n -> p c n", p=128), r_We[slot])

            load_e_w(0, 0)
            hi_ = 0
            for e_ in range(NEXP + 1):
                slot = e_ % 2
                if e_ + 1 <= NEXP:
                    load_e_w(e_ + 1, 1 - slot)
                Wg_, Wu_, Wdn = We[slot]
                for t4 in range(4):
                    cols = slice(t4 * 512, (t4 + 1) * 512)
                    rh = r_h2T[t4 * 4:t4 * 4 + 4]
                    for ffc in range(2):
                        for kc in range(8):
                            mm(bank(ffc)[:, :], Wg_[:, kc, ffc * 128:(ffc + 1) * 128], h2T[:, kc, cols], kc == 0, kc == 7, [r_We[slot]] + rh, [pb[ffc]])
                        for kc in range(8):
                            mm(bank(2 + ffc)[:, :], Wu_[:, kc, ffc * 128:(ffc + 1) * 128], h2T[:, kc, cols], kc == 0, kc == 7, [r_We[slot]] + rh, [pb[2 + ffc]])
                    hh = []
                    for ffc in range(2):
                        act(sgm[ffc], bank(ffc)[:, :], AF.Silu, [pb[ffc]], [r_sgm[ffc]])
                        hx = hi_ % 4
                        hi_ += 1
                        tt("dve", hid[hx], bank(2 + ffc)[:, :], sgm[ffc], ALU.mult, [pb[2 + ffc], r_sgm[ffc]], [r_hid[hx]])
                        hh.append(hx)
                    for sub in range(4):
                        t = t4 * 4 + sub
                        for hf in range(2):
                            bk = 4 + (sub * 2 + hf) % 4
                            for ffc in range(2):
                                mm(bank(bk)[:, :], hid[hh[ffc]][:, sub * 128:(sub + 1) * 128], Wdn[:, ffc, hf * 512:(hf + 1) * 512],
                                   ffc == 0, ffc == 1, [r_hid[hh[ffc]], r_We[slot]], [pb[bk]])
                            wcol = Wr_all[:, t, e_:e_ + 1] if e_ < NEXP else ones1
                            a_ = acc[:, t, hf * 512:(hf + 1) * 512]
                            if e_ == 0:
                                ts("dve", a_, bank(bk)[:, :], wcol, None, ALU.mult, None, [pb[bk], r_Wr[t]], [r_acc[t]])
                            else:
                                stt(a_, bank(bk)[:, :], wcol, a_, ALU.mult, ALU.add, [pb[bk], r_Wr[t], r_acc[t]], [r_acc[t]])
            if dbg and "acc" in dbg and b == 0:
                S.dma("sp", dbg_d["acc"].rearrange("(t p) n -> p t n", p=128), acc, reads=r_acc)

            S.barrier()
            arena.off = a5
            xo = [arena.alloc((D,), F32) for _ in range(2)]
            r_xo = RL(2)
            x1r = [arena.alloc((D,), F32) for _ in range(2)]
            r_x1r = RL(2)
            junk6 = arena.alloc((D,), F32)
            r_j6 = Res()
            r_st6 = Res()
            for t in range(NT):
                bi = t % 2
                cs = slice(t * 128, (t + 1) * 128)
                S.dma("sp", x1r[bi], out_d[b, cs, :], reads=[r_x1[t]], writes=[r_x1r[bi]])
                act(junk6, acc[:, t, :], AF.Square, [r_acc[t]], [r_j6, r_st6], accum_out=st6[:, 0:1])
                rstd_from_ssq(st6[:, 2:3], st6[:, 0:1], D, st6[:, 1:2], r_st6)
                stt(xo[bi], acc[:, t, :], st6[:, 2:3], G2[:, b, :], ALU.mult, ALU.mult, [r_acc[t], r_st6, r_m], [r_xo[bi]])
                tt("dve", xo[bi], xo[bi], x1r[bi], ALU.add, [r_xo[bi], r_x1r[bi]], [r_xo[bi]])
                S.dma("sp", out_d[b, cs, :], xo[bi], reads=[r_xo[bi]], writes=[r_x1[t]])
            S.barrier()


        S.emit()
    return nc


def make_in_maps(inputs, n_cores=8):
    consts = host_consts()
    shared = {}
    for k, shp in WSHAPES.items():
        shared[k] = np.ascontiguousarray(np.asarray(inputs[k], dtype=np.float32).reshape(shp))
    for k, v in consts.items():
        shared["k_" + k] = np.ascontiguousarray(v.astype(np.float32))
    x = np.asarray(inputs["x"], dtype=np.float32)
    c = np.asarray(inputs["c"], dtype=np.float32)
    ctx = np.asarray(inputs["ctx"], dtype=np.float32)
    c_ctx = np.asarray(inputs["c_ctx"], dtype=np.float32)
    maps = []
    for i in range(n_cores):
        m = dict(shared)
        m["x"] = np.ascontiguousarray(x[i * NB:(i + 1) * NB])
        m["ctx"] = np.ascontiguousarray(ctx[i * NB:(i + 1) * NB])
        m["c3"] = np.ascontiguousarray(np.concatenate([c[i * NB:(i + 1) * NB], c_ctx[None, :]], axis=0))
        maps.append(m)
    return maps


def kernel(**inputs):
    nc = build()
    maps = make_in_maps(inputs)
    res = run_bass_kernel_spmd(nc, maps, core_ids=list(range(8)))
    return np.concatenate([r["out"] for r in res.results], axis=0).astype(np.float32)
```

```python
from contextlib import ExitStack
import numpy as np
import concourse.bass as bass
import concourse.mybir as mybir
from concourse.bass_utils import run_bass_kernel_spmd

F32 = mybir.dt.float32
BF16 = mybir.dt.bfloat16
AF = mybir.ActivationFunctionType
ALU = mybir.AluOpType
AX = mybir.AxisListType

S_LEN = 2048
L_CTX = 256
D = 1024
NB = 2
NT = S_LEN // 128
NEXP = 64
BIG = 30000.0
SPARSE = True
BLK = 256
NBLK = (S_LEN * 8) // BLK + NEXP
NSLOT = NBLK * BLK
I32 = mybir.dt.int32
U32 = mybir.dt.uint32


class Res:
    __slots__ = ("w", "r")

    def __init__(self):
        self.w = None
        self.r = []


def RL(n):
    return [Res() for _ in range(n)]


ENGS = ("pe", "act", "dve", "pool", "sp")
NDMA = 48


class Sched:
    def __init__(self, nc, stack):
        self.nc = nc
        self.esem = {e: stack.enter_context(nc.semaphore("s_" + e)) for e in ENGS if e != "sp"}
        self.dsem = [stack.enter_context(nc.semaphore("d%d" % i)) for i in range(NDMA)]
        self.dcnt = [0] * NDMA
        self.dnext = 0
        self.ops = {e: [] for e in ENGS}
        self.cnt = {e: 0 for e in ENGS}
        self.seen = {e: {} for e in ENGS}

    def _deps(self, eng, reads, writes):
        deps = {}

        def add(tok, kind):
            if tok is None:
                return
            key, val = tok
            if key == eng and (eng == "pe" or kind != "raw"):
                return
            if deps.get(key, 0) < val:
                deps[key] = val

        for r in reads:
            add(r.w, "raw")
        for w in writes:
            add(w.w, "waw")
            for t in w.r:
                add(t, "war")
        waits = []
        for key, val in deps.items():
            if self.seen[eng].get(key, 0) >= val:
                continue
            self.seen[eng][key] = val
            waits.append((key, val))
        return waits

    def _mark(self, tok, reads, writes):
        for r in reads:
            r.r.append(tok)
            if len(r.r) > 48:
                best = {}
                for k, v in r.r:
                    if best.get(k, 0) < v:
                        best[k] = v
                r.r = list(best.items())
        for w in writes:
            w.w = tok
            w.r = []

    def op(self, eng, fn, reads=(), writes=(), inc=True):
        waits = self._deps(eng, reads, writes)
        if inc:
            self.cnt[eng] += 1
            tok = (eng, self.cnt[eng])
        else:
            tok = (eng, self.cnt[eng] + 1)
        self.ops[eng].append((waits, fn, ("e", inc)))
        self._mark(tok, reads, writes)

    def dma(self, q, out, in_, reads=(), writes=(), **kw):
        return self.dma_fn(q, (lambda e: e.dma_start(out=out, in_=in_, **kw)), reads, writes)

    def dma_fn(self, q, fn, reads=(), writes=()):
        k = self.dnext
        self.dnext = (self.dnext + 1) % NDMA
        waits = self._deps(q, reads, writes)
        key = "d%d" % k
        prev = 16 * self.dcnt[k]
        if prev and self.seen[q].get(key, 0) < prev:
            self.seen[q][key] = prev
            waits.append((key, prev))
        self.dcnt[k] += 1
        tok = (key, 16 * self.dcnt[k])
        self.ops[q].append((waits, fn, ("d", k)))
        self._mark(tok, reads, writes)
        return tok

    def barrier(self):
        toks = [(e, self.cnt[e]) for e in ("pe", "act", "dve", "pool") if self.cnt[e]]
        toks += [("d%d" % k, 16 * self.dcnt[k]) for k in range(NDMA) if self.dcnt[k]]
        for e in ENGS:
            waits = []
            for key, val in toks:
                if key == e or self.seen[e].get(key, 0) >= val:
                    continue
                self.seen[e][key] = val
                waits.append((key, val))
            self.ops[e].append((waits, None, None))

    def _sem(self, key):
        if key in self.esem:
            return self.esem[key]
        return self.dsem[int(key[1:])]

    def emit(self):
        nc = self.nc
        finals = [("d%d" % k, 16 * self.dcnt[k]) for k in range(NDMA) if self.dcnt[k]]
        self.ops["sp"].append((finals, None, None))

        def runner(ename):
            def run(e):
                for waits, fn, kind in self.ops[ename]:
                    for key, val in waits:
                        e.wait_ge(self._sem(key), val)
                    if fn is None:
                        continue
                    ins = fn(e)
                    if kind[0] == "e":
                        if kind[1]:
                            ins.then_inc(self.esem[ename], 1)
                    else:
                        ins.then_inc(self.dsem[kind[1]], 16)
            return run

        with nc.Block() as block:
            block.sync(runner("sp"))
            block.tensor(runner("pe"))
            block.scalar(runner("act"))
            block.vector(runner("dve"))
            block.gpsimd(runner("pool"))


class Arena:
    def __init__(self, nc, st, nbytes):
        self.t = st.enter_context(nc.sbuf_tensor("arena", [128, nbytes // 4], F32))
        self.v = {F32: self.t, BF16: self.t.bitcast(BF16)}
        self.off = 0
        self.cap = nbytes

    def alloc(self, free, dt):
        sz = 4 if dt == F32 else 2
        n = int(np.prod(free))
        off = self.off
        self.off += (n * sz + 63) // 64 * 64
        assert self.off <= self.cap, ("arena overflow", self.off, self.cap)
        v = self.v[dt][:, off // sz: off // sz + n]
        if len(free) == 2:
            v = v.rearrange("p (a b) -> p a b", a=free[0], b=free[1])
        elif len(free) == 3:
            v = v.rearrange("p (a b c) -> p a b c", a=free[0], b=free[1], c=free[2])
        return v


def host_consts():
    c = {}
    c["ident"] = np.eye(128, dtype=np.float32)
    k = np.arange(128)
    c["trif"] = (k[:, None] <= k[None, :]).astype(np.float32)
    c["trib"] = (k[:, None] >= k[None, :]).astype(np.float32)
    c["negf"] = np.where(k[:, None] <= k[None, :], 0.0, -BIG).astype(np.float32)
    c["negb"] = np.where(k[:, None] >= k[None, :], 0.0, -BIG).astype(np.float32)
    c["ones"] = np.ones((128, 128), np.float32)
    c["tris"] = (k[:, None] < k[None, :]).astype(np.float32)
    c["pswap"] = (k[:, None] == (k[None, :] ^ 32)).astype(np.float32)
    c["pidx"] = np.tile(np.arange(128, dtype=np.float32)[:, None], (1, 128))
    c["jtab"] = np.tile((np.arange(128, dtype=np.float32) * BLK)[None, :], (128, 1))
    sel = np.zeros((3, 3, 128), np.float32)
    for b in range(3):
        sel[b, b, :] = 1.0
    c["sel3"] = sel
    t = np.arange(S_LEN)
    row = (t // 64).astype(np.float32)
    col = (t % 64).astype(np.float32)
    inv = (np.float32(10000.0) ** (-np.arange(0, 32, 2, dtype=np.float32) / np.float32(32))).astype(np.float32)
    ang = np.concatenate([row[:, None] * inv[None, :], col[:, None] * inv[None, :]], axis=-1)
    ang = np.concatenate([ang, ang], axis=-1).astype(np.float32)
    cos = np.cos(ang).astype(np.float32).T
    sin = np.sin(ang).astype(np.float32).T
    sign = np.where(np.arange(64) < 32, -1.0, 1.0).astype(np.float32)[:, None]
    c["cosT"] = np.concatenate([cos, cos], axis=0)
    c["sinT"] = np.concatenate([sin * sign, sin * sign], axis=0)
    return c


CONST_SHAPES = {"ident": [128, 128], "trif": [128, 128], "trib": [128, 128], "negf": [128, 128],
                "negb": [128, 128], "ones": [128, 128], "tris": [128, 128], "jtab": [128, 128], "pidx": [128, 128], "pswap": [128, 128], "sel3": [3, 3, 128],
                "cosT": [128, S_LEN], "sinT": [128, S_LEN]}

WSHAPES = {
    "w_ada": [D, 6 * D], "b_ada": [6 * D], "g_pre_mix": [D], "g_post_mix": [D], "g_pre_ffn": [D],
    "g_post_ffn": [D], "w_in": [D, 8224], "lam_q1": [64], "lam_k1": [64], "lam_q2": [64], "lam_k2": [64],
    "g_attn_subln": [128], "conv_w": [3, 2048], "conv_b": [2048], "dt_bias": [32], "a_log": [32],
    "d_skip": [16], "g_ssd_norm": [D], "w_branch_attn": [D, D], "w_branch_ssd": [D, D], "w_out": [D, D],
    "w_router": [D, 64], "router_bias": [64], "w_e_gate": [NEXP, D, 256], "w_e_up": [NEXP, D, 256],
    "w_e_down": [NEXP, 256, D], "w_sh_gate": [D, 256], "w_sh_up": [D, 256], "w_sh_down": [256, D],
}


def build(dbg=None, stop=None, nseq=NB):
    nc = bass.Bass("TRN2", target_bir_lowering=False)
    di = lambda name, shape: nc.dram_tensor(name, shape, F32, kind="ExternalInput").ap()
    x_d = di("x", [NB, S_LEN, D])
    c3_d = di("c3", [3, D])
    ctx_d = di("ctx", [NB, L_CTX, D])
    Wd = {k: di(k, v) for k, v in WSHAPES.items()}
    Cd = {k: di("k_" + k, v) for k, v in CONST_SHAPES.items()}
    out_d = nc.dram_tensor("out", [NB, S_LEN, D], F32, kind="ExternalOutput").ap()
    dbg_d = {}
    if dbg:
        for k, shp in dbg.items():
            dbg_d[k] = nc.dram_tensor("dbg_" + k, shp, F32, kind="ExternalOutput").ap()

    wgb = nc.dram_tensor("wgb", [NEXP * 128, 2048], BF16, kind="Internal").ap()
    wub = nc.dram_tensor("wub", [NEXP * 128, 2048], BF16, kind="Internal").ap()
    wdb = nc.dram_tensor("wdb", [NEXP * 128, 2048], BF16, kind="Internal").ap()
    xbuf = nc.dram_tensor("xbuf", [NSLOT, D], BF16, kind="Internal").ap()
    ybuf = nc.dram_tensor("ybuf", [NSLOT, D], BF16, kind="Internal").ap()
    st = ExitStack()
    with st:
        S = Sched(nc, st)
        r_wconv = Res()
        conv_list = []
        for e_ in range(NEXP):
            rows_ = slice(e_ * 128, (e_ + 1) * 128)
            conv_list.append((wgb[rows_, :].rearrange("p (k f) -> p k f", k=8), Wd["w_e_gate"][e_].rearrange("(k p) f -> p k f", p=128)))
            conv_list.append((wub[rows_, :].rearrange("p (k f) -> p k f", k=8), Wd["w_e_up"][e_].rearrange("(k p) f -> p k f", p=128)))
            conv_list.append((wdb[rows_, :].rearrange("p (c n) -> p c n", c=2), Wd["w_e_down"][e_].rearrange("(c p) n -> p c n", p=128)))

        def conv_some(n):
            for _ in range(n):
                if conv_list and SPARSE:
                    d_, s_ = conv_list.pop(0)
                    S.dma("pool", d_, s_, writes=[r_wconv])
        sbt = lambda name, shape, dt=F32: st.enter_context(nc.sbuf_tensor(name, shape, dt))
        PSALL = st.enter_context(nc.psum_tensor("psall", [128, 4096], F32))
        PSALLB = PSALL.bitcast(BF16)
        PS = [PSALL[:, i * 1024:(i + 1) * 1024] for i in range(4)]
        PSB = [PSALLB[:, i * 2048:(i + 1) * 2048] for i in range(4)]
        pb = RL(8)

        def bank(b):
            return PS[b // 2][:, (b % 2) * 512:(b % 2) * 512 + 512]

        def bankb(b):
            return PSB[b // 2][:, (b % 2) * 1024:(b % 2) * 1024 + 1024]

        def mm(out, lhsT, rhs, start, stop_, R, W, inc=None):
            if inc is None:
                inc = stop_
            S.op("pe", lambda e: e.matmul(out, lhsT, rhs, start=start, stop=stop_), reads=R, writes=W, inc=inc)

        def tr(out, in_, idn, R, W, inc=True):
            S.op("pe", lambda e: e.transpose(out, in_, idn), reads=R, writes=W, inc=inc)

        def act(out, in_, func, R, W, **kw):
            S.op("act", lambda e: e.activation(out, in_, func, **kw), reads=R, writes=W)

        def tt(eng, out, a, b, op, R, W):
            S.op(eng, lambda e: e.tensor_tensor(out, a, b, op), reads=R, writes=W)

        def ts(eng, out, a, s1, s2, op0, op1, R, W):
            if s2 is None:
                S.op(eng, lambda e: e.tensor_scalar(out, a, s1, None, op0), reads=R, writes=W)
            else:
                S.op(eng, lambda e: e.tensor_scalar(out, a, s1, s2, op0, op1), reads=R, writes=W)

        def stt(out, in0, sc, in1, op0, op1, R, W):
            S.op("dve", lambda e: e.scalar_tensor_tensor(out, in0, sc, in1, op0, op1), reads=R, writes=W)

        def cp(eng, out, in_, R, W):
            if eng == "act":
                S.op("act", lambda e: e.copy(out, in_), reads=R, writes=W)
            else:
                S.op(eng, lambda e: e.tensor_copy(out, in_), reads=R, writes=W)

        def red(out, in_, op, R, W, axis=AX.X):
            S.op("dve", lambda e: e.tensor_reduce(out, in_, axis, op), reads=R, writes=W)

        def recip(out, in_, R, W):
            S.op("dve", lambda e: e.reciprocal(out, in_), reads=R, writes=W)

        def rstd_from_ssq(dst, ssq, n, scratch, R):
            ts("dve", scratch, ssq, 1.0 / n, 1e-6, ALU.mult, ALU.add, [R], [R])
            act(scratch, scratch, AF.Ln, [R], [R])
            act(dst, scratch, AF.Exp, [R], [R], scale=-0.5)

        r_c = Res()
        cst = {}
        for k in ("ident", "trif", "trib", "negf", "negb", "ones", "tris", "jtab", "pidx", "pswap"):
            cst[k] = sbt("c_" + k, [128, 128])
            S.dma("sp", cst[k][:], Cd[k], writes=[r_c])
        ident = cst["ident"]
        identb = sbt("identb", [128, 128], BF16)
        trisb = sbt("trisb", [128, 128], BF16)
        onesbp = sbt("onesbp", [128, 128], BF16)
        cp("dve", trisb[:], cst["tris"][:], [r_c], [r_c])
        cp("dve", onesbp[:], cst["ones"][:], [r_c], [r_c])
        pswapb = sbt("pswapb", [128, 128], BF16)
        cp("dve", pswapb[:], cst["pswap"][:], [r_c], [r_c])
        mhalf = sbt("mhalf", [128, 1])
        S.op("pool", lambda e: e.memset(mhalf[:], -0.5), writes=[r_c])
        cp("dve", identb[:], ident[:], [r_c], [r_c])
        sel3 = sbt("sel3", [3, 3, 128])
        S.dma("sp", sel3[:], Cd["sel3"], writes=[r_c])

        def bcast_load(name, n, parts=128):
            t = sbt("b_" + name, [parts, n])
            S.dma("sp", t[:], Wd[name].partition_broadcast(parts), writes=[r_c])
            return t

        rbiasB = bcast_load("router_bias", 64)
        dtbB = bcast_load("dt_bias", 32)
        alogB = bcast_load("a_log", 32)
        dskB = bcast_load("d_skip", 16)
        lamv = sbt("lamv", [128, 4, 64])
        for i, nm in enumerate(("lam_q1", "lam_k1", "lam_q2", "lam_k2")):
            S.dma("sp", lamv[:, i, :], Wd[nm].partition_broadcast(128), writes=[r_c])
        gpmT = sbt("gpmT", [128, 8])
        gpfT = sbt("gpfT", [128, 8])
        cwT = sbt("cwT", [128, 16, 3])
        cbT = sbt("cbT", [128, 16])
        S.dma("sp", gpmT[:], Wd["g_pre_mix"].rearrange("(k p) -> p k", p=128), writes=[r_c], allow_slow_non_contiguous=True)
        S.dma("sp", gpfT[:], Wd["g_pre_ffn"].rearrange("(k p) -> p k", p=128), writes=[r_c], allow_slow_non_contiguous=True)
        S.dma("sp", cbT[:], Wd["conv_b"].rearrange("(k p) -> p k", p=128), writes=[r_c], allow_slow_non_contiguous=True)
        for i in range(3):
            S.dma("sp", cwT[:, :, i], Wd["conv_w"][i].rearrange("(k p) -> p k", p=128), writes=[r_c], allow_slow_non_contiguous=True)
        wrt = sbt("wrt", [128, 8, 64])
        S.dma("sp", wrt[:], Wd["w_router"].rearrange("(k p) n -> p k n", p=128), writes=[r_c])

        sm = sbt("sm", [128, 64])
        lamt = sbt("lamt", [128, 2, 64])
        tt("dve", lamt[:, 0, :], lamv[:, 0, :], lamv[:, 1, :], ALU.mult, [r_c], [r_c])
        tt("dve", lamt[:, 1, :], lamv[:, 2, :], lamv[:, 3, :], ALU.mult, [r_c], [r_c])
        red(sm[:, 0:2], lamt[:], ALU.add, [r_c], [r_c])
        act(sm[:, 2:4], sm[:, 0:2], AF.Exp, [r_c], [r_c])
        tt("dve", sm[:, 4:5], sm[:, 3:4], sm[:, 2:3], ALU.subtract, [r_c], [r_c])
        ts("dve", sm[:, 5:6], sm[:, 4:5], -0.2, None, ALU.add, None, [r_c], [r_c])
        nlam = sm[:, 5:6]
        aB = sbt("aB", [128, 32])
        act(aB[:], alogB[:], AF.Exp, [r_c], [r_c])
        ts("dve", aB[:], aB[:], -1.0, None, ALU.mult, None, [r_c], [r_c])

        st1 = sbt("st1", [128, 4])
        gsubT = sbt("gsubT", [128, 1])
        sc4 = [sbt("sc4_%d" % d_, [128, 8, 4]) for d_ in range(2)]
        Wr_all = sbt("Wr_all", [128, NT, 64])
        st4 = sbt("st4", [128, 8])
        rt = sbt("rt", [128, 6, 64])
        rt8 = sbt("rt8", [128, 6, 8])
        st6 = sbt("st6", [128, 4])
        dest8u = sbt("dest8u", [128, NT, 8], U32)
        w8 = sbt("w8", [128, NT, 8])
        eb_i = sbt("eb_i", [128, NBLK], U32)
        csum = sbt("csum", [128, 64])
        sp64 = sbt("sp64", [128, 6, 64])
        sp64i = sbt("sp64i", [128, 64], I32)
        d8f = sbt("d8f", [128, 8])
        rem = int(nc.sbuf_bytes_remaining) - 18 * 1024
        arena = Arena(nc, st, rem // 256 * 256)
        a0 = arena.off
        gpostmixB = arena.alloc((D,), F32)
        gpostffnB = arena.alloc((D,), F32)
        S.dma("sp", gpostmixB, Wd["g_post_mix"].partition_broadcast(128), writes=[r_c])
        S.dma("sp", gpostffnB, Wd["g_post_ffn"].partition_broadcast(128), writes=[r_c])
        c3s = arena.alloc((D,), F32)
        mod_rm = arena.alloc((6 * D,), F32)
        bada3 = arena.alloc((6 * D,), F32)
        wab = [arena.alloc((8, 512), F32) for _ in range(2)]
        r_wab = RL(2)
        r_m = Res()
        S.dma("sp", c3s[0:3, :], c3_d, writes=[r_m])
        S.dma("sp", bada3[0:3, :], Wd["b_ada"].partition_broadcast(3), writes=[r_m])
        act(c3s[0:3, :], c3s[0:3, :], AF.Silu, [r_m], [r_m])
        cT = sbt("cT", [128, 8, 3])
        for kc in range(8):
            tr(bank(0)[:, kc * 3:kc * 3 + 3], c3s[0:3, kc * 128:(kc + 1) * 128], ident[0:3, 0:3], [r_m, r_c], [pb[0]])
        cp("dve", cT[:].rearrange("p a b -> p (a b)"), bank(0)[:, 0:24], [pb[0]], [r_m])
        wa_v = Wd["w_ada"].rearrange("(k p) n -> p k n", p=128)
        for nb_ in range(12):
            bi = nb_ % 2
            S.dma("sp", wab[bi], wa_v[:, :, nb_ * 512:(nb_ + 1) * 512], writes=[r_wab[bi]])
            pbk = 2 + bi
            for kc in range(8):
                mm(bank(pbk)[0:3, :], cT[:, kc, :], wab[bi][:, kc, :], kc == 0, kc == 7, [r_m, r_wab[bi]], [pb[pbk]])
            tt("dve", mod_rm[0:3, nb_ * 512:(nb_ + 1) * 512], bank(pbk)[0:3, :], bada3[0:3, nb_ * 512:(nb_ + 1) * 512],
               ALU.add, [pb[pbk], r_m], [r_m])
        modT = sbt("modT", [128, 48, 3])
        for j in range(48):
            tr(bank(0)[:, j * 3:j * 3 + 3], mod_rm[0:3, j * 128:(j + 1) * 128], ident[0:3, 0:3], [r_m, r_c], [pb[0]])
        cp("dve", modT[:].rearrange("p a b -> p (a b)"), bank(0)[:, 0:144], [pb[0]], [r_m])
        AB = sbt("AB", [128, 3, 4, 8])
        for b in range(3):
            ts("dve", AB[:, b, 0, :], modT[:, 8:16, b], 1.0, None, ALU.add, None, [r_m], [r_m])
            tt("dve", AB[:, b, 0, :], AB[:, b, 0, :], gpmT[:], ALU.mult, [r_m, r_c], [r_m])
            cp("dve", AB[:, b, 1, :], modT[:, 0:8, b], [r_m], [r_m])
            ts("dve", AB[:, b, 2, :], modT[:, 32:40, b], 1.0, None, ALU.add, None, [r_m], [r_m])
            tt("dve", AB[:, b, 2, :], AB[:, b, 2, :], gpfT[:], ALU.mult, [r_m, r_c], [r_m])
            cp("dve", AB[:, b, 3, :], modT[:, 24:32, b], [r_m], [r_m])
        G1 = sbt("G1", [128, NB, D])
        G2 = sbt("G2", [128, NB, D])
        for b in range(NB):
            for (G, coff, gB) in ((G1, 2 * D, gpostmixB), (G2, 5 * D, gpostffnB)):
                for hf in range(2):
                    mm(bank(4 + hf)[:, :], sel3[0:3, b, :], mod_rm[0:3, coff + hf * 512:coff + hf * 512 + 512], True, True,
                       [r_m, r_c], [pb[4 + hf]])
                    tt("dve", G[:, b, hf * 512:(hf + 1) * 512], bank(4 + hf)[:, :], gB[:, hf * 512:(hf + 1) * 512], ALU.mult,
                       [pb[4 + hf], r_c], [r_m])
        S.barrier()
        arena.off = a0

        win_v = Wd["w_in"].rearrange("(k p) n -> p k n", p=128)

        def ldw(dst, src, r):
            S.dma("pool", dst, src, writes=[r])

        for b in range(nseq):
            arena.off = a0
            hT = arena.alloc((8, S_LEN), BF16)
            r_hT = RL(NT)
            hcT = arena.alloc((8, L_CTX), BF16)
            r_hcT = RL(2)
            yT = arena.alloc((8, S_LEN), BF16)
            r_yT = RL(NT)
            a1 = arena.off

            xt = [arena.alloc((D,), F32) for _ in range(2)]
            r_xt = RL(2)
            xn = arena.alloc((D,), F32)
            r_xn = Res()
            junk = arena.alloc((D,), F32)
            r_junk = Res()
            r_st1 = Res()

            def norm_to_T(src_ap, bi, dstT, tcol, r_dst, ab_idx, abrow):
                act(junk, src_ap, AF.Square, [r_xt[bi]], [r_junk, r_st1], accum_out=st1[:, 0:1])
                rstd_from_ssq(st1[:, 2:3], st1[:, 0:1], D, st1[:, 1:2], r_st1)
                ts("dve", xn, src_ap, st1[:, 2:3], None, ALU.mult, None, [r_xt[bi], r_st1], [r_xn])
                for kc in range(8):
                    bk = 0 + kc // 4
                    tr(bank(bk)[:, (kc % 4) * 128:(kc % 4) * 128 + 128], xn[:, kc * 128:(kc + 1) * 128], ident[:],
                       [r_xn, r_c], [pb[bk]])
                for kc in range(8):
                    bk = 0 + kc // 4
                    act(dstT[:, kc, tcol:tcol + 128], bank(bk)[:, (kc % 4) * 128:(kc % 4) * 128 + 128], AF.Identity,
                        [pb[bk], r_m], [r_dst], bias=AB[:, abrow, ab_idx + 1, kc:kc + 1], scale=AB[:, abrow, ab_idx, kc:kc + 1])

            tiles = [("c", i) for i in range(2)] + [("x", i) for i in range(NT)]
            for n, (kind, i) in enumerate(tiles):
                bi = n % 2
                src = ctx_d[b, i * 128:(i + 1) * 128, :] if kind == "c" else x_d[b, i * 128:(i + 1) * 128, :]
                S.dma("sp", xt[bi], src, writes=[r_xt[bi]])
                if kind == "c":
                    norm_to_T(xt[bi], bi, hcT, i * 128, r_hcT[i], 0, 2)
                else:
                    norm_to_T(xt[bi], bi, hT, i * 128, r_hT[i], 0, b)
            if dbg and "hT" in dbg and b == 0:
                dtmp = arena.alloc((2, S_LEN), F32)
                r_d = Res()
                for kk in range(4):
                    cp("dve", dtmp, hT[:, kk * 2:kk * 2 + 2, :], r_hT, [r_d])
                    S.dma("sp", dbg_d["hT"][kk * 256:(kk + 1) * 256, :].rearrange("(k p) t -> p k t", p=128), dtmp, reads=[r_d])
            if stop == "p1":
                break
            conv_some(8)
            S.barrier()
            arena.off = a1

            gssdB = arena.alloc((D,), F32)
            r_g3 = Res()
            S.dma("sp", gssdB, Wd["g_ssd_norm"].partition_broadcast(128), writes=[r_g3])
            Wz = arena.alloc((8, 256), BF16)
            r_Wz = Res()
            Wdt = arena.alloc((8, 32), BF16)
            ldw(Wdt, win_v[:, :, 6144:6176], r_g3)
            dt_all = arena.alloc((18, 32), F32)
            dta_all = arena.alloc((18, 32), F32)
            r_dt = Res()
            for tl in range(18):
                bk = 6 + tl % 2
                for kc in range(8):
                    lhs = hcT[:, kc, tl * 128:(tl + 1) * 128] if tl < 2 else hT[:, kc, (tl - 2) * 128:(tl - 1) * 128]
                    rr = [r_hcT[tl]] if tl < 2 else [r_hT[tl - 2]]
                    mm(bank(bk)[:, 0:32], lhs, Wdt[:, kc, :], kc == 0, kc == 7, [r_g3] + rr, [pb[bk]])
                tt("dve", dt_all[:, tl, :], bank(bk)[:, 0:32], dtbB[:, :], ALU.add, [pb[bk], r_c], [r_dt])
            act(dt_all, dt_all, AF.Exp, [r_dt], [r_dt])
            act(dt_all, dt_all, AF.Ln, [r_dt], [r_dt], bias=1.0)
            tt("dve", dta_all, dt_all, aB[:, :].unsqueeze(1).to_broadcast([128, 18, 32]), ALU.mult, [r_dt, r_c], [r_dt])
            NTOK = L_CTX + S_LEN
            Wg4 = [arena.alloc((4, 8, 128), BF16)] * 2
            r_Wg4 = [Res()] * 2
            upad = arena.alloc((NTOK + 4,), F32)
            r_up = Res()
            cacc = PSALL[:, 0:NTOK + 4]
            r_ca = Res()
            xTf = arena.alloc((2, NTOK), BF16)
            BTf = arena.alloc((NTOK,), BF16)
            CTf = arena.alloc((NTOK,), BF16)
            r_xx = Res()
            r_bc = Res()
            x_tok = arena.alloc((18, 256), BF16)
            B_tok = arena.alloc((18, 128), BF16)
            r_tok = Res()
            ydir = [xTf.rearrange("p a b -> p (a b)")[:, 0:16 * 256].rearrange("p (a b) -> p a b", a=16, b=256), arena.alloc((16, 256), BF16)]
            r_yd = [RL(16), RL(16)]
            r_sc4 = RL(2)
            rb_off = arena.off
            Rb = [arena.alloc((4, 128), F32) for _ in range(2)] * 2
            R2b = [arena.alloc((4, 128), F32) for _ in range(2)] * 2
            Eb = [arena.alloc((4, 128), BF16) for _ in range(4)]
            MTb = [arena.alloc((4, 128), BF16) for _ in range(4)]
            xdt = [arena.alloc((4, 64), BF16) for _ in range(4)]
            xdt2 = [arena.alloc((4, 64), BF16) for _ in range(4)]
            r_R, r_R2, r_Eb, r_MT, r_x1_, r_x2 = RL(2) * 2, RL(2) * 2, RL(4), RL(4), RL(4), RL(4)
            ytmp = [arena.alloc((4, 64), F32) for _ in range(2)]
            r_yt = RL(2)
            Sst = [arena.alloc((256,), F32) for _ in range(2)]
            Sbf = [arena.alloc((256,), BF16) for _ in range(2)]
            r_S = RL(2)
            r_Sb = RL(2)
            nac_all = arena.alloc((18, 2, 4), F32)
            ea_all = arena.alloc((18, 2, 4), F32)
            dec_all = arena.alloc((18, 2, 4), F32)
            wst_all = arena.alloc((18, 2, 4), F32)
            pbh = [RL(2) for _ in range(8)]
            _fl = lambda a: a.rearrange("p a b -> p (a b)")
            _sz = 4 * (NTOK + 4)
            assert 4 * 2048 + 4 * 1024 >= _sz
            _o0 = arena.off
            arena.off = rb_off
            upad2 = arena.alloc((NTOK + 4,), F32)
            arena.off = _o0
            upads = [upad, upad2]
            r_ups = [r_up, Res()]
            S.op("pool", lambda e: e.memset(upad2, 0.0), writes=[r_ups[1]])
            fz_zs = [_fl(Rb[0])[:, 0:256], _fl(Rb[1])[:, 0:256], _fl(R2b[0])[:, 0:256]]
            fz_y = [_fl(Rb[0])[:, 256:512], _fl(Rb[1])[:, 256:512], _fl(R2b[0])[:, 256:512]]
            fz_yb = [_fl(Eb[0])[:, 0:256], _fl(Eb[1])[:, 0:256], _fl(Eb[2])[:, 0:256]]
            fz_jk = _fl(Eb[3])[:, 0:256]
            r_fzs = RL(3)
            r_fjk = Res()
            S.op("pool", lambda e: e.memset(upad, 0.0), writes=[r_up])

            def load_g_w(g, slot):
                xo = 4096 + g * 256
                for i, c0 in enumerate((xo, xo + 128, 4096 + 1024 + g * 128, 4096 + 1536 + g * 128)):
                    ldw(Wg4[slot][:, i, :, :], win_v[:, :, c0:c0 + 128], r_Wg4[slot])

            load_g_w(0, 0)
            for g in range(4):
                slot = 0
                Wg = Wg4[slot]
                ccs = (2 * g, 2 * g + 1, 8 + g, 12 + g)
                if g > 0:
                    S.op("pool", lambda e: e.memset(upad2[:, 0:1], 0.0), writes=[r_ups[1]])
                    S.op("pool", lambda e: e.memset(upad2[:, 257:259], 0.0), writes=[r_ups[1]])
                    S.op("pool", lambda e: e.memset(upad2[:, NTOK + 3:NTOK + 4], 0.0), writes=[r_ups[1]])
                for i in range(4):
                    cc = ccs[i]
                    up_ = upads[i % 2]
                    rup_ = r_ups[i % 2]
                    for kc in range(8):
                        mm(bank(6)[:, 0:L_CTX], Wg[:, i, kc, :], hcT[:, kc, :], kc == 0, kc == 7, [r_Wg4[slot]] + r_hcT, [pb[6]])
                    cp("act", up_[:, 1:1 + L_CTX], bank(6)[:, 0:L_CTX], [pb[6]], [rup_])
                    for t4 in range(4):
                        bk = 6 + (t4 + 1) % 2
                        for kc in range(8):
                            mm(bank(bk)[:, :], Wg[:, i, kc, :], hT[:, kc, t4 * 512:(t4 + 1) * 512], kc == 0, kc == 7,
                               [r_Wg4[slot]] + r_hT[t4 * 4:t4 * 4 + 4], [pb[bk]])
                        cp("act", up_[:, 259 + t4 * 512:259 + (t4 + 1) * 512], bank(bk)[:, :], [pb[bk]], [rup_])
                    n_ = NTOK + 2
                    ts("dve", cacc[:, 1:1 + n_], up_[:, 1:1 + n_], cwT[:, cc, 1:2], cbT[:, cc:cc + 1], ALU.mult, ALU.add, [rup_, r_c], [r_ca] + pb[0:5])
                    stt(cacc[:, 1:1 + n_], up_[:, 0:n_], cwT[:, cc, 0:1], cacc[:, 1:1 + n_], ALU.mult, ALU.add, [rup_, r_c, r_ca], [r_ca] + pb[0:5])
                    stt(cacc[:, 1:1 + n_], up_[:, 2:2 + n_], cwT[:, cc, 2:3], cacc[:, 1:1 + n_], ALU.mult, ALU.add, [rup_, r_c, r_ca], [r_ca] + pb[0:5])
                    dst = xTf[:, i, :] if i < 2 else (BTf if i == 2 else CTf)
                    rdst = r_xx if i < 2 else r_bc
                    act(dst[:, 0:L_CTX], cacc[:, 1:1 + L_CTX], AF.Silu, [r_ca] + pb[0:5], [rdst])
                    act(dst[:, L_CTX:NTOK], cacc[:, 259:259 + S_LEN], AF.Silu, [r_ca] + pb[0:5], [rdst])
                if g + 1 < 4:
                    load_g_w(g + 1, 0)
                for tl in range(18):
                    bk = (0, 1, 2, 3, 6, 7)[tl % 6]
                    cs = slice(tl * 128, (tl + 1) * 128)
                    tr(bankb(bk)[:, 0:128], xTf[:, 0, cs], identb[:], [r_xx, r_c], [pb[bk]], inc=False)
                    tr(bankb(bk)[:, 128:256], xTf[:, 1, cs], identb[:], [r_xx, r_c], [pb[bk]], inc=False)
                    tr(bankb(bk)[:, 256:384], BTf[:, cs], identb[:], [r_bc, r_c], [pb[bk]], inc=True)
                    cp("act", x_tok[:, tl, :], bankb(bk)[:, 0:256], [pb[bk]], [r_tok])
                    cp("dve", B_tok[:, tl, :], bankb(bk)[:, 256:384], [pb[bk]], [r_tok])

                ldw(Wz, win_v[:, :, 3 * D + g * 256:3 * D + (g + 1) * 256], r_Wz)
                for d_ in range(2):
                    S.op("pool", lambda e, d_=d_: e.memset(Sst[d_], 0.0), writes=[r_S[d_]])
                    S.op("pool", lambda e, d_=d_: e.memset(Sbf[d_], 0.0), writes=[r_S[d_]])
                dtg = dt_all.rearrange("p t (d h) -> p t d h", d=2, h=16)[:, :, :, g * 4:(g + 1) * 4]
                dtag = dta_all.rearrange("p t (d h) -> p t d h", d=2, h=16)[:, :, :, g * 4:(g + 1) * 4]
                pa = bank(0)[:, 0:144].rearrange("p (t d h) -> p t d h", t=18, d=2, h=4)
                pt_ = bank(1)[:, 0:144].rearrange("p (t d h) -> p t d h", t=18, d=2, h=4)
                for d_ in range(2):
                    tri = cst["trif"] if d_ == 0 else cst["trib"]
                    mm(pa[:, :, d_, :], tri[:], dtag[:, :, d_, :], True, True, [r_c, r_dt], [pb[0]], inc=False)
                    mm(pt_[:, :, d_, :], cst["ones"][:], dtag[:, :, d_, :], True, True, [r_c, r_dt], [pb[1]], inc=(d_ == 1))
                r_sm = Res()
                ts("dve", nac_all, pa, -1.0, None, ALU.mult, None, [pb[0]], [r_sm])
                act(ea_all, pa, AF.Exp, [pb[0]], [r_sm])
                act(dec_all, pt_, AF.Exp, [pb[1]], [r_sm])
                tt("dve", wst_all, pt_, nac_all, ALU.add, [pb[1], r_sm], [r_sm])
                act(wst_all, wst_all, AF.Exp, [r_sm], [r_sm])
                tt("dve", wst_all, wst_all, dtg, ALU.mult, [r_sm, r_dt], [r_sm])
                order = [[0, 1] + list(range(2, 18)), [1, 0] + list(range(17, 1, -1))]
                S.barrier()

                def pre(step, d_):
                    tl = order[d_][step]
                    lat = tl >= 2
                    par = step % 2
                    k_ = d_ * 2 + par
                    cs = slice(tl * 128, (tl + 1) * 128)
                    tri = cst["trif"] if d_ == 0 else cst["trib"]
                    neg = cst["negf"] if d_ == 0 else cst["negb"]
                    bA = 2 * k_
                    bB = 2 * k_ + 1
                    xv = x_tok[:, tl, :].rearrange("p (a b) -> p a b", a=4, b=64)
                    tt("dve", xdt2[k_], xv, wst_all[:, tl, d_, :].unsqueeze(2).to_broadcast([128, 4, 64]), ALU.mult, [r_tok, r_sm], [r_x2[k_]])
                    yield
                    mm(bank(bB)[:, 256:512], B_tok[:, tl, :], xdt2[k_].rearrange("p a b -> p (a b)"), True, True, [r_tok, r_x2[k_]], [pbh[bB][1]])
                    yield
                    if lat:
                        tt("pool", Rb[d_], tri[:, :].unsqueeze(1).to_broadcast([128, 4, 128]),
                           dtag[:, tl, d_, :].unsqueeze(2).to_broadcast([128, 4, 128]), ALU.mult, [r_c, r_dt], [r_R[d_]])
                        yield
                        tt("pool", R2b[d_], neg[:, :].unsqueeze(1).to_broadcast([128, 4, 128]),
                           nac_all[:, tl, d_, :].unsqueeze(2).to_broadcast([128, 4, 128]), ALU.add, [r_c, r_sm], [r_R2[d_]])
                        yield
                        mm(bank(bA)[:, :], cst["ones"][:], Rb[d_].rearrange("p a b -> p (a b)"), True, False, [r_c, r_R[d_]], pbh[bA], inc=False)
                        mm(bank(bA)[:, :], ident[:], R2b[d_].rearrange("p a b -> p (a b)"), False, True, [r_c, r_R2[d_]], pbh[bA])
                        yield
                        mm(bank(bB)[:, 0:128], BTf[:, cs], CTf[:, cs], True, True, [r_bc], [pbh[bB][0]])
                        yield
                        act(Eb[k_].rearrange("p a b -> p (a b)"), bank(bA)[:, :], AF.Exp, pbh[bA], [r_Eb[k_]])
                        yield
                        tt("dve", MTb[k_], Eb[k_], bank(bB)[:, 0:128].unsqueeze(1).to_broadcast([128, 4, 128]), ALU.mult, [r_Eb[k_], pbh[bB][0]], [r_MT[k_]])
                        yield
                        tt("dve", xdt[k_], xv, dtg[:, tl, d_, :].unsqueeze(2).to_broadcast([128, 4, 64]), ALU.mult, [r_tok, r_dt], [r_x1_[k_]])
                        yield
                        for h in range(4):
                            mm(bank(bA)[:, h * 64:(h + 1) * 64], MTb[k_][:, h, :], xdt[k_][:, h, :], True, True, [r_MT[k_], r_x1_[k_]], [pbh[bA][0]], inc=(h == 3))
                        yield

                def dep(step, d_):
                    tl = order[d_][step]
                    lat = tl >= 2
                    par = step % 2
                    k_ = d_ * 2 + par
                    cs = slice(tl * 128, (tl + 1) * 128)
                    bA = 2 * k_
                    bB = 2 * k_ + 1
                    if lat:
                        t = tl - 2
                        mm(bank(bA)[:, 256:512], CTf[:, cs], Sbf[d_], True, True, [r_bc, r_Sb[d_]], [pbh[bA][1]])
                        yield
                    Sv = Sst[d_].rearrange("p (a b) -> p a b", a=4, b=64)
                    tt("dve", Sv, Sv, dec_all[:, tl, d_, :].unsqueeze(2).to_broadcast([128, 4, 64]), ALU.mult, [r_S[d_], r_sm], [r_S[d_]])
                    yield
                    tt("dve", Sst[d_], Sst[d_], bank(bB)[:, 256:512], ALU.add, [r_S[d_], pbh[bB][1]], [r_S[d_]])
                    yield
                    cp("act", Sbf[d_], Sst[d_], [r_S[d_]], [r_Sb[d_]])
                    yield
                    if lat:
                        tt("dve", ytmp[d_], bank(bA)[:, 256:512].rearrange("p (a b) -> p a b", a=4, b=64),
                           ea_all[:, tl, d_, :].unsqueeze(2).to_broadcast([128, 4, 64]), ALU.mult, [pbh[bA][1], r_sm], [r_yt[d_]])
                        yield
                        tt("dve", ydir[d_][:, t, :], ytmp[d_].rearrange("p a b -> p (a b)"), bank(bA)[:, 0:256], ALU.add,
                           [r_yt[d_], pbh[bA][0]], [r_yd[d_][t]] + ([r_xx] if d_ == 0 else []))
                        yield

                def rr(gens):
                    while gens:
                        nxt = []
                        for g_ in gens:
                            try:
                                next(g_)
                                nxt.append(g_)
                            except StopIteration:
                                pass
                        gens = nxt

                rr([pre(0, 0), pre(0, 1)])
                for step in range(18):
                    gl = [dep(step, 0), dep(step, 1)]
                    if step + 1 < 18:
                        gl = [pre(step + 1, 0), pre(step + 1, 1)] + gl
                    rr(gl)
                    conv_some(1)
                S.barrier()
                pass
                def fin(t):
                    i = t % 3
                    cs = slice(t * 128, (t + 1) * 128)
                    zs_, yf_, ybf_, jk_ = fz_zs[i], fz_y[i], fz_yb[i], fz_jk
                    rz = r_fzs[i]
                    scl = sc4[0][:, 4 + i, :]
                    for kc in range(8):
                        mm(bank(i)[:, 0:256], hT[:, kc, cs], Wz[:, kc, :], kc == 0, kc == 7, [r_Wz, r_hT[t]], [pb[i]])
                    yield
                    act(zs_, bank(i)[:, 0:256], AF.Silu, [pb[i]], [rz])
                    yield
                    xv = x_tok[:, t + 2, :].rearrange("p (a b) -> p a b", a=4, b=64)
                    tt("pool", yf_.rearrange("p (a b) -> p a b", a=4, b=64), xv, dskB[:, g * 4:(g + 1) * 4].unsqueeze(2).to_broadcast([128, 4, 64]),
                       ALU.mult, [r_tok, r_c], [rz])
                    yield
                    tt("dve", yf_, yf_, ydir[0][:, t, :], ALU.add, [rz, r_yd[0][t], r_xx], [rz])
                    yield
                    tt("dve", yf_, yf_, ydir[1][:, t, :], ALU.add, [rz, r_yd[1][t]], [rz])
                    yield
                    tt("dve", yf_, yf_, zs_, ALU.mult, [rz], [rz])
                    yield
                    act(jk_, yf_, AF.Square, [rz], [r_fjk, rz], accum_out=scl[:, 0:1])
                    yield
                    ts("dve", scl[:, 1:2], scl[:, 0:1], 1.0 / 256, 1e-6, ALU.mult, ALU.add, [rz], [rz])
                    yield
                    act(scl[:, 1:2], scl[:, 1:2], AF.Ln, [rz], [rz])
                    yield
                    act(scl[:, 2:3], scl[:, 1:2], AF.Exp, [rz], [rz], scale=-0.5)
                    yield
                    stt(ybf_, yf_, scl[:, 2:3], gssdB[:, g * 256:(g + 1) * 256], ALU.mult, ALU.mult, [rz, r_g3], [rz])
                    yield
                    tr(bankb(3 + i)[:, 0:128], ybf_[:, 0:128], identb[:], [rz, r_c], [pb[3 + i]], inc=False)
                    tr(bankb(3 + i)[:, 128:256], ybf_[:, 128:256], identb[:], [rz, r_c], [pb[3 + i]], inc=True)
                    yield
                    cp("act", yT[:, 2 * g:2 * g + 2, cs], bankb(3 + i)[:, 0:256].rearrange("p (a b) -> p a b", a=2, b=128), [pb[3 + i]], [r_yT[t]])
                    yield

                active = []
                nxt_ = 0
                while nxt_ < NT or active:
                    if len(active) < 3 and nxt_ < NT:
                        active.append(fin(nxt_))
                        nxt_ += 1
                    new_ = []
                    for g_ in active:
                        try:
                            next(g_)
                            new_.append(g_)
                        except StopIteration:
                            pass
                    active = new_
                S.barrier()
            if dbg and "yT" in dbg and b == 0:
                S.barrier()
                arena.off = a1
                dtmp = arena.alloc((2, S_LEN), F32)
                r_d = Res()
                for kk in range(4):
                    cp("dve", dtmp, yT[:, kk * 2:kk * 2 + 2, :], r_yT, [r_d])
                    S.dma("sp", dbg_d["yT"][kk * 256:(kk + 1) * 256, :].rearrange("(k p) t -> p k t", p=128), dtmp, reads=[r_d])
            if stop == "p3":
                break
            S.barrier()
            arena.off = a1
            oaT = arena.alloc((8, S_LEN), BF16)
            r_oaT = RL(4)
            a2 = arena.off
            cosT = arena.alloc((S_LEN,), F32)
            sinT = arena.alloc((S_LEN,), F32)
            r_cs = Res()
            S.dma("sp", cosT, Cd["cosT"], writes=[r_cs])
            S.dma("sp", sinT, Cd["sinT"], writes=[r_cs])
            wq = [arena.alloc((3, 8, 128), BF16) for _ in range(2)]
            qpre = arena.alloc((512,), BF16)
            r_qpre = Res()
            tmpk1 = arena.alloc((512,), F32)
            tmpk2 = arena.alloc((512,), F32)
            qprek = arena.alloc((512,), BF16)
            r_tk = RL(3)
            r_wq = RL(2)
            qT = arena.alloc((2, S_LEN), BF16)
            r_qT = RL(4)
            S.op("pool", lambda e: e.memset(qT, 0.0), writes=r_qT)
            kT = arena.alloc((L_CTX + S_LEN,), BF16)
            r_kT = RL(5)
            va = arena.alloc((18, 128), BF16)
            r_va = Res()
            onesb = arena.alloc((128,), BF16)
            cp("dve", onesb, cst["ones"][:], [r_c], [r_va])
            Et = [arena.alloc((512,), BF16) for _ in range(3)]
            r_E = RL(3)
            fo = arena.alloc((512,), F32)
            ft = arena.alloc((512,), F32)
            fr = arena.alloc((512,), F32)
            ftb = arena.alloc((512,), BF16)
            r_f = Res()
            tmp1, tmp2 = fr, ft
            r_tmp = [r_f, r_f]
            S.dma("sp", gsubT[:], Wd["g_attn_subln"].rearrange("(p o) -> p o", o=1), writes=[r_f])
            ts("dve", gsubT[:], gsubT[:], 0.8, None, ALU.mult, None, [r_f], [r_f])

            wstg = [arena.alloc((8, 128), F32)] * 2
            r_wstg = [Res()] * 2
            stg_i = [0]

            def load_head_w(hd, slot):
                c0 = hd * 128
                for (wi, base) in ((0, c0), (1, D + c0), (2, 2 * D + c0)):
                    si = stg_i[0] % 2
                    stg_i[0] += 1
                    S.dma("sp", wstg[si], win_v[:, :, base:base + 128], writes=[r_wstg[si]])
                    cp("pool", wq[slot][:, wi, :, :], wstg[si], [r_wstg[si]], [r_wq[slot]])

            load_head_w(0, 0)
            for hd in range(8):
                slot = hd % 2
                if hd + 1 < 8:
                    load_head_w(hd + 1, 1 - slot)
                W = wq[slot]
                rW = r_wq[slot]
                def gen_qk(is_q):
                    wi = 0 if is_q else 1
                    bA, bB = (6, 7) if is_q else (0, 1)
                    t1_, t2_, qp_ = (tmp1, tmp2, qpre) if is_q else (tmpk1, tmpk2, qprek)
                    rt1, rt2, rqp = (r_tmp[0], r_tmp[1], r_qpre) if is_q else (r_tk[0], r_tk[1], r_tk[2])
                    if not is_q:
                        for kc in range(8):
                            mm(bank(bA)[:, 0:L_CTX], W[:, 1, kc, :], hcT[:, kc, :], kc == 0, kc == 7, [rW] + r_hcT, [pb[bA]])
                        yield
                        cp("act", kT[:, 0:L_CTX], bank(bA)[:, 0:L_CTX], [pb[bA]], [r_kT[0]])
                        yield
                    for t4 in range(4):
                        cols = slice(t4 * 512, (t4 + 1) * 512)
                        for kc in range(8):
                            mm(bank(bA)[:, :], W[:, wi, kc, :], hT[:, kc, cols], kc == 0, kc == 7, [rW] + r_hT[t4 * 4:t4 * 4 + 4], [pb[bA]])
                        yield
                        cp("dve", qp_, bank(bA)[:, :], [pb[bA]], [rqp])
                        yield
                        mm(bank(bB)[:, :], pswapb[:], qp_, True, True, [r_c, rqp], [pb[bB]])
                        yield
                        tt("dve", t1_, bank(bA)[:, :], cosT[:, cols], ALU.mult, [pb[bA], r_cs], [rt1])
                        yield
                        tt("dve", t2_, bank(bB)[:, :], sinT[:, cols], ALU.mult, [pb[bB], r_cs], [rt2])
                        yield
                        if is_q:
                            tt("pool", qT[0:64, 0, cols], t1_[0:64, :], t2_[0:64, :], ALU.add, [rt1, rt2], [r_qT[t4]])
                            tt("dve", qT[64:128, 1, cols], t1_[64:128, :], t2_[64:128, :], ALU.add, [rt1, rt2], [r_qT[t4]])
                        else:
                            tt("pool", kT[:, L_CTX + t4 * 512:L_CTX + (t4 + 1) * 512], t1_, t2_, ALU.add, [rt1, rt2], [r_kT[1 + t4]])
                        yield

                def gen_v():
                    for vt in range(18):
                        bk = 2 + (vt % 4)
                        for kc in range(8):
                            lhs = hcT[:, kc, vt * 128:(vt + 1) * 128] if vt < 2 else hT[:, kc, (vt - 2) * 128:(vt - 1) * 128]
                            rr_ = [r_hcT[vt]] if vt < 2 else [r_hT[vt - 2]]
                            mm(bank(bk)[:, 0:128], lhs, W[:, 2, kc, :], kc == 0, kc == 7, [rW] + rr_, [pb[bk]])
                        yield
                        cp("act", va[:, vt, :], bank(bk)[:, 0:128], [pb[bk]], [r_va])
                        yield

                gens_ = [gen_qk(True), gen_qk(False), gen_v()]
                while gens_:
                    nx_ = []
                    for g_ in gens_:
                        try:
                            next(g_)
                            nx_.append(g_)
                        except StopIteration:
                            pass
                    gens_ = nx_
                SB_ = (0, 1, 7)
                LA = 2
                its = [(qt, c_, kc) for qt in range(4) for c_ in range(2) for kc in range(18)]

                def emit_S(i):
                    qt, c_, kc = its[i]
                    prt = slice(c_ * 64, c_ * 64 + 64)
                    sb_ = SB_[i % 3]
                    kr = [r_kT[0]] if kc < 2 else [r_kT[1 + (kc - 2) // 4]]
                    mm(bank(sb_)[:, :], kT[:, kc * 128:(kc + 1) * 128], qT[:, c_, qt * 512:(qt + 1) * 512], True, True,
                       kr + [r_qT[qt]], [pb[sb_]])
                    act(Et[i % 3], bank(sb_)[:, :], AF.Exp, [pb[sb_]], [r_E[i % 3]], scale=0.125)

                pending = []
                for i in range(LA):
                    emit_S(i)
                for i in range(len(its)):
                    if i + LA < len(its):
                        emit_S(i + LA)
                    qt, c_, kc = its[i]
                    qcols = slice(qt * 512, (qt + 1) * 512)
                    bo = 2 + 2 * c_
                    E = Et[i % 3]
                    rE = r_E[i % 3]
                    mm(bank(bo)[:, :], va[:, kc, :], E, kc == 0, kc == 17, [rE, r_va], [pb[bo]])
                    mm(bank(bo + 1)[:, :], onesb, E, kc == 0, kc == 17, [rE, r_va], [pb[bo + 1]])
                    if kc == 8:
                        conv_some(1)
                    if kc == 17 and c_ == 0:
                        recip(fr, bank(3)[:, :], [pb[3]], [r_f])
                        tt("dve", fo, bank(2)[:, :], fr, ALU.mult, [pb[2], r_f], [r_f])
                    if kc == 17 and c_ == 1:
                        recip(fr, bank(5)[:, :], [pb[5]], [r_f])
                        tt("dve", ft, bank(4)[:, :], fr, ALU.mult, [pb[4], r_f], [r_f])
                        stt(fo, ft, nlam, fo, ALU.mult, ALU.add, [r_f, r_c], [r_f])
                        tt("pool", ftb, fo, fo, ALU.mult, [r_f], [r_f])

                        def partB(qcols=qcols, qt=qt):
                            mm(bank(6)[:, :], onesb, ftb, True, True, [r_f, r_va], [pb[6]])
                            ts("dve", fr, bank(6)[:, :], 1.0 / 128, 1e-6, ALU.mult, ALU.add, [pb[6]], [r_f])
                            act(fr, fr, AF.Ln, [r_f], [r_f])
                            act(fr, fr, AF.Exp, [r_f], [r_f], scale=-0.5)
                            tt("dve", fo, fo, fr, ALU.mult, [r_f], [r_f])
                            ts("dve", oaT[:, hd, qcols], fo, gsubT[:, 0:1], None, ALU.mult, None, [r_f], [r_oaT[qt]])
                        pending.append((i + 8, partB))
                    while pending and (pending[0][0] <= i or i == len(its) - 1):
                        pending.pop(0)[1]()
            if dbg and "oaT" in dbg and b == 0:
                S.barrier()
                arena.off = a2
                dtmp = arena.alloc((2, S_LEN), F32)
                r_d = Res()
                for kk in range(4):
                    cp("dve", dtmp, oaT[:, kk * 2:kk * 2 + 2, :], r_oaT, [r_d])
                    S.dma("sp", dbg_d["oaT"][kk * 256:(kk + 1) * 256, :].rearrange("(k p) t -> p k t", p=128), dtmp, reads=[r_d])
            if stop == "p2":
                break
            S.barrier()
            arena.off = a2


            mT = arena.alloc((8, S_LEN), BF16)
            r_mT = RL(4)
            a3 = arena.off
            Wm = [arena.alloc((4, 8, 128), BF16) for _ in range(2)]
            r_Wm = RL(2)
            sg = [arena.alloc((512,), F32) for _ in range(2)]
            t12 = [arena.alloc((512,), F32) for _ in range(2)]
            r_sg = RL(2)
            r_t12 = RL(2)
            wba_v = Wd["w_branch_attn"].rearrange("(k p) n -> p k n", p=128)
            wbs_v = Wd["w_branch_ssd"].rearrange("(k p) n -> p k n", p=128)
            wout_v = Wd["w_out"].rearrange("(k p) n -> p k n", p=128)

            mstg = [arena.alloc((8, 128), F32) for _ in range(2)]
            r_mstg = RL(2)
            mstg_i = [0]

            def load_m_w(m, slot):
                cs_ = slice(m * 128, (m + 1) * 128)
                srcs_ = (wba_v[:, :, cs_], wbs_v[:, :, cs_], win_v[:, :, 6176 + m * 128:6176 + (m + 1) * 128],
                         win_v[:, :, 6176 + D + m * 128:6176 + D + (m + 1) * 128])
                for i_, src_ in enumerate(srcs_):
                    si = mstg_i[0] % 2
                    mstg_i[0] += 1
                    S.dma("sp", mstg[si], src_, writes=[r_mstg[si]])
                    cp("act" if i_ % 2 == 0 else "dve", Wm[slot][:, i_, :, :], mstg[si], [r_mstg[si]], [r_Wm[slot]])

            load_m_w(0, 0)
            it = 0
            for m in range(8):
                slot = m % 2
                if m + 1 < 8:
                    load_m_w(m + 1, 1 - slot)
                for t4 in range(4):
                    cols = slice(t4 * 512, (t4 + 1) * 512)
                    bb = 4 * (it % 2)
                    it += 1
                    srcs = ((oaT, [r_oaT[t4]]), (yT, r_yT[t4 * 4:t4 * 4 + 4]), (hT, r_hT[t4 * 4:t4 * 4 + 4]), (hT, r_hT[t4 * 4:t4 * 4 + 4]))
                    for i in range(4):
                        for kc in range(8):
                            mm(bank(bb + i)[:, :], Wm[slot][:, i, kc, :], srcs[i][0][:, kc, cols], kc == 0, kc == 7,
                               [r_Wm[slot]] + srcs[i][1], [pb[bb + i]])
                    for i in range(2):
                        act(sg[i], bank(bb + 2 + i)[:, :], AF.Sigmoid, [pb[bb + 2 + i]], [r_sg[i]])
                        tt("dve", t12[i], bank(bb + i)[:, :], sg[i], ALU.mult, [pb[bb + i], r_sg[i]], [r_t12[i]])
                    tt("dve", mT[:, m, cols], t12[0], t12[1], ALU.add, r_t12, [r_mT[t4]])
                    conv_some(2)
            S.barrier()
            arena.off = a1
            Wout = arena.alloc((8, D), BF16)
            r_Wout = Res()
            for hf in range(2):
                ldw(Wout[:, :, hf * 512:(hf + 1) * 512], wout_v[:, :, hf * 512:(hf + 1) * 512], r_Wout)
            xt4 = [arena.alloc((D,), F32) for _ in range(2)]
            r_xt4 = RL(2)
            x1t = [arena.alloc((D,), F32) for _ in range(2)]
            r_x1t = RL(2)
            assert arena.off <= a2
            arena.off = a3
            tmpfs = [arena.alloc((D,), F32) for _ in range(2)]
            r_tmpfs = RL(2)
            h2fs = [arena.alloc((8, 128), F32) for _ in range(2)]
            r_h2fs = RL(2)
            st4b = arena.alloc((8,), F32)
            st4s = [st4, st4b]
            r_st4s = RL(2)
            r_Wr = RL(NT)
            r_st4 = Res()
            r_rt = Res()
            r_x1 = RL(NT)
            if SPARSE:
                h2tfs = [arena.alloc((D,), F32)] * 2
                r_h2tfs = [Res()] * 2
                A2R = arena.alloc((D,), F32)
                B2R = arena.alloc((D,), F32)
                r_AR = Res()
                diag = arena.alloc((128,), F32)
                r_diag = Res()
                rank_all = arena.alloc((NT, 64), F32)
                r_rank = Res()
                maskb = arena.alloc((64,), BF16)
                r_mk = Res()
                a4 = arena.off
                arena.off = a1 - 8 * S_LEN * 2
                h2tok = arena.alloc((NT, D), BF16)
                arena.off = a4
                r_h2tok = RL(NT)
                for (dstR, idx_) in ((A2R, 2), (B2R, 3)):
                    for kc in range(8):
                        ts("dve", diag, ident[:], AB[:, b, idx_, kc:kc + 1], None, ALU.mult, None, [r_c, r_m], [r_diag])
                        bk = 6 + kc // 4
                        mm(bank(bk)[:, (kc % 4) * 128:(kc % 4) * 128 + 128], cst["ones"][:], diag, True, True, [r_c, r_diag], [pb[bk]])
                    cp("dve", dstR[:, 0:512], bank(6)[:, :], [pb[6]], [r_AR])
                    cp("act", dstR[:, 512:1024], bank(7)[:, :], [pb[7]], [r_AR])
                S.op("pool", lambda e: e.memset(csum[:], 0.0), writes=[r_rank])
            A2v = AB[:, b, 2, :]
            B2v = AB[:, b, 3, :]
            def partA(t):
                bi = t % 2
                sl = t % 2
                bo_ = 0 if sl == 0 else 4
                rb_ = bo_
                tmpf, h2f, h2tf = tmpfs[sl], h2fs[sl], h2tfs[sl]
                r_tmpf, r_h2f, r_h2tf = r_tmpfs[sl], r_h2fs[sl], r_h2tfs[sl]
                st4 = st4s[sl]
                r_st4 = r_st4s[sl]
                cs = slice(t * 128, (t + 1) * 128)
                S.dma("sp", xt4[bi], x_d[b, cs, :], writes=[r_xt4[bi]])
                for hf in range(2):
                    for kc in range(8):
                        mm(bank(bo_ + hf)[:, :], mT[:, kc, cs], Wout[:, kc, hf * 512:(hf + 1) * 512], kc == 0, kc == 7,
                           [r_mT[t // 4], r_Wout], [pb[bo_ + hf]])
                yield
                for hf in range(2):
                    act(tmpf[:, hf * 512:(hf + 1) * 512], bank(bo_ + hf)[:, :], AF.Square, [pb[bo_ + hf]], [r_tmpf, r_st4], accum_out=st4[:, hf:hf + 1])
                yield
                tt("dve", st4[:, 2:3], st4[:, 0:1], st4[:, 1:2], ALU.add, [r_st4], [r_st4])
                yield
                ts("dve", st4[:, 3:4], st4[:, 2:3], 1.0 / D, 1e-6, ALU.mult, ALU.add, [r_st4], [r_st4])
                yield
                act(st4[:, 3:4], st4[:, 3:4], AF.Ln, [r_st4], [r_st4])
                yield
                act(st4[:, 4:5], st4[:, 3:4], AF.Exp, [r_st4], [r_st4], scale=-0.5)
                yield
                for hf in range(2):
                    hs_ = slice(hf * 512, (hf + 1) * 512)
                    stt(tmpf[:, hs_], bank(bo_ + hf)[:, :], st4[:, 4:5], G1[:, b, hs_], ALU.mult, ALU.mult, [pb[bo_ + hf], r_st4, r_m], [r_tmpf])
                    yield
                tt("dve", x1t[bi], tmpf, xt4[bi], ALU.add, [r_tmpf, r_xt4[bi]], [r_x1t[bi]])
                yield
                S.dma("sp", out_d[b, cs, :], x1t[bi], reads=[r_x1t[bi]], writes=[r_x1[t]])
                act(tmpf, x1t[bi], AF.Square, [r_x1t[bi]], [r_tmpf, r_st4], accum_out=st4[:, 5:6])
                yield
                ts("dve", st4[:, 6:7], st4[:, 5:6], 1.0 / D, 1e-6, ALU.mult, ALU.add, [r_st4], [r_st4])
                yield
                act(st4[:, 6:7], st4[:, 6:7], AF.Ln, [r_st4], [r_st4])
                yield
                act(st4[:, 7:8], st4[:, 6:7], AF.Exp, [r_st4], [r_st4], scale=-0.5)
                yield
                ts("dve", tmpf, x1t[bi], st4[:, 7:8], None, ALU.mult, None, [r_x1t[bi], r_st4], [r_tmpf])
                yield
                if SPARSE:
                    tt("dve", h2tf, tmpf, A2R, ALU.mult, [r_tmpf, r_AR], [r_h2tf])
                    tt("pool", h2tok[:, t, :], h2tf, B2R, ALU.add, [r_h2tf, r_AR], [r_h2tok[t]])
                    yield
                for kc in range(8):
                    bk = bo_ + 2 + kc // 4
                    tr(bank(bk)[:, (kc % 4) * 128:(kc % 4) * 128 + 128], tmpf[:, kc * 128:(kc + 1) * 128], ident[:],
                       [r_tmpf, r_c], [pb[bk]], inc=(kc % 4 == 3))
                yield
                pv = PS[bo_ // 2 + 1][:, :].rearrange("p (a b) -> p a b", a=8, b=128)
                tt("dve", h2f, pv, A2v.unsqueeze(2).to_broadcast([128, 8, 128]), ALU.mult, [pb[bo_ + 2], pb[bo_ + 3], r_m], [r_h2f])
                yield
                tt("dve", h2f, h2f, B2v.unsqueeze(2).to_broadcast([128, 8, 128]), ALU.add, [r_h2f, r_m], [r_h2f])
                yield
                cp("act", hT[:, :, cs], h2f, [r_h2f], [r_hT[t]])
                for kc in range(8):
                    mm(bank(rb_)[:, 0:64], h2f[:, kc, :], wrt[:, kc, :], kc == 0, kc == 7, [r_h2f, r_c], [pb[rb_]])
                yield

            def partB(t):
                rb_ = 0 if t % 2 == 0 else 4
                kb_ = 1 if t % 2 == 0 else 5
                sc_ = rt[:, 0, :]
                sel_ = rt[:, 1, :]
                act(sc_, bank(rb_)[:, 0:64], AF.Exp, [pb[rb_]], [r_rt], scale=-1.0)
                ts("dve", sc_, sc_, 1.0, None, ALU.add, None, [r_rt], [r_rt])
                recip(sc_, sc_, [r_rt], [r_rt])
                tt("dve", sel_, sc_, rbiasB[:, :], ALU.add, [r_rt, r_c], [r_rt])
                sel3v = sel_.rearrange("p (a b) -> p a b", a=8, b=8)
                red(rt8[:, 0, :], sel3v, ALU.max, [r_rt], [r_rt])
                tt("dve", rt[:, 2, :].rearrange("p (a b) -> p a b", a=8, b=8), sel3v,
                   rt8[:, 0, :].unsqueeze(2).to_broadcast([128, 8, 8]), ALU.is_equal, [r_rt], [r_rt])
                stt(rt[:, 2, :], rt[:, 2, :], -BIG, sel_, ALU.mult, ALU.add, [r_rt], [r_rt])
                red(rt8[:, 1, :], rt[:, 2, :].rearrange("p (a b) -> p a b", a=8, b=8), ALU.max, [r_rt], [r_rt])
                tt("dve", rt8[:, 2, :], rt8[:, 0, :], rt8[:, 1, :], ALU.add, [r_rt], [r_rt])
                S.op("dve", lambda e: e.max(rt8[:, 3, :], rt8[:, 2, :]), reads=[r_rt], writes=[r_rt])
                ts("dve", rt8[:, 4, :], rt8[:, 2, :], rt8[:, 3, 3:4], None, ALU.is_ge, None, [r_rt], [r_rt])
                ts("dve", rt8[:, 4, :], rt8[:, 4, :], -1.0, BIG, ALU.add, ALU.mult, [r_rt], [r_rt])
                tt("dve", rt[:, 3, :].rearrange("p (a b) -> p a b", a=8, b=8), sel3v,
                   rt8[:, 4, :].unsqueeze(2).to_broadcast([128, 8, 8]), ALU.add, [r_rt], [r_rt])
                S.op("dve", lambda e: e.max(rt8[:, 5, :], rt[:, 3, :]), reads=[r_rt], writes=[r_rt])
                ts("dve", rt[:, 4, :], rt[:, 3, :], rt8[:, 5, 7:8], None, ALU.is_ge, None, [r_rt], [r_rt])
                tt("dve", rt[:, 4, :], rt[:, 4, :], sc_, ALU.mult, [r_rt], [r_rt])
                red(sm[:, 8:9], rt[:, 4, :], ALU.add, [r_rt], [r_rt])
                recip(sm[:, 9:10], sm[:, 8:9], [r_rt], [r_rt])
                ts("dve", Wr_all[:, t, :], rt[:, 4, :], sm[:, 9:10], 2.5, ALU.mult, ALU.mult, [r_rt], [r_Wr[t]])
                if SPARSE:
                    ts("dve", maskb, Wr_all[:, t, :], 0.0, None, ALU.is_gt, None, [r_Wr[t]], [r_mk])
                    mm(bank(kb_)[:, 0:64], trisb[:], maskb, True, True, [r_c, r_mk], [pb[kb_]], inc=False)
                    mm(bank(kb_)[:, 64:128], onesbp[:], maskb, True, True, [r_c, r_mk], [pb[kb_]])
                    tt("dve", rank_all[:, t, :], bank(kb_)[:, 0:64], csum[:], ALU.add, [pb[kb_], r_rank], [r_rank])
                    tt("dve", csum[:], csum[:], bank(kb_)[:, 64:128], ALU.add, [pb[kb_], r_rank], [r_rank])
            act_ = []
            nt_ = 0
            while nt_ < NT or act_:
                while len(act_) < 2 and nt_ < NT:
                    act_.append((nt_, partA(nt_)))
                    nt_ += 1
                nw_ = []
                for (t_, g_) in act_:
                    try:
                        next(g_)
                        nw_.append((t_, g_))
                    except StopIteration:
                        partB(t_)
                act_ = nw_
            h2T = hT
            r_h2T = r_hT
            if dbg and "Wr" in dbg and b == 0:
                S.dma("sp", dbg_d["Wr"].rearrange("(t p) e -> p t e", p=128), Wr_all[:], reads=r_Wr)
            if dbg and "h2T" in dbg and b == 0:
                S.barrier()
                arena.off = a1
                dtmp = arena.alloc((2, S_LEN), F32)
                r_d = Res()
                for kk in range(4):
                    cp("dve", dtmp, h2T[:, kk * 2:kk * 2 + 2, :], r_h2T, [r_d])
                    S.dma("sp", dbg_d["h2T"][kk * 256:(kk + 1) * 256, :].rearrange("(k p) t -> p k t", p=128), dtmp, reads=[r_d])
            if stop == "p4":
                break
            if SPARSE:
                r_sp = Res()
                r_d8 = Res()
                r_w8 = Res()
                r_xbuf = Res()
                r_ybuf = Res()
                r_eb = Res()
                ones64 = cst["ones"][:, 0:64]
                S.barrier()
                arena.off = a1
                ts("dve", sp64[:, 0, :], csum[:], 255.0, None, ALU.add, None, [r_rank], [r_sp])
                cp("dve", sp64i[:], sp64[:, 0, :], [r_sp], [r_sp])
                ts("dve", sp64i[:], sp64i[:], 8, None, ALU.arith_shift_right, None, [r_sp], [r_sp])
                ts("dve", sp64i[:], sp64i[:], 8, None, ALU.logical_shift_left, None, [r_sp], [r_sp])
                cp("dve", sp64[:, 1, :], sp64i[:], [r_sp], [r_sp])
                S.op("dve", lambda e: e.tensor_tensor_scan(sp64[:, 2, :], ones64, sp64[:, 1, :], 0.0, ALU.mult, ALU.add),
                     reads=[r_sp, r_c], writes=[r_sp])
                tt("dve", sp64[:, 3, :], sp64[:, 2, :], sp64[:, 1, :], ALU.subtract, [r_sp], [r_sp])
                cmpb = arena.alloc((32, 64), F32)
                ebf = arena.alloc((NBLK,), F32)
                for ch in range(NBLK // 32):
                    tt("dve", cmpb, sp64[:, 2, :].unsqueeze(1).to_broadcast([128, 32, 64]),
                       cst["jtab"][:, ch * 32:(ch + 1) * 32].unsqueeze(2).to_broadcast([128, 32, 64]), ALU.is_le, [r_sp, r_c], [r_sp])
                    red(ebf[:, ch * 32:(ch + 1) * 32], cmpb, ALU.add, [r_sp], [r_sp])
                ts("dve", ebf, ebf, 63.0, None, ALU.min, None, [r_sp], [r_sp])
                ts("dve", ebf, ebf, 128.0, cst["pidx"][:, 0:1], ALU.mult, ALU.add, [r_sp, r_c], [r_sp])
                cp("dve", eb_i[:], ebf, [r_sp], [r_eb])
                dm = arena.alloc((NT, 64), F32)
                mk = arena.alloc((NT, 64), F32)
                d8a = arena.alloc((NT, 8), F32)
                tt("dve", dm, rank_all, sp64[:, 3, :].unsqueeze(1).to_broadcast([128, NT, 64]), ALU.add, [r_rank, r_sp], [r_sp])
                ts("dve", mk, Wr_all[:], 0.0, None, ALU.is_gt, None, r_Wr, [r_sp])
                stt(dm.rearrange("p a b -> p (a b)"), dm.rearrange("p a b -> p (a b)"), 1.0, mk.rearrange("p a b -> p (a b)"), ALU.add, ALU.mult, [r_sp], [r_sp])
                ts("dve", dm, dm, -1.0, None, ALU.add, None, [r_sp], [r_sp])
                for t in range(NT):
                    S.op("dve", lambda e, t=t: e.max(d8a[:, t, :], dm[:, t, :]), reads=[r_sp], writes=[r_sp])
                cp("dve", dest8u[:], d8a, [r_sp], [r_d8])
                for t in range(NT):
                    for j in range(8):
                        S.dma_fn("pool", (lambda e, t=t, j=j: e.indirect_dma_start(
                            out=xbuf, out_offset=bass.IndirectOffsetOnAxis(ap=dest8u[:, t, j:j + 1], axis=0),
                            in_=h2tok[:, t, :], in_offset=None)), reads=[r_d8, r_h2tok[t]], writes=[Res()])
                for j in range(8):
                    tt("dve", mk, dm, d8a[:, :, j:j + 1].to_broadcast([128, NT, 64]), ALU.is_equal, [r_sp], [r_sp])
                    tt("dve", mk, mk, Wr_all[:], ALU.mult, [r_sp] + r_Wr, [r_sp])
                    red(w8[:, :, j:j + 1].rearrange("p a b -> p (a b)"), mk, ALU.add, [r_sp], [r_w8])
                conv_some(1000)
                S.barrier()
                arena.off = a1 - 8 * S_LEN * 2

                NWS = 6
                Wblk = [(arena.alloc((8, 256), BF16), arena.alloc((8, 256), BF16), arena.alloc((2, D), BF16)) for _ in range(NWS)]
                r_Wb = [RL(3) for _ in range(NWS)]
                xtok = [arena.alloc((2, D), BF16) for _ in range(NWS)]
                r_xtok = RL(NWS)
                xTb = [arena.alloc((8, 256), BF16) for _ in range(2)]
                r_xTb = RL(2)
                sgb = arena.alloc((512,), F32)
                r_sgb = Res()
                hidb = [arena.alloc((2, 256), BF16) for _ in range(2)]
                r_hidb = RL(2)
                ysb = [arena.alloc((2, D), BF16) for _ in range(2)]
                r_ysb = RL(2)
                def load_blk(j, slot):
                    for wi_, src_t in enumerate((wgb, wub, wdb)):
                        dst = Wblk[slot][wi_]
                        dst2 = dst.rearrange("p a b -> p (a b)")
                        S.dma_fn("pool", (lambda e, dst2=dst2, src_t=src_t, j=j: e.indirect_dma_start(
                            out=dst2, out_offset=None, in_=src_t,
                            in_offset=bass.IndirectOffsetOnAxis(ap=eb_i[:, j:j + 1], axis=0))), reads=[r_eb, r_wconv], writes=[r_Wb[slot][wi_]])
                    S.dma("sp", xtok[slot], xbuf[j * BLK:(j + 1) * BLK, :].rearrange("(s p) n -> p s n", p=128), reads=[r_xbuf], writes=[r_xtok[slot]])

                def emit_T(j):
                    slot = j % 2
                    ws = j % NWS
                    for s_ in range(2):
                        for kc in range(8):
                            bk = kc // 4
                            o_ = (kc % 4) * 256 + s_ * 128
                            tr(bankb(bk)[:, o_:o_ + 128], xtok[ws][:, s_, kc * 128:(kc + 1) * 128], identb[:], [r_xtok[ws], r_c], [pb[bk]],
                               inc=(s_ == 1 and kc % 4 == 3))
                    cp("act", xTb[slot][:, 0:4, :].rearrange("p a b -> p (a b)"), bankb(0)[:, 0:1024], [pb[0]], [r_xTb[slot]])
                    cp("dve", xTb[slot][:, 4:8, :].rearrange("p a b -> p (a b)"), bankb(1)[:, 0:1024], [pb[1]], [r_xTb[slot]])

                def emit_GU(j):
                    slot = j % 2
                    ws = j % NWS
                    Wg_, Wu_, _ = Wblk[ws]
                    for (Wx, bk, wi_) in ((Wg_, 2, 0), (Wu_, 3, 1)):
                        for ffc in range(2):
                            for kc in range(8):
                                mm(bank(bk)[:, ffc * 256:(ffc + 1) * 256], Wx[:, kc, ffc * 128:(ffc + 1) * 128], xTb[slot][:, kc, :], kc == 0, kc == 7,
                                   [r_Wb[ws][wi_], r_xTb[slot]], [pb[bk]])
                    act(sgb, bank(2)[:, :], AF.Silu, [pb[2]], [r_sgb])
                    tt("dve", hidb[slot].rearrange("p a b -> p (a b)"), bank(3)[:, :], sgb, ALU.mult, [pb[3], r_sgb], [r_hidb[slot]])

                def emit_D(j):
                    slot = j % 2
                    ws = j % NWS
                    Wdn = Wblk[ws][2]
                    for s_ in range(2):
                        for hf in range(2):
                            bk = 4 + s_ * 2 + hf
                            for ffc in range(2):
                                mm(bank(bk)[:, :], hidb[slot][:, ffc, s_ * 128:(s_ + 1) * 128], Wdn[:, ffc, hf * 512:(hf + 1) * 512], ffc == 0, ffc == 1,
                                   [r_hidb[slot], r_Wb[ws][2]], [pb[bk]])
                            cp("act" if hf == 0 else "dve", ysb[slot][:, s_, hf * 512:(hf + 1) * 512], bank(bk)[:, :], [pb[bk]], [r_ysb[slot]])
                    S.dma("sp", ybuf[j * BLK:(j + 1) * BLK, :].rearrange("(s p) n -> p s n", p=128), ysb[slot], reads=[r_ysb[slot]], writes=[Res()])

                for j0_ in range(NWS - 1):
                    load_blk(j0_, j0_)
                emit_T(0)
                for j in range(NBLK):
                    if j + NWS - 1 < NBLK:
                        load_blk(j + NWS - 1, (j + NWS - 1) % NWS)
                    emit_GU(j)
                    if j + 1 < NBLK:
                        emit_T(j + 1)
                    emit_D(j)
                S.barrier()
                arena.off = a1

                Wsh = (arena.alloc((8, 256), BF16), arena.alloc((8, 256), BF16), arena.alloc((2, D), BF16))
                r_Wsh = Res()
                ldw(Wsh[0], Wd["w_sh_gate"].rearrange("(k p) f -> p k f", p=128), r_Wsh)
                ldw(Wsh[1], Wd["w_sh_up"].rearrange("(k p) f -> p k f", p=128), r_Wsh)
                ldw(Wsh[2], Wd["w_sh_down"].rearrange("(c p) n -> p c n", p=128), r_Wsh)
                sgs = arena.alloc((512,), F32)
                r_sgs = Res()
                hsh = [arena.alloc((512,), BF16) for _ in range(4)]
                r_hsh = RL(4)
                NYG = 16
                NDG = 16
                dgs = [arena.alloc((128,), BF16) for _ in range(NDG)]
                r_dg = RL(NDG)
                dgi = [0]
                yg = [arena.alloc((D,), BF16) for _ in range(NYG)]
                r_yg = RL(NYG)
                facc = [arena.alloc((D,), F32) for _ in range(2)]
                r_facc = RL(2)
                xo = [arena.alloc((D,), F32) for _ in range(2)]
                r_xo = RL(2)
                x1r = [arena.alloc((D,), F32) for _ in range(2)]
                r_x1r = RL(2)
                junk6 = arena.alloc((D,), F32)
                r_j6 = Res()
                r_st6 = Res()
                gi = 0
                for t4 in range(4):
                    cols = slice(t4 * 512, (t4 + 1) * 512)
                    rh = r_hT[t4 * 4:t4 * 4 + 4]
                    hh = []
                    for ffc in range(2):
                        for kc in range(8):
                            mm(bank(ffc)[:, :], Wsh[0][:, kc, ffc * 128:(ffc + 1) * 128], hT[:, kc, cols], kc == 0, kc == 7, [r_Wsh] + rh, [pb[ffc]])
                        for kc in range(8):
                            mm(bank(2 + ffc)[:, :], Wsh[1][:, kc, ffc * 128:(ffc + 1) * 128], hT[:, kc, cols], kc == 0, kc == 7, [r_Wsh] + rh, [pb[2 + ffc]])
                    for ffc in range(2):
                        hx = (t4 * 2 + ffc) % 4
                        act(sgs, bank(ffc)[:, :], AF.Silu, [pb[ffc]], [r_sgs])
                        tt("dve", hsh[hx], bank(2 + ffc)[:, :], sgs, ALU.mult, [pb[2 + ffc], r_sgs], [r_hsh[hx]])
                        hh.append(hx)
                    for sub in range(4):
                        t = t4 * 4 + sub
                        bi = t % 2
                        cs = slice(t * 128, (t + 1) * 128)
                        S.dma("sp", x1r[bi], out_d[b, cs, :], reads=[r_x1[t]], writes=[r_x1r[bi]])
                        gl_ = []
                        for j in range(8):
                            g_ = gi % NYG
                            gi += 1
                            S.dma_fn("pool", (lambda e, t=t, j=j, g_=g_: e.indirect_dma_start(
                                out=yg[g_], out_offset=None, in_=ybuf,
                                in_offset=bass.IndirectOffsetOnAxis(ap=dest8u[:, t, j:j + 1], axis=0))), reads=[r_d8], writes=[r_yg[g_]])
                            dj = dgi[0] % NDG
                            dgi[0] += 1
                            ts("dve", dgs[dj], identb[:], w8[:, t, j:j + 1], None, ALU.mult, None, [r_c, r_d8, r_w8], [r_dg[dj]])
                            gl_.append((g_, dj))
                        for hf in range(2):
                            bk = 4 + 2 * bi + hf
                            for ffc in range(2):
                                mm(bank(bk)[:, :], hsh[hh[ffc]][:, sub * 128:(sub + 1) * 128], Wsh[2][:, ffc, hf * 512:(hf + 1) * 512],
                                   ffc == 0, False, [r_hsh[hh[ffc]], r_Wsh], [pb[bk]], inc=False)
                            for j, (g_, dj) in enumerate(gl_):
                                mm(bank(bk)[:, :], dgs[dj], yg[g_][:, hf * 512:(hf + 1) * 512], False, j == 7, [r_dg[dj], r_yg[g_]], [pb[bk]])
                            cp("act", facc[bi][:, hf * 512:(hf + 1) * 512], bank(bk)[:, :], [pb[bk]], [r_facc[bi]])
                        if dbg and "acc" in dbg and b == 0:
                            S.dma("sp", dbg_d["acc"][cs, :], facc[bi], reads=[r_facc[bi]])
                        act(junk6, facc[bi], AF.Square, [r_facc[bi]], [r_j6, r_st6], accum_out=st6[:, 0:1])
                        rstd_from_ssq(st6[:, 2:3], st6[:, 0:1], D, st6[:, 1:2], r_st6)
                        stt(xo[bi], facc[bi], st6[:, 2:3], G2[:, b, :], ALU.mult, ALU.mult, [r_facc[bi], r_st6, r_m], [r_xo[bi]])
                        tt("dve", xo[bi], xo[bi], x1r[bi], ALU.add, [r_xo[bi], r_x1r[bi]], [r_xo[bi]])
                        S.dma("sp", out_d[b, cs, :], xo[bi], reads=[r_xo[bi]], writes=[r_x1[t]])
                S.barrier()
                continue
            S.barrier()
            arena.off = a1

            acc = arena.alloc((NT, D), F32)
            r_acc = RL(NT)
            a5 = arena.off
            We = [(arena.alloc((8, 256), BF16), arena.alloc((8, 256), BF16), arena.alloc((2, D), BF16)) for _ in range(2)]
            r_We = RL(2)
            sgm = [arena.alloc((512,), F32) for _ in range(2)]
            r_sgm = RL(2)
            hid = [arena.alloc((512,), BF16) for _ in range(4)]
            r_hid = RL(4)
            ones1 = cst["ones"][:, 0:1]

            def load_e_w(e_, slot):
                if e_ < NEXP:
                    g_, u_, d__ = Wd["w_e_gate"][e_], Wd["w_e_up"][e_], Wd["w_e_down"][e_]
                else:
                    g_, u_, d__ = Wd["w_sh_gate"], Wd["w_sh_up"], Wd["w_sh_down"]
                ldw(We[slot][0], g_.rearrange("(k p) f -> p k f", p=128), r_We[slot])
                ldw(We[slot][1], u_.rearrange("(k p) f -> p k f", p=128), r_We[slot])
                ldw(We[slot][2], d__.rearrange("(c p) n -> p c n", p=128), r_We[slot])

            load_e_w(0, 0)
            hi_ = 0
            for e_ in range(NEXP + 1):
                slot = e_ % 2
                if e_ + 1 <= NEXP:
                    load_e_w(e_ + 1, 1 - slot)
                Wg_, Wu_, Wdn = We[slot]
                for t4 in range(4):
                    cols = slice(t4 * 512, (t4 + 1) * 512)
                    rh = r_h2T[t4 * 4:t4 * 4 + 4]
                    for ffc in range(2):
                        for kc in range(8):
                            mm(bank(ffc)[:, :], Wg_[:, kc, ffc * 128:(ffc + 1) * 128], h2T[:, kc, cols], kc == 0, kc == 7, [r_We[slot]] + rh, [pb[ffc]])
                        for kc in range(8):
                            mm(bank(2 + ffc)[:, :], Wu_[:, kc, ffc * 128:(ffc + 1) * 128], h2T[:, kc, cols], kc == 0, kc == 7, [r_We[slot]] + rh, [pb[2 + ffc]])
                    hh = []
                    for ffc in range(2):
                        act(sgm[ffc], bank(ffc)[:, :], AF.Silu, [pb[ffc]], [r_sgm[ffc]])
                        hx = hi_ % 4
                        hi_ += 1
                        tt("dve", hid[hx], bank(2 + ffc)[:, :], sgm[ffc], ALU.mult, [pb[2 + ffc], r_sgm[ffc]], [r_hid[hx]])
                        hh.append(hx)
                    for sub in range(4):
                        t = t4 * 4 + sub
                        for hf in range(2):
                            bk = 4 + (sub * 2 + hf) % 4
                            for ffc in range(2):
                                mm(bank(bk)[:, :], hid[hh[ffc]][:, sub * 128:(sub + 1) * 128], Wdn[:, ffc, hf * 512:(hf + 1) * 512],
                                   ffc == 0, ffc == 1, [r_hid[hh[ffc]], r_We[slot]], [pb[bk]])
                            wcol = Wr_all[:, t, e_:e_ + 1] if e_ < NEXP else ones1
                            a_ = acc[:, t, hf * 512:(hf + 1) * 512]
                            if e_ == 0:
                                ts("dve", a_, bank(bk)[:, :], wcol, None, ALU.mult, None, [pb[bk], r_Wr[t]], [r_acc[t]])
                            else:
                                stt(a_, bank(bk)[:, :], wcol, a_, ALU.mult, ALU.add, [pb[bk], r_Wr[t], r_acc[t]], [r_acc[t]])
            if dbg and "acc" in dbg and b == 0:
                S.dma("sp", dbg_d["acc"].rearrange("(t p) n -> p t n", p=128), acc, reads=r_acc)

            S.barrier()
            arena.off = a5
            xo = [arena.alloc((D,), F32) for _ in range(2)]
            r_xo = RL(2)
            x1r = [arena.alloc((D,), F32) for _ in range(2)]
            r_x1r = RL(2)
            junk6 = arena.alloc((D,), F32)
            r_j6 = Res()
            r_st6 = Res()
            for t in range(NT):
                bi = t % 2
                cs = slice(t * 128, (t + 1) * 128)
                S.dma("sp", x1r[bi], out_d[b, cs, :], reads=[r_x1[t]], writes=[r_x1r[bi]])
                act(junk6, acc[:, t, :], AF.Square, [r_acc[t]], [r_j6, r_st6], accum_out=st6[:, 0:1])
                rstd_from_ssq(st6[:, 2:3], st6[:, 0:1], D, st6[:, 1:2], r_st6)
                stt(xo[bi], acc[:, t, :], st6[:, 2:3], G2[:, b, :], ALU.mult, ALU.mult, [r_acc[t], r_st6, r_m], [r_xo[bi]])
                tt("dve", xo[bi], xo[bi], x1r[bi], ALU.add, [r_xo[bi], r_x1r[bi]], [r_xo[bi]])
                S.dma("sp", out_d[b, cs, :], xo[bi], reads=[r_xo[bi]], writes=[r_x1[t]])
            S.barrier()


        S.emit()
    return nc


def make_in_maps(inputs, n_cores=8):
    consts = host_consts()
    shared = {}
    for k, shp in WSHAPES.items():
        shared[k] = np.ascontiguousarray(np.asarray(inputs[k], dtype=np.float32).reshape(shp))
    for k, v in consts.items():
        shared["k_" + k] = np.ascontiguousarray(v.astype(np.float32))
    x = np.asarray(inputs["x"], dtype=np.float32)
    c = np.asarray(inputs["c"], dtype=np.float32)
    ctx = np.asarray(inputs["ctx"], dtype=np.float32)
    c_ctx = np.asarray(inputs["c_ctx"], dtype=np.float32)
    maps = []
    for i in range(n_cores):
        m = dict(shared)
        m["x"] = np.ascontiguousarray(x[i * NB:(i + 1) * NB])
        m["ctx"] = np.ascontiguousarray(ctx[i * NB:(i + 1) * NB])
        m["c3"] = np.ascontiguousarray(np.concatenate([c[i * NB:(i + 1) * NB], c_ctx[None, :]], axis=0))
        maps.append(m)
    return maps


def kernel(**inputs):
    nc = build()
    maps = make_in_maps(inputs)
    res = run_bass_kernel_spmd(nc, maps, core_ids=list(range(8)))
    return np.concatenate([r["out"] for r in res.results], axis=0).astype(np.float32)
```

```python
from contextlib import ExitStack
import numpy as np
import concourse.bass as bass
import concourse.mybir as mybir
from concourse.bass_utils import run_bass_kernel_spmd

F32 = mybir.dt.float32
BF16 = mybir.dt.bfloat16
AF = mybir.ActivationFunctionType
ALU = mybir.AluOpType
AX = mybir.AxisListType

S_LEN = 2048
L_CTX = 256
D = 1024
NB = 2
NT = S_LEN // 128
NEXP = 64
BIG = 30000.0
SPARSE = True
BLK = 256
NBLK = (S_LEN * 8) // BLK + NEXP
NSLOT = NBLK * BLK
I32 = mybir.dt.int32
U32 = mybir.dt.uint32


class Res:
    __slots__ = ("w", "r")

    def __init__(self):
        self.w = None
        self.r = []


def RL(n):
    return [Res() for _ in range(n)]


ENGS = ("pe", "act", "dve", "pool", "sp")
NDMA = 48


class Sched:
    def __init__(self, nc, stack):
        self.nc = nc
        self.esem = {e: stack.enter_context(nc.semaphore("s_" + e)) for e in ENGS if e != "sp"}
        self.dsem = [stack.enter_context(nc.semaphore("d%d" % i)) for i in range(NDMA)]
        self.dcnt = [0] * NDMA
        self.dnext = 0
        self.ops = {e: [] for e in ENGS}
        self.cnt = {e: 0 for e in ENGS}
        self.seen = {e: {} for e in ENGS}

    def _deps(self, eng, reads, writes):
        deps = {}

        def add(tok, kind):
            if tok is None:
                return
            key, val = tok
            if key == eng and (eng == "pe" or kind != "raw"):
                return
            if deps.get(key, 0) < val:
                deps[key] = val

        for r in reads:
            add(r.w, "raw")
        for w in writes:
            add(w.w, "waw")
            for t in w.r:
                add(t, "war")
        waits = []
        for key, val in deps.items():
            if self.seen[eng].get(key, 0) >= val:
                continue
            self.seen[eng][key] = val
            waits.append((key, val))
        return waits

    def _mark(self, tok, reads, writes):
        for r in reads:
            r.r.append(tok)
            if len(r.r) > 48:
                best = {}
                for k, v in r.r:
                    if best.get(k, 0) < v:
                        best[k] = v
                r.r = list(best.items())
        for w in writes:
            w.w = tok
            w.r = []

    def op(self, eng, fn, reads=(), writes=(), inc=True):
        waits = self._deps(eng, reads, writes)
        if inc:
            self.cnt[eng] += 1
            tok = (eng, self.cnt[eng])
        else:
            tok = (eng, self.cnt[eng] + 1)
        self.ops[eng].append((waits, fn, ("e", inc)))
        self._mark(tok, reads, writes)

    def dma(self, q, out, in_, reads=(), writes=(), **kw):
        return self.dma_fn(q, (lambda e: e.dma_start(out=out, in_=in_, **kw)), reads, writes)

    def dma_fn(self, q, fn, reads=(), writes=()):
        k = self.dnext
        self.dnext = (self.dnext + 1) % NDMA
        waits = self._deps(q, reads, writes)
        key = "d%d" % k
        prev = 16 * self.dcnt[k]
        if prev and self.seen[q].get(key, 0) < prev:
            self.seen[q][key] = prev
            waits.append((key, prev))
        self.dcnt[k] += 1
        tok = (key, 16 * self.dcnt[k])
        self.ops[q].append((waits, fn, ("d", k)))
        self._mark(tok, reads, writes)
        return tok

    def barrier(self):
        toks = [(e, self.cnt[e]) for e in ("pe", "act", "dve", "pool") if self.cnt[e]]
        toks += [("d%d" % k, 16 * self.dcnt[k]) for k in range(NDMA) if self.dcnt[k]]
        for e in ENGS:
            waits = []
            for key, val in toks:
                if key == e or self.seen[e].get(key, 0) >= val:
                    continue
                self.seen[e][key] = val
                waits.append((key, val))
            self.ops[e].append((waits, None, None))

    def _sem(self, key):
        if key in self.esem:
            return self.esem[key]
        return self.dsem[int(key[1:])]

    def emit(self):
        nc = self.nc
        finals = [("d%d" % k, 16 * self.dcnt[k]) for k in range(NDMA) if self.dcnt[k]]
        self.ops["sp"].append((finals, None, None))

        def runner(ename):
            def run(e):
                for waits, fn, kind in self.ops[ename]:
                    for key, val in waits:
                        e.wait_ge(self._sem(key), val)
                    if fn is None:
                        continue
                    ins = fn(e)
                    if kind[0] == "e":
                        if kind[1]:
                            ins.then_inc(self.esem[ename], 1)
                    else:
                        ins.then_inc(self.dsem[kind[1]], 16)
            return run

        with nc.Block() as block:
            block.sync(runner("sp"))
            block.tensor(runner("pe"))
            block.scalar(runner("act"))
            block.vector(runner("dve"))
            block.gpsimd(runner("pool"))


class Arena:
    def __init__(self, nc, st, nbytes):
        self.t = st.enter_context(nc.sbuf_tensor("arena", [128, nbytes // 4], F32))
        self.v = {F32: self.t, BF16: self.t.bitcast(BF16)}
        self.off = 0
        self.cap = nbytes

    def alloc(self, free, dt):
        sz = 4 if dt == F32 else 2
        n = int(np.prod(free))
        off = self.off
        self.off += (n * sz + 63) // 64 * 64
        assert self.off <= self.cap, ("arena overflow", self.off, self.cap)
        v = self.v[dt][:, off // sz: off // sz + n]
        if len(free) == 2:
            v = v.rearrange("p (a b) -> p a b", a=free[0], b=free[1])
        elif len(free) == 3:
            v = v.rearrange("p (a b c) -> p a b c", a=free[0], b=free[1], c=free[2])
        return v


def host_consts():
    c = {}
    c["ident"] = np.eye(128, dtype=np.float32)
    k = np.arange(128)
    c["trif"] = (k[:, None] <= k[None, :]).astype(np.float32)
    c["trib"] = (k[:, None] >= k[None, :]).astype(np.float32)
    c["negf"] = np.where(k[:, None] <= k[None, :], 0.0, -BIG).astype(np.float32)
    c["negb"] = np.where(k[:, None] >= k[None, :], 0.0, -BIG).astype(np.float32)
    c["ones"] = np.ones((128, 128), np.float32)
    c["tris"] = (k[:, None] < k[None, :]).astype(np.float32)
    c["pswap"] = (k[:, None] == (k[None, :] ^ 32)).astype(np.float32)
    c["pidx"] = np.tile(np.arange(128, dtype=np.float32)[:, None], (1, 128))
    c["jtab"] = np.tile((np.arange(128, dtype=np.float32) * BLK)[None, :], (128, 1))
    sel = np.zeros((3, 3, 128), np.float32)
    for b in range(3):
        sel[b, b, :] = 1.0
    c["sel3"] = sel
    t = np.arange(S_LEN)
    row = (t // 64).astype(np.float32)
    col = (t % 64).astype(np.float32)
    inv = (np.float32(10000.0) ** (-np.arange(0, 32, 2, dtype=np.float32) / np.float32(32))).astype(np.float32)
    ang = np.concatenate([row[:, None] * inv[None, :], col[:, None] * inv[None, :]], axis=-1)
    ang = np.concatenate([ang, ang], axis=-1).astype(np.float32)
    cos = np.cos(ang).astype(np.float32).T
    sin = np.sin(ang).astype(np.float32).T
    sign = np.where(np.arange(64) < 32, -1.0, 1.0).astype(np.float32)[:, None]
    c["cosT"] = np.concatenate([cos, cos], axis=0)
    c["sinT"] = np.concatenate([sin * sign, sin * sign], axis=0)
    return c


CONST_SHAPES = {"ident": [128, 128], "trif": [128, 128], "trib": [128, 128], "negf": [128, 128],
                "negb": [128, 128], "ones": [128, 128], "tris": [128, 128], "jtab": [128, 128], "pidx": [128, 128], "pswap": [128, 128], "sel3": [3, 3, 128],
                "cosT": [128, S_LEN], "sinT": [128, S_LEN]}

WSHAPES = {
    "w_ada": [D, 6 * D], "b_ada": [6 * D], "g_pre_mix": [D], "g_post_mix": [D], "g_pre_ffn": [D],
    "g_post_ffn": [D], "w_in": [D, 8224], "lam_q1": [64], "lam_k1": [64], "lam_q2": [64], "lam_k2": [64],
    "g_attn_subln": [128], "conv_w": [3, 2048], "conv_b": [2048], "dt_bias": [32], "a_log": [32],
    "d_skip": [16], "g_ssd_norm": [D], "w_branch_attn": [D, D], "w_branch_ssd": [D, D], "w_out": [D, D],
    "w_router": [D, 64], "router_bias": [64], "w_e_gate": [NEXP, D, 256], "w_e_up": [NEXP, D, 256],
    "w_e_down": [NEXP, 256, D], "w_sh_gate": [D, 256], "w_sh_up": [D, 256], "w_sh_down": [256, D],
}


def build(dbg=None, stop=None, nseq=NB):
    nc = bass.Bass("TRN2", target_bir_lowering=False)
    di = lambda name, shape: nc.dram_tensor(name, shape, F32, kind="ExternalInput").ap()
    x_d = di("x", [NB, S_LEN, D])
    c3_d = di("c3", [3, D])
    ctx_d = di("ctx", [NB, L_CTX, D])
    Wd = {k: di(k, v) for k, v in WSHAPES.items()}
    Cd = {k: di("k_" + k, v) for k, v in CONST_SHAPES.items()}
    out_d = nc.dram_tensor("out", [NB, S_LEN, D], F32, kind="ExternalOutput").ap()
    dbg_d = {}
    if dbg:
        for k, shp in dbg.items():
            dbg_d[k] = nc.dram_tensor("dbg_" + k, shp, F32, kind="ExternalOutput").ap()

    wgb = nc.dram_tensor("wgb", [NEXP * 128, 2048], BF16, kind="Internal").ap()
    wub = nc.dram_tensor("wub", [NEXP * 128, 2048], BF16, kind="Internal").ap()
    wdb = nc.dram_tensor("wdb", [NEXP * 128, 2048], BF16, kind="Internal").ap()
    xbuf = nc.dram_tensor("xbuf", [NSLOT, D], BF16, kind="Internal").ap()
    ybuf = nc.dram_tensor("ybuf", [NSLOT, D], BF16, kind="Internal").ap()
    st = ExitStack()
    with st:
        S = Sched(nc, st)
        r_wconv = Res()
        conv_list = []
        for e_ in range(NEXP):
            rows_ = slice(e_ * 128, (e_ + 1) * 128)
            conv_list.append((wgb[rows_, :].rearrange("p (k f) -> p k f", k=8), Wd["w_e_gate"][e_].rearrange("(k p) f -> p k f", p=128)))
            conv_list.append((wub[rows_, :].rearrange("p (k f) -> p k f", k=8), Wd["w_e_up"][e_].rearrange("(k p) f -> p k f", p=128)))
            conv_list.append((wdb[rows_, :].rearrange("p (c n) -> p c n", c=2), Wd["w_e_down"][e_].rearrange("(c p) n -> p c n", p=128)))

        def conv_some(n):
            for _ in range(n):
                if conv_list and SPARSE:
                    d_, s_ = conv_list.pop(0)
                    S.dma("pool", d_, s_, writes=[r_wconv])
        sbt = lambda name, shape, dt=F32: st.enter_context(nc.sbuf_tensor(name, shape, dt))
        PSALL = st.enter_context(nc.psum_tensor("psall", [128, 4096], F32))
        PSALLB = PSALL.bitcast(BF16)
        PS = [PSALL[:, i * 1024:(i + 1) * 1024] for i in range(4)]
        PSB = [PSALLB[:, i * 2048:(i + 1) * 2048] for i in range(4)]
        pb = RL(8)

        def bank(b):
            return PS[b // 2][:, (b % 2) * 512:(b % 2) * 512 + 512]

        def bankb(b):
            return PSB[b // 2][:, (b % 2) * 1024:(b % 2) * 1024 + 1024]

        def mm(out, lhsT, rhs, start, stop_, R, W, inc=None):
            if inc is None:
                inc = stop_
            S.op("pe", lambda e: e.matmul(out, lhsT, rhs, start=start, stop=stop_), reads=R, writes=W, inc=inc)

        def tr(out, in_, idn, R, W, inc=True):
            S.op("pe", lambda e: e.transpose(out, in_, idn), reads=R, writes=W, inc=inc)

        def act(out, in_, func, R, W, **kw):
            S.op("act", lambda e: e.activation(out, in_, func, **kw), reads=R, writes=W)

        def tt(eng, out, a, b, op, R, W):
            S.op(eng, lambda e: e.tensor_tensor(out, a, b, op), reads=R, writes=W)

        def ts(eng, out, a, s1, s2, op0, op1, R, W):
            if s2 is None:
                S.op(eng, lambda e: e.tensor_scalar(out, a, s1, None, op0), reads=R, writes=W)
            else:
                S.op(eng, lambda e: e.tensor_scalar(out, a, s1, s2, op0, op1), reads=R, writes=W)

        def stt(out, in0, sc, in1, op0, op1, R, W):
            S.op("dve", lambda e: e.scalar_tensor_tensor(out, in0, sc, in1, op0, op1), reads=R, writes=W)

        def cp(eng, out, in_, R, W):
            if eng == "act":
                S.op("act", lambda e: e.copy(out, in_), reads=R, writes=W)
            else:
                S.op(eng, lambda e: e.tensor_copy(out, in_), reads=R, writes=W)

        def red(out, in_, op, R, W, axis=AX.X):
            S.op("dve", lambda e: e.tensor_reduce(out, in_, axis, op), reads=R, writes=W)

        def recip(out, in_, R, W):
            S.op("dve", lambda e: e.reciprocal(out, in_), reads=R, writes=W)

        def rstd_from_ssq(dst, ssq, n, scratch, R):
            ts("dve", scratch, ssq, 1.0 / n, 1e-6, ALU.mult, ALU.add, [R], [R])
            act(scratch, scratch, AF.Ln, [R], [R])
            act(dst, scratch, AF.Exp, [R], [R], scale=-0.5)

        r_c = Res()
        cst = {}
        for k in ("ident", "trif", "trib", "negf", "negb", "ones", "tris", "jtab", "pidx", "pswap"):
            cst[k] = sbt("c_" + k, [128, 128])
            S.dma("sp", cst[k][:], Cd[k], writes=[r_c])
        ident = cst["ident"]
        identb = sbt("identb", [128, 128], BF16)
        trisb = sbt("trisb", [128, 128], BF16)
        onesbp = sbt("onesbp", [128, 128], BF16)
        cp("dve", trisb[:], cst["tris"][:], [r_c], [r_c])
        cp("dve", onesbp[:], cst["ones"][:], [r_c], [r_c])
        pswapb = sbt("pswapb", [128, 128], BF16)
        cp("dve", pswapb[:], cst["pswap"][:], [r_c], [r_c])
        mhalf = sbt("mhalf", [128, 1])
        S.op("pool", lambda e: e.memset(mhalf[:], -0.5), writes=[r_c])
        cp("dve", identb[:], ident[:], [r_c], [r_c])
        sel3 = sbt("sel3", [3, 3, 128])
        S.dma("sp", sel3[:], Cd["sel3"], writes=[r_c])

        def bcast_load(name, n, parts=128):
            t = sbt("b_" + name, [parts, n])
            S.dma("sp", t[:], Wd[name].partition_broadcast(parts), writes=[r_c])
            return t

        rbiasB = bcast_load("router_bias", 64)
        dtbB = bcast_load("dt_bias", 32)
        alogB = bcast_load("a_log", 32)
        dskB = bcast_load("d_skip", 16)
        lamv = sbt("lamv", [128, 4, 64])
        for i, nm in enumerate(("lam_q1", "lam_k1", "lam_q2", "lam_k2")):
            S.dma("sp", lamv[:, i, :], Wd[nm].partition_broadcast(128), writes=[r_c])
        gpmT = sbt("gpmT", [128, 8])
        gpfT = sbt("gpfT", [128, 8])
        cwT = sbt("cwT", [128, 16, 3])
        cbT = sbt("cbT", [128, 16])
        S.dma("sp", gpmT[:], Wd["g_pre_mix"].rearrange("(k p) -> p k", p=128), writes=[r_c], allow_slow_non_contiguous=True)
        S.dma("sp", gpfT[:], Wd["g_pre_ffn"].rearrange("(k p) -> p k", p=128), writes=[r_c], allow_slow_non_contiguous=True)
        S.dma("sp", cbT[:], Wd["conv_b"].rearrange("(k p) -> p k", p=128), writes=[r_c], allow_slow_non_contiguous=True)
        for i in range(3):
            S.dma("sp", cwT[:, :, i], Wd["conv_w"][i].rearrange("(k p) -> p k", p=128), writes=[r_c], allow_slow_non_contiguous=True)
        wrt = sbt("wrt", [128, 8, 64])
        S.dma("sp", wrt[:], Wd["w_router"].rearrange("(k p) n -> p k n", p=128), writes=[r_c])

        sm = sbt("sm", [128, 64])
        lamt = sbt("lamt", [128, 2, 64])
        tt("dve", lamt[:, 0, :], lamv[:, 0, :], lamv[:, 1, :], ALU.mult, [r_c], [r_c])
        tt("dve", lamt[:, 1, :], lamv[:, 2, :], lamv[:, 3, :], ALU.mult, [r_c], [r_c])
        red(sm[:, 0:2], lamt[:], ALU.add, [r_c], [r_c])
        act(sm[:, 2:4], sm[:, 0:2], AF.Exp, [r_c], [r_c])
        tt("dve", sm[:, 4:5], sm[:, 3:4], sm[:, 2:3], ALU.subtract, [r_c], [r_c])
        ts("dve", sm[:, 5:6], sm[:, 4:5], -0.2, None, ALU.add, None, [r_c], [r_c])
        nlam = sm[:, 5:6]
        aB = sbt("aB", [128, 32])
        act(aB[:], alogB[:], AF.Exp, [r_c], [r_c])
        ts("dve", aB[:], aB[:], -1.0, None, ALU.mult, None, [r_c], [r_c])

        st1 = sbt("st1", [128, 4])
        gsubT = sbt("gsubT", [128, 1])
        sc4 = [sbt("sc4_%d" % d_, [128, 8, 4]) for d_ in range(2)]
        Wr_all = sbt("Wr_all", [128, NT, 64])
        st4 = sbt("st4", [128, 8])
        rt = sbt("rt", [128, 6, 64])
        rt8 = sbt("rt8", [128, 6, 8])
        st6 = sbt("st6", [128, 4])
        dest8u = sbt("dest8u", [128, NT, 8], U32)
        w8 = sbt("w8", [128, NT, 8])
        eb_i = sbt("eb_i", [128, NBLK], U32)
        csum = sbt("csum", [128, 64])
        sp64 = sbt("sp64", [128, 6, 64])
        sp64i = sbt("sp64i", [128, 64], I32)
        d8f = sbt("d8f", [128, 8])
        rem = int(nc.sbuf_bytes_remaining) - 18 * 1024
        arena = Arena(nc, st, rem // 256 * 256)
        a0 = arena.off
        gpostmixB = arena.alloc((D,), F32)
        gpostffnB = arena.alloc((D,), F32)
        S.dma("sp", gpostmixB, Wd["g_post_mix"].partition_broadcast(128), writes=[r_c])
        S.dma("sp", gpostffnB, Wd["g_post_ffn"].partition_broadcast(128), writes=[r_c])
        c3s = arena.alloc((D,), F32)
        mod_rm = arena.alloc((6 * D,), F32)
        bada3 = arena.alloc((6 * D,), F32)
        wab = [arena.alloc((8, 512), F32) for _ in range(2)]
        r_wab = RL(2)
        r_m = Res()
        S.dma("sp", c3s[0:3, :], c3_d, writes=[r_m])
        S.dma("sp", bada3[0:3, :], Wd["b_ada"].partition_broadcast(3), writes=[r_m])
        act(c3s[0:3, :], c3s[0:3, :], AF.Silu, [r_m], [r_m])
        cT = sbt("cT", [128, 8, 3])
        for kc in range(8):
            tr(bank(0)[:, kc * 3:kc * 3 + 3], c3s[0:3, kc * 128:(kc + 1) * 128], ident[0:3, 0:3], [r_m, r_c], [pb[0]])
        cp("dve", cT[:].rearrange("p a b -> p (a b)"), bank(0)[:, 0:24], [pb[0]], [r_m])
        wa_v = Wd["w_ada"].rearrange("(k p) n -> p k n", p=128)
        for nb_ in range(12):
            bi = nb_ % 2
            S.dma("sp", wab[bi], wa_v[:, :, nb_ * 512:(nb_ + 1) * 512], writes=[r_wab[bi]])
            pbk = 2 + bi
            for kc in range(8):
                mm(bank(pbk)[0:3, :], cT[:, kc, :], wab[bi][:, kc, :], kc == 0, kc == 7, [r_m, r_wab[bi]], [pb[pbk]])
            tt("dve", mod_rm[0:3, nb_ * 512:(nb_ + 1) * 512], bank(pbk)[0:3, :], bada3[0:3, nb_ * 512:(nb_ + 1) * 512],
               ALU.add, [pb[pbk], r_m], [r_m])
        modT = sbt("modT", [128, 48, 3])
        for j in range(48):
            tr(bank(0)[:, j * 3:j * 3 + 3], mod_rm[0:3, j * 128:(j + 1) * 128], ident[0:3, 0:3], [r_m, r_c], [pb[0]])
        cp("dve", modT[:].rearrange("p a b -> p (a b)"), bank(0)[:, 0:144], [pb[0]], [r_m])
        AB = sbt("AB", [128, 3, 4, 8])
        for b in range(3):
            ts("dve", AB[:, b, 0, :], modT[:, 8:16, b], 1.0, None, ALU.add, None, [r_m], [r_m])
            tt("dve", AB[:, b, 0, :], AB[:, b, 0, :], gpmT[:], ALU.mult, [r_m, r_c], [r_m])
            cp("dve", AB[:, b, 1, :], modT[:, 0:8, b], [r_m], [r_m])
            ts("dve", AB[:, b, 2, :], modT[:, 32:40, b], 1.0, None, ALU.add, None, [r_m], [r_m])
            tt("dve", AB[:, b, 2, :], AB[:, b, 2, :], gpfT[:], ALU.mult, [r_m, r_c], [r_m])
            cp("dve", AB[:, b, 3, :], modT[:, 24:32, b], [r_m], [r_m])
        G1 = sbt("G1", [128, NB, D])
        G2 = sbt("G2", [128, NB, D])
        for b in range(NB):
            for (G, coff, gB) in ((G1, 2 * D, gpostmixB), (G2, 5 * D, gpostffnB)):
                for hf in range(2):
                    mm(bank(4 + hf)[:, :], sel3[0:3, b, :], mod_rm[0:3, coff + hf * 512:coff + hf * 512 + 512], True, True,
                       [r_m, r_c], [pb[4 + hf]])
                    tt("dve", G[:, b, hf * 512:(hf + 1) * 512], bank(4 + hf)[:, :], gB[:, hf * 512:(hf + 1) * 512], ALU.mult,
                       [pb[4 + hf], r_c], [r_m])
        S.barrier()
        arena.off = a0

        win_v = Wd["w_in"].rearrange("(k p) n -> p k n", p=128)

        def ldw(dst, src, r):
            S.dma("pool", dst, src, writes=[r])

        for b in range(nseq):
            arena.off = a0
            hT = arena.alloc((8, S_LEN), BF16)
            r_hT = RL(NT)
            hcT = arena.alloc((8, L_CTX), BF16)
            r_hcT = RL(2)
            yT = arena.alloc((8, S_LEN), BF16)
            r_yT = RL(NT)
            a1 = arena.off

            xt = [arena.alloc((D,), F32) for _ in range(2)]
            r_xt = RL(2)
            xn = arena.alloc((D,), F32)
            r_xn = Res()
            junk = arena.alloc((D,), F32)
            r_junk = Res()
            r_st1 = Res()

            def norm_to_T(src_ap, bi, dstT, tcol, r_dst, ab_idx, abrow):
                act(junk, src_ap, AF.Square, [r_xt[bi]], [r_junk, r_st1], accum_out=st1[:, 0:1])
                rstd_from_ssq(st1[:, 2:3], st1[:, 0:1], D, st1[:, 1:2], r_st1)
                ts("dve", xn, src_ap, st1[:, 2:3], None, ALU.mult, None, [r_xt[bi], r_st1], [r_xn])
                for kc in range(8):
                    bk = 0 + kc // 4
                    tr(bank(bk)[:, (kc % 4) * 128:(kc % 4) * 128 + 128], xn[:, kc * 128:(kc + 1) * 128], ident[:],
                       [r_xn, r_c], [pb[bk]])
                for kc in range(8):
                    bk = 0 + kc // 4
                    act(dstT[:, kc, tcol:tcol + 128], bank(bk)[:, (kc % 4) * 128:(kc % 4) * 128 + 128], AF.Identity,
                        [pb[bk], r_m], [r_dst], bias=AB[:, abrow, ab_idx + 1, kc:kc + 1], scale=AB[:, abrow, ab_idx, kc:kc + 1])

            tiles = [("c", i) for i in range(2)] + [("x", i) for i in range(NT)]
            for n, (kind, i) in enumerate(tiles):
                bi = n % 2
                src = ctx_d[b, i * 128:(i + 1) * 128, :] if kind == "c" else x_d[b, i * 128:(i + 1) * 128, :]
                S.dma("sp", xt[bi], src, writes=[r_xt[bi]])
                if kind == "c":
                    norm_to_T(xt[bi], bi, hcT, i * 128, r_hcT[i], 0, 2)
                else:
                    norm_to_T(xt[bi], bi, hT, i * 128, r_hT[i], 0, b)
            if dbg and "hT" in dbg and b == 0:
                dtmp = arena.alloc((2, S_LEN), F32)
                r_d = Res()
                for kk in range(4):
                    cp("dve", dtmp, hT[:, kk * 2:kk * 2 + 2, :], r_hT, [r_d])
                    S.dma("sp", dbg_d["hT"][kk * 256:(kk + 1) * 256, :].rearrange("(k p) t -> p k t", p=128), dtmp, reads=[r_d])
            if stop == "p1":
                break
            conv_some(8)
            S.barrier()
            arena.off = a1

            gssdB = arena.alloc((D,), F32)
            r_g3 = Res()
            S.dma("sp", gssdB, Wd["g_ssd_norm"].partition_broadcast(128), writes=[r_g3])
            Wz = arena.alloc((8, 256), BF16)
            r_Wz = Res()
            Wdt = arena.alloc((8, 32), BF16)
            ldw(Wdt, win_v[:, :, 6144:6176], r_g3)
            dt_all = arena.alloc((18, 32), F32)
            dta_all = arena.alloc((18, 32), F32)
            r_dt = Res()
            for tl in range(18):
                bk = 6 + tl % 2
                for kc in range(8):
                    lhs = hcT[:, kc, tl * 128:(tl + 1) * 128] if tl < 2 else hT[:, kc, (tl - 2) * 128:(tl - 1) * 128]
                    rr = [r_hcT[tl]] if tl < 2 else [r_hT[tl - 2]]
                    mm(bank(bk)[:, 0:32], lhs, Wdt[:, kc, :], kc == 0, kc == 7, [r_g3] + rr, [pb[bk]])
                tt("dve", dt_all[:, tl, :], bank(bk)[:, 0:32], dtbB[:, :], ALU.add, [pb[bk], r_c], [r_dt])
            act(dt_all, dt_all, AF.Exp, [r_dt], [r_dt])
            act(dt_all, dt_all, AF.Ln, [r_dt], [r_dt], bias=1.0)
            tt("dve", dta_all, dt_all, aB[:, :].unsqueeze(1).to_broadcast([128, 18, 32]), ALU.mult, [r_dt, r_c], [r_dt])
            NTOK = L_CTX + S_LEN
            Wg4 = [arena.alloc((4, 8, 128), BF16)] * 2
            r_Wg4 = [Res()] * 2
            upad = arena.alloc((NTOK + 4,), F32)
            r_up = Res()
            cacc = PSALL[:, 0:NTOK + 4]
            r_ca = Res()
            xTf = arena.alloc((2, NTOK), BF16)
            BTf = arena.alloc((NTOK,), BF16)
            CTf = arena.alloc((NTOK,), BF16)
            r_xx = Res()
            r_bc = Res()
            x_tok = arena.alloc((18, 256), BF16)
            B_tok = arena.alloc((18, 128), BF16)
            r_tok = Res()
            ydir = [xTf.rearrange("p a b -> p (a b)")[:, 0:16 * 256].rearrange("p (a b) -> p a b", a=16, b=256), arena.alloc((16, 256), BF16)]
            r_yd = [RL(16), RL(16)]
            r_sc4 = RL(2)
            rb_off = arena.off
            Rb = [arena.alloc((4, 128), F32) for _ in range(2)] * 2
            R2b = [arena.alloc((4, 128), F32) for _ in range(2)] * 2
            Eb = [arena.alloc((4, 128), BF16) for _ in range(4)]
            MTb = [arena.alloc((4, 128), BF16) for _ in range(4)]
            xdt = [arena.alloc((4, 64), BF16) for _ in range(4)]
            xdt2 = [arena.alloc((4, 64), BF16) for _ in range(4)]
            r_R, r_R2, r_Eb, r_MT, r_x1_, r_x2 = RL(2) * 2, RL(2) * 2, RL(4), RL(4), RL(4), RL(4)
            ytmp = [arena.alloc((4, 64), F32) for _ in range(2)]
            r_yt = RL(2)
            Sst = [arena.alloc((256,), F32) for _ in range(2)]
            Sbf = [arena.alloc((256,), BF16) for _ in range(2)]
            r_S = RL(2)
            r_Sb = RL(2)
            nac_all = arena.alloc((18, 2, 4), F32)
            ea_all = arena.alloc((18, 2, 4), F32)
            dec_all = arena.alloc((18, 2, 4), F32)
            wst_all = arena.alloc((18, 2, 4), F32)
            pbh = [RL(2) for _ in range(8)]
            _fl = lambda a: a.rearrange("p a b -> p (a b)")
            _sz = 4 * (NTOK + 4)
            assert 4 * 2048 + 4 * 1024 >= _sz
            _o0 = arena.off
            arena.off = rb_off
            upad2 = arena.alloc((NTOK + 4,), F32)
            arena.off = _o0
            upads = [upad, upad2]
            r_ups = [r_up, Res()]
            S.op("pool", lambda e: e.memset(upad2, 0.0), writes=[r_ups[1]])
            fz_zs = [_fl(Rb[0])[:, 0:256], _fl(Rb[1])[:, 0:256], _fl(R2b[0])[:, 0:256], _fl(R2b[1])[:, 0:256]]
            fz_y = [_fl(Rb[0])[:, 256:512], _fl(Rb[1])[:, 256:512], _fl(R2b[0])[:, 256:512], _fl(R2b[1])[:, 256:512]]
            fz_yb = [_fl(Eb[0])[:, 0:256], _fl(Eb[1])[:, 0:256], _fl(Eb[2])[:, 0:256], _fl(Eb[3])[:, 0:256]]
            fz_jk = _fl(MTb[0])[:, 0:256]
            r_fzs = RL(4)
            r_fjk = Res()
            S.op("pool", lambda e: e.memset(upad, 0.0), writes=[r_up])

            def load_g_w(g, slot):
                xo = 4096 + g * 256
                for i, c0 in enumerate((xo, xo + 128, 4096 + 1024 + g * 128, 4096 + 1536 + g * 128)):
                    ldw(Wg4[slot][:, i, :, :], win_v[:, :, c0:c0 + 128], r_Wg4[slot])

            load_g_w(0, 0)
            for g in range(4):
                slot = 0
                Wg = Wg4[slot]
                ccs = (2 * g, 2 * g + 1, 8 + g, 12 + g)
                if g > 0:
                    S.op("pool", lambda e: e.memset(upad2[:, 0:1], 0.0), writes=[r_ups[1]])
                    S.op("pool", lambda e: e.memset(upad2[:, 257:259], 0.0), writes=[r_ups[1]])
                    S.op("pool", lambda e: e.memset(upad2[:, NTOK + 3:NTOK + 4], 0.0), writes=[r_ups[1]])
                for i in range(4):
                    cc = ccs[i]
                    up_ = upads[i % 2]
                    rup_ = r_ups[i % 2]
                    for kc in range(8):
                        mm(bank(6)[:, 0:L_CTX], Wg[:, i, kc, :], hcT[:, kc, :], kc == 0, kc == 7, [r_Wg4[slot]] + r_hcT, [pb[6]])
                    cp("act", up_[:, 1:1 + L_CTX], bank(6)[:, 0:L_CTX], [pb[6]], [rup_])
                    for t4 in range(4):
                        bk = 6 + (t4 + 1) % 2
                        for kc in range(8):
                            mm(bank(bk)[:, :], Wg[:, i, kc, :], hT[:, kc, t4 * 512:(t4 + 1) * 512], kc == 0, kc == 7,
                               [r_Wg4[slot]] + r_hT[t4 * 4:t4 * 4 + 4], [pb[bk]])
                        cp("act", up_[:, 259 + t4 * 512:259 + (t4 + 1) * 512], bank(bk)[:, :], [pb[bk]], [rup_])
                    n_ = NTOK + 2
                    ts("dve", cacc[:, 1:1 + n_], up_[:, 1:1 + n_], cwT[:, cc, 1:2], cbT[:, cc:cc + 1], ALU.mult, ALU.add, [rup_, r_c], [r_ca] + pb[0:5])
                    stt(cacc[:, 1:1 + n_], up_[:, 0:n_], cwT[:, cc, 0:1], cacc[:, 1:1 + n_], ALU.mult, ALU.add, [rup_, r_c, r_ca], [r_ca] + pb[0:5])
                    stt(cacc[:, 1:1 + n_], up_[:, 2:2 + n_], cwT[:, cc, 2:3], cacc[:, 1:1 + n_], ALU.mult, ALU.add, [rup_, r_c, r_ca], [r_ca] + pb[0:5])
                    dst = xTf[:, i, :] if i < 2 else (BTf if i == 2 else CTf)
                    rdst = r_xx if i < 2 else r_bc
                    act(dst[:, 0:L_CTX], cacc[:, 1:1 + L_CTX], AF.Silu, [r_ca] + pb[0:5], [rdst])
                    act(dst[:, L_CTX:NTOK], cacc[:, 259:259 + S_LEN], AF.Silu, [r_ca] + pb[0:5], [rdst])
                if g + 1 < 4:
                    load_g_w(g + 1, 0)
                for tl in range(18):
                    bk = (0, 1, 2, 3, 6, 7)[tl % 6]
                    cs = slice(tl * 128, (tl + 1) * 128)
                    tr(bankb(bk)[:, 0:128], xTf[:, 0, cs], identb[:], [r_xx, r_c], [pb[bk]], inc=False)
                    tr(bankb(bk)[:, 128:256], xTf[:, 1, cs], identb[:], [r_xx, r_c], [pb[bk]], inc=False)
                    tr(bankb(bk)[:, 256:384], BTf[:, cs], identb[:], [r_bc, r_c], [pb[bk]], inc=True)
                    cp("act", x_tok[:, tl, :], bankb(bk)[:, 0:256], [pb[bk]], [r_tok])
                    cp("dve", B_tok[:, tl, :], bankb(bk)[:, 256:384], [pb[bk]], [r_tok])

                ldw(Wz, win_v[:, :, 3 * D + g * 256:3 * D + (g + 1) * 256], r_Wz)
                for d_ in range(2):
                    S.op("pool", lambda e, d_=d_: e.memset(Sst[d_], 0.0), writes=[r_S[d_]])
                    S.op("pool", lambda e, d_=d_: e.memset(Sbf[d_], 0.0), writes=[r_S[d_]])
                dtg = dt_all.rearrange("p t (d h) -> p t d h", d=2, h=16)[:, :, :, g * 4:(g + 1) * 4]
                dtag = dta_all.rearrange("p t (d h) -> p t d h", d=2, h=16)[:, :, :, g * 4:(g + 1) * 4]
                pa = bank(0)[:, 0:144].rearrange("p (t d h) -> p t d h", t=18, d=2, h=4)
                pt_ = bank(1)[:, 0:144].rearrange("p (t d h) -> p t d h", t=18, d=2, h=4)
                for d_ in range(2):
                    tri = cst["trif"] if d_ == 0 else cst["trib"]
                    mm(pa[:, :, d_, :], tri[:], dtag[:, :, d_, :], True, True, [r_c, r_dt], [pb[0]], inc=False)
                    mm(pt_[:, :, d_, :], cst["ones"][:], dtag[:, :, d_, :], True, True, [r_c, r_dt], [pb[1]], inc=(d_ == 1))
                r_sm = Res()
                ts("dve", nac_all, pa, -1.0, None, ALU.mult, None, [pb[0]], [r_sm])
                act(ea_all, pa, AF.Exp, [pb[0]], [r_sm])
                act(dec_all, pt_, AF.Exp, [pb[1]], [r_sm])
                tt("dve", wst_all, pt_, nac_all, ALU.add, [pb[1], r_sm], [r_sm])
                act(wst_all, wst_all, AF.Exp, [r_sm], [r_sm])
                tt("dve", wst_all, wst_all, dtg, ALU.mult, [r_sm, r_dt], [r_sm])
                order = [[0, 1] + list(range(2, 18)), [1, 0] + list(range(17, 1, -1))]
                S.barrier()

                def pre(step, d_):
                    tl = order[d_][step]
                    lat = tl >= 2
                    par = step % 2
                    k_ = d_ * 2 + par
                    cs = slice(tl * 128, (tl + 1) * 128)
                    tri = cst["trif"] if d_ == 0 else cst["trib"]
                    neg = cst["negf"] if d_ == 0 else cst["negb"]
                    bA = 2 * k_
                    bB = 2 * k_ + 1
                    xv = x_tok[:, tl, :].rearrange("p (a b) -> p a b", a=4, b=64)
                    tt("dve", xdt2[k_], xv, wst_all[:, tl, d_, :].unsqueeze(2).to_broadcast([128, 4, 64]), ALU.mult, [r_tok, r_sm], [r_x2[k_]])
                    yield
                    mm(bank(bB)[:, 256:512], B_tok[:, tl, :], xdt2[k_].rearrange("p a b -> p (a b)"), True, True, [r_tok, r_x2[k_]], [pbh[bB][1]])
                    yield
                    if lat:
                        tt("pool", Rb[d_], tri[:, :].unsqueeze(1).to_broadcast([128, 4, 128]),
                           dtag[:, tl, d_, :].unsqueeze(2).to_broadcast([128, 4, 128]), ALU.mult, [r_c, r_dt], [r_R[d_]])
                        yield
                        tt("pool", R2b[d_], neg[:, :].unsqueeze(1).to_broadcast([128, 4, 128]),
                           nac_all[:, tl, d_, :].unsqueeze(2).to_broadcast([128, 4, 128]), ALU.add, [r_c, r_sm], [r_R2[d_]])
                        yield
                        mm(bank(bA)[:, :], cst["ones"][:], Rb[d_].rearrange("p a b -> p (a b)"), True, False, [r_c, r_R[d_]], pbh[bA], inc=False)
                        mm(bank(bA)[:, :], ident[:], R2b[d_].rearrange("p a b -> p (a b)"), False, True, [r_c, r_R2[d_]], pbh[bA])
                        yield
                        mm(bank(bB)[:, 0:128], BTf[:, cs], CTf[:, cs], True, True, [r_bc], [pbh[bB][0]])
                        yield
                        act(Eb[k_].rearrange("p a b -> p (a b)"), bank(bA)[:, :], AF.Exp, pbh[bA], [r_Eb[k_]])
                        yield
                        tt("dve", MTb[k_], Eb[k_], bank(bB)[:, 0:128].unsqueeze(1).to_broadcast([128, 4, 128]), ALU.mult, [r_Eb[k_], pbh[bB][0]], [r_MT[k_]])
                        yield
                        tt("dve", xdt[k_], xv, dtg[:, tl, d_, :].unsqueeze(2).to_broadcast([128, 4, 64]), ALU.mult, [r_tok, r_dt], [r_x1_[k_]])
                        yield
                        for h in range(4):
                            mm(bank(bA)[:, h * 64:(h + 1) * 64], MTb[k_][:, h, :], xdt[k_][:, h, :], True, True, [r_MT[k_], r_x1_[k_]], [pbh[bA][0]], inc=(h == 3))
                        yield

                def dep(step, d_):
                    tl = order[d_][step]
                    lat = tl >= 2
                    par = step % 2
                    k_ = d_ * 2 + par
                    cs = slice(tl * 128, (tl + 1) * 128)
                    bA = 2 * k_
                    bB = 2 * k_ + 1
                    if lat:
                        t = tl - 2
                        mm(bank(bA)[:, 256:512], CTf[:, cs], Sbf[d_], True, True, [r_bc, r_Sb[d_]], [pbh[bA][1]])
                        yield
                    Sv = Sst[d_].rearrange("p (a b) -> p a b", a=4, b=64)
                    tt("dve", Sv, Sv, dec_all[:, tl, d_, :].unsqueeze(2).to_broadcast([128, 4, 64]), ALU.mult, [r_S[d_], r_sm], [r_S[d_]])
                    yield
                    tt("dve", Sst[d_], Sst[d_], bank(bB)[:, 256:512], ALU.add, [r_S[d_], pbh[bB][1]], [r_S[d_]])
                    yield
                    cp("act", Sbf[d_], Sst[d_], [r_S[d_]], [r_Sb[d_]])
                    yield
                    if lat:
                        tt("dve", ytmp[d_], bank(bA)[:, 256:512].rearrange("p (a b) -> p a b", a=4, b=64),
                           ea_all[:, tl, d_, :].unsqueeze(2).to_broadcast([128, 4, 64]), ALU.mult, [pbh[bA][1], r_sm], [r_yt[d_]])
                        yield
                        tt("dve", ydir[d_][:, t, :], ytmp[d_].rearrange("p a b -> p (a b)"), bank(bA)[:, 0:256], ALU.add,
                           [r_yt[d_], pbh[bA][0]], [r_yd[d_][t]] + ([r_xx] if d_ == 0 else []))
                        yield

                def rr(gens):
                    while gens:
                        nxt = []
                        for g_ in gens:
                            try:
                                next(g_)
                                nxt.append(g_)
                            except StopIteration:
                                pass
                        gens = nxt

                rr([pre(0, 0), pre(0, 1)])
                for step in range(18):
                    gl = [dep(step, 0), dep(step, 1)]
                    if step + 1 < 18:
                        gl = [pre(step + 1, 0), pre(step + 1, 1)] + gl
                    rr(gl)
                    conv_some(1)
                S.barrier()
                pass
                def fin(t):
                    i = t % 4
                    cs = slice(t * 128, (t + 1) * 128)
                    zs_, yf_, ybf_, jk_ = fz_zs[i], fz_y[i], fz_yb[i], fz_jk
                    rz = r_fzs[i]
                    scl = sc4[0][:, 4 + i, :]
                    for kc in range(8):
                        mm(bank(i)[:, 0:256], hT[:, kc, cs], Wz[:, kc, :], kc == 0, kc == 7, [r_Wz, r_hT[t]], [pb[i]])
                    yield
                    act(zs_, bank(i)[:, 0:256], AF.Silu, [pb[i]], [rz])
                    yield
                    xv = x_tok[:, t + 2, :].rearrange("p (a b) -> p a b", a=4, b=64)
                    tt("pool", yf_.rearrange("p (a b) -> p a b", a=4, b=64), xv, dskB[:, g * 4:(g + 1) * 4].unsqueeze(2).to_broadcast([128, 4, 64]),
                       ALU.mult, [r_tok, r_c], [rz])
                    yield
                    tt("dve", yf_, yf_, ydir[0][:, t, :], ALU.add, [rz, r_yd[0][t], r_xx], [rz])
                    yield
                    tt("dve", yf_, yf_, ydir[1][:, t, :], ALU.add, [rz, r_yd[1][t]], [rz])
                    yield
                    tt("dve", yf_, yf_, zs_, ALU.mult, [rz], [rz])
                    yield
                    act(jk_, yf_, AF.Square, [rz], [r_fjk, rz], accum_out=scl[:, 0:1])
                    yield
                    ts("dve", scl[:, 1:2], scl[:, 0:1], 1.0 / 256, 1e-6, ALU.mult, ALU.add, [rz], [rz])
                    yield
                    act(scl[:, 1:2], scl[:, 1:2], AF.Ln, [rz], [rz])
                    yield
                    act(scl[:, 2:3], scl[:, 1:2], AF.Exp, [rz], [rz], scale=-0.5)
                    yield
                    stt(ybf_, yf_, scl[:, 2:3], gssdB[:, g * 256:(g + 1) * 256], ALU.mult, ALU.mult, [rz, r_g3], [rz])
                    yield
                    tr(bankb(4 + i)[:, 0:128], ybf_[:, 0:128], identb[:], [rz, r_c], [pb[4 + i]], inc=False)
                    tr(bankb(4 + i)[:, 128:256], ybf_[:, 128:256], identb[:], [rz, r_c], [pb[4 + i]], inc=True)
                    yield
                    cp("act", yT[:, 2 * g:2 * g + 2, cs], bankb(4 + i)[:, 0:256].rearrange("p (a b) -> p a b", a=2, b=128), [pb[4 + i]], [r_yT[t]])
                    yield

                active = []
                nxt_ = 0
                while nxt_ < NT or active:
                    if len(active) < 4 and nxt_ < NT:
                        active.append(fin(nxt_))
                        nxt_ += 1
                    new_ = []
                    for g_ in active:
                        try:
                            next(g_)
                            new_.append(g_)
                        except StopIteration:
                            pass
                    active = new_
                S.barrier()
            if dbg and "yT" in dbg and b == 0:
                S.barrier()
                arena.off = a1
                dtmp = arena.alloc((2, S_LEN), F32)
                r_d = Res()
                for kk in range(4):
                    cp("dve", dtmp, yT[:, kk * 2:kk * 2 + 2, :], r_yT, [r_d])
                    S.dma("sp", dbg_d["yT"][kk * 256:(kk + 1) * 256, :].rearrange("(k p) t -> p k t", p=128), dtmp, reads=[r_d])
            if stop == "p3":
                break
            S.barrier()
            arena.off = a1
            oaT = arena.alloc((8, S_LEN), BF16)
            r_oaT = RL(4)
            a2 = arena.off
            cosT = arena.alloc((S_LEN,), F32)
            sinT = arena.alloc((S_LEN,), F32)
            r_cs = Res()
            S.dma("sp", cosT, Cd["cosT"], writes=[r_cs])
            S.dma("sp", sinT, Cd["sinT"], writes=[r_cs])
            wq = [arena.alloc((3, 8, 128), BF16) for _ in range(2)]
            qpre = arena.alloc((512,), BF16)
            r_qpre = Res()
            tmpk1 = arena.alloc((512,), F32)
            tmpk2 = arena.alloc((512,), F32)
            qprek = arena.alloc((512,), BF16)
            r_tk = RL(3)
            r_wq = RL(2)
            qT = arena.alloc((2, S_LEN), BF16)
            r_qT = RL(4)
            S.op("pool", lambda e: e.memset(qT, 0.0), writes=r_qT)
            kT = arena.alloc((L_CTX + S_LEN,), BF16)
            r_kT = RL(5)
            va = arena.alloc((18, 128), BF16)
            r_va = Res()
            onesb = arena.alloc((128,), BF16)
            cp("dve", onesb, cst["ones"][:], [r_c], [r_va])
            Et = [arena.alloc((512,), BF16) for _ in range(3)]
            r_E = RL(3)
            fo = arena.alloc((512,), F32)
            ft = arena.alloc((512,), F32)
            fr = arena.alloc((512,), F32)
            ftb = arena.alloc((512,), BF16)
            r_f = Res()
            tmp1, tmp2 = fr, ft
            r_tmp = [r_f, r_f]
            S.dma("sp", gsubT[:], Wd["g_attn_subln"].rearrange("(p o) -> p o", o=1), writes=[r_f])
            ts("dve", gsubT[:], gsubT[:], 0.8, None, ALU.mult, None, [r_f], [r_f])

            wstg = [arena.alloc((8, 128), F32)] * 2
            r_wstg = [Res()] * 2
            stg_i = [0]

            def load_head_w(hd, slot):
                c0 = hd * 128
                for (wi, base) in ((0, c0), (1, D + c0), (2, 2 * D + c0)):
                    si = stg_i[0] % 2
                    stg_i[0] += 1
                    S.dma("sp", wstg[si], win_v[:, :, base:base + 128], writes=[r_wstg[si]])
                    cp("pool", wq[slot][:, wi, :, :], wstg[si], [r_wstg[si]], [r_wq[slot]])

            load_head_w(0, 0)
            for hd in range(8):
                slot = hd % 2
                if hd + 1 < 8:
                    load_head_w(hd + 1, 1 - slot)
                W = wq[slot]
                rW = r_wq[slot]
                def gen_qk(is_q):
                    wi = 0 if is_q else 1
                    bA, bB = (6, 7) if is_q else (0, 1)
                    t1_, t2_, qp_ = (tmp1, tmp2, qpre) if is_q else (tmpk1, tmpk2, qprek)
                    rt1, rt2, rqp = (r_tmp[0], r_tmp[1], r_qpre) if is_q else (r_tk[0], r_tk[1], r_tk[2])
                    if not is_q:
                        for kc in range(8):
                            mm(bank(bA)[:, 0:L_CTX], W[:, 1, kc, :], hcT[:, kc, :], kc == 0, kc == 7, [rW] + r_hcT, [pb[bA]])
                        yield
                        cp("act", kT[:, 0:L_CTX], bank(bA)[:, 0:L_CTX], [pb[bA]], [r_kT[0]])
                        yield
                    for t4 in range(4):
                        cols = slice(t4 * 512, (t4 + 1) * 512)
                        for kc in range(8):
                            mm(bank(bA)[:, :], W[:, wi, kc, :], hT[:, kc, cols], kc == 0, kc == 7, [rW] + r_hT[t4 * 4:t4 * 4 + 4], [pb[bA]])
                        yield
                        cp("dve", qp_, bank(bA)[:, :], [pb[bA]], [rqp])
                        yield
                        mm(bank(bB)[:, :], pswapb[:], qp_, True, True, [r_c, rqp], [pb[bB]])
                        yield
                        tt("dve", t1_, bank(bA)[:, :], cosT[:, cols], ALU.mult, [pb[bA], r_cs], [rt1])
                        yield
                        tt("dve", t2_, bank(bB)[:, :], sinT[:, cols], ALU.mult, [pb[bB], r_cs], [rt2])
                        yield
                        if is_q:
                            tt("pool", qT[0:64, 0, cols], t1_[0:64, :], t2_[0:64, :], ALU.add, [rt1, rt2], [r_qT[t4]])
                            tt("dve", qT[64:128, 1, cols], t1_[64:128, :], t2_[64:128, :], ALU.add, [rt1, rt2], [r_qT[t4]])
                        else:
                            tt("pool", kT[:, L_CTX + t4 * 512:L_CTX + (t4 + 1) * 512], t1_, t2_, ALU.add, [rt1, rt2], [r_kT[1 + t4]])
                        yield

                def gen_v():
                    for vt in range(18):
                        bk = 2 + (vt % 4)
                        for kc in range(8):
                            lhs = hcT[:, kc, vt * 128:(vt + 1) * 128] if vt < 2 else hT[:, kc, (vt - 2) * 128:(vt - 1) * 128]
                            rr_ = [r_hcT[vt]] if vt < 2 else [r_hT[vt - 2]]
                            mm(bank(bk)[:, 0:128], lhs, W[:, 2, kc, :], kc == 0, kc == 7, [rW] + rr_, [pb[bk]])
                        yield
                        cp("act", va[:, vt, :], bank(bk)[:, 0:128], [pb[bk]], [r_va])
                        yield

                gens_ = [gen_qk(True), gen_qk(False), gen_v()]
                while gens_:
                    nx_ = []
                    for g_ in gens_:
                        try:
                            next(g_)
                            nx_.append(g_)
                        except StopIteration:
                            pass
                    gens_ = nx_
                SB_ = (0, 1, 7)
                LA = 2
                its = [(qt, c_, kc) for qt in range(4) for c_ in range(2) for kc in range(18)]

                def emit_S(i):
                    qt, c_, kc = its[i]
                    prt = slice(c_ * 64, c_ * 64 + 64)
                    sb_ = SB_[i % 3]
                    kr = [r_kT[0]] if kc < 2 else [r_kT[1 + (kc - 2) // 4]]
                    mm(bank(sb_)[:, :], kT[:, kc * 128:(kc + 1) * 128], qT[:, c_, qt * 512:(qt + 1) * 512], True, True,
                       kr + [r_qT[qt]], [pb[sb_]])
                    act(Et[i % 3], bank(sb_)[:, :], AF.Exp, [pb[sb_]], [r_E[i % 3]], scale=0.125)

                pending = []
                for i in range(LA):
                    emit_S(i)
                for i in range(len(its)):
                    if i + LA < len(its):
                        emit_S(i + LA)
                    qt, c_, kc = its[i]
                    qcols = slice(qt * 512, (qt + 1) * 512)
                    bo = 2 + 2 * c_
                    E = Et[i % 3]
                    rE = r_E[i % 3]
                    mm(bank(bo)[:, :], va[:, kc, :], E, kc == 0, kc == 17, [rE, r_va], [pb[bo]])
                    mm(bank(bo + 1)[:, :], onesb, E, kc == 0, kc == 17, [rE, r_va], [pb[bo + 1]])
                    if kc == 8:
                        conv_some(1)
                    if kc == 17 and c_ == 0:
                        recip(fr, bank(3)[:, :], [pb[3]], [r_f])
                        tt("dve", fo, bank(2)[:, :], fr, ALU.mult, [pb[2], r_f], [r_f])
                    if kc == 17 and c_ == 1:
                        recip(fr, bank(5)[:, :], [pb[5]], [r_f])
                        tt("dve", ft, bank(4)[:, :], fr, ALU.mult, [pb[4], r_f], [r_f])
                        stt(fo, ft, nlam, fo, ALU.mult, ALU.add, [r_f, r_c], [r_f])
                        tt("pool", ftb, fo, fo, ALU.mult, [r_f], [r_f])

                        def partB(qcols=qcols, qt=qt):
                            mm(bank(6)[:, :], onesb, ftb, True, True, [r_f, r_va], [pb[6]])
                            ts("dve", fr, bank(6)[:, :], 1.0 / 128, 1e-6, ALU.mult, ALU.add, [pb[6]], [r_f])
                            act(fr, fr, AF.Ln, [r_f], [r_f])
                            act(fr, fr, AF.Exp, [r_f], [r_f], scale=-0.5)
                            tt("dve", fo, fo, fr, ALU.mult, [r_f], [r_f])
                            ts("dve", oaT[:, hd, qcols], fo, gsubT[:, 0:1], None, ALU.mult, None, [r_f], [r_oaT[qt]])
                        pending.append((i + 8, partB))
                    while pending and (pending[0][0] <= i or i == len(its) - 1):
                        pending.pop(0)[1]()
            if dbg and "oaT" in dbg and b == 0:
                S.barrier()
                arena.off = a2
                dtmp = arena.alloc((2, S_LEN), F32)
                r_d = Res()
                for kk in range(4):
                    cp("dve", dtmp, oaT[:, kk * 2:kk * 2 + 2, :], r_oaT, [r_d])
                    S.dma("sp", dbg_d["oaT"][kk * 256:(kk + 1) * 256, :].rearrange("(k p) t -> p k t", p=128), dtmp, reads=[r_d])
            if stop == "p2":
                break
            S.barrier()
            arena.off = a2


            mT = arena.alloc((8, S_LEN), BF16)
            r_mT = RL(4)
            a3 = arena.off
            Wm = [arena.alloc((4, 8, 128), BF16) for _ in range(2)]
            r_Wm = RL(2)
            sg = [arena.alloc((512,), F32) for _ in range(2)]
            t12 = [arena.alloc((512,), F32) for _ in range(2)]
            r_sg = RL(2)
            r_t12 = RL(2)
            wba_v = Wd["w_branch_attn"].rearrange("(k p) n -> p k n", p=128)
            wbs_v = Wd["w_branch_ssd"].rearrange("(k p) n -> p k n", p=128)
            wout_v = Wd["w_out"].rearrange("(k p) n -> p k n", p=128)

            mstg = [arena.alloc((8, 128), F32) for _ in range(2)]
            r_mstg = RL(2)
            mstg_i = [0]

            def load_m_w(m, slot):
                cs_ = slice(m * 128, (m + 1) * 128)
                srcs_ = (wba_v[:, :, cs_], wbs_v[:, :, cs_], win_v[:, :, 6176 + m * 128:6176 + (m + 1) * 128],
                         win_v[:, :, 6176 + D + m * 128:6176 + D + (m + 1) * 128])
                for i_, src_ in enumerate(srcs_):
                    si = mstg_i[0] % 2
                    mstg_i[0] += 1
                    S.dma("sp", mstg[si], src_, writes=[r_mstg[si]])
                    cp("act" if i_ % 2 == 0 else "dve", Wm[slot][:, i_, :, :], mstg[si], [r_mstg[si]], [r_Wm[slot]])

            load_m_w(0, 0)
            it = 0
            for m in range(8):
                slot = m % 2
                if m + 1 < 8:
                    load_m_w(m + 1, 1 - slot)
                for t4 in range(4):
                    cols = slice(t4 * 512, (t4 + 1) * 512)
                    bb = 4 * (it % 2)
                    it += 1
                    srcs = ((oaT, [r_oaT[t4]]), (yT, r_yT[t4 * 4:t4 * 4 + 4]), (hT, r_hT[t4 * 4:t4 * 4 + 4]), (hT, r_hT[t4 * 4:t4 * 4 + 4]))
                    for i in range(4):
                        for kc in range(8):
                            mm(bank(bb + i)[:, :], Wm[slot][:, i, kc, :], srcs[i][0][:, kc, cols], kc == 0, kc == 7,
                               [r_Wm[slot]] + srcs[i][1], [pb[bb + i]])
                    for i in range(2):
                        act(sg[i], bank(bb + 2 + i)[:, :], AF.Sigmoid, [pb[bb + 2 + i]], [r_sg[i]])
                        tt("dve", t12[i], bank(bb + i)[:, :], sg[i], ALU.mult, [pb[bb + i], r_sg[i]], [r_t12[i]])
                    tt("dve", mT[:, m, cols], t12[0], t12[1], ALU.add, r_t12, [r_mT[t4]])
                    conv_some(2)
            S.barrier()
            arena.off = a1
            Wout = arena.alloc((8, D), BF16)
            r_Wout = Res()
            for hf in range(2):
                ldw(Wout[:, :, hf * 512:(hf + 1) * 512], wout_v[:, :, hf * 512:(hf + 1) * 512], r_Wout)
            xt4 = [arena.alloc((D,), F32) for _ in range(2)]
            r_xt4 = RL(2)
            x1t = [arena.alloc((D,), F32) for _ in range(2)]
            r_x1t = RL(2)
            assert arena.off <= a2
            arena.off = a3
            tmpfs = [arena.alloc((D,), F32) for _ in range(2)]
            r_tmpfs = RL(2)
            h2fs = [arena.alloc((8, 128), F32) for _ in range(2)]
            r_h2fs = RL(2)
            st4b = arena.alloc((8,), F32)
            st4s = [st4, st4b]
            r_st4s = RL(2)
            r_Wr = RL(NT)
            r_st4 = Res()
            r_rt = Res()
            r_x1 = RL(NT)
            if SPARSE:
                h2tfs = [arena.alloc((D,), F32)] * 2
                r_h2tfs = [Res()] * 2
                A2R = arena.alloc((D,), F32)
                B2R = arena.alloc((D,), F32)
                r_AR = Res()
                diag = arena.alloc((128,), F32)
                r_diag = Res()
                rank_all = arena.alloc((NT, 64), F32)
                r_rank = Res()
                maskb = arena.alloc((64,), BF16)
                r_mk = Res()
                a4 = arena.off
                arena.off = a1 - 8 * S_LEN * 2
                h2tok = arena.alloc((NT, D), BF16)
                arena.off = a4
                r_h2tok = RL(NT)
                for (dstR, idx_) in ((A2R, 2), (B2R, 3)):
                    for kc in range(8):
                        ts("dve", diag, ident[:], AB[:, b, idx_, kc:kc + 1], None, ALU.mult, None, [r_c, r_m], [r_diag])
                        bk = 6 + kc // 4
                        mm(bank(bk)[:, (kc % 4) * 128:(kc % 4) * 128 + 128], cst["ones"][:], diag, True, True, [r_c, r_diag], [pb[bk]])
                    cp("dve", dstR[:, 0:512], bank(6)[:, :], [pb[6]], [r_AR])
                    cp("act", dstR[:, 512:1024], bank(7)[:, :], [pb[7]], [r_AR])
                S.op("pool", lambda e: e.memset(csum[:], 0.0), writes=[r_rank])
            A2v = AB[:, b, 2, :]
            B2v = AB[:, b, 3, :]
            def partA(t):
                bi = t % 2
                sl = t % 2
                bo_ = 0 if sl == 0 else 4
                rb_ = bo_
                tmpf, h2f, h2tf = tmpfs[sl], h2fs[sl], h2tfs[sl]
                r_tmpf, r_h2f, r_h2tf = r_tmpfs[sl], r_h2fs[sl], r_h2tfs[sl]
                st4 = st4s[sl]
                r_st4 = r_st4s[sl]
                cs = slice(t * 128, (t + 1) * 128)
                S.dma("sp", xt4[bi], x_d[b, cs, :], writes=[r_xt4[bi]])
                for hf in range(2):
                    for kc in range(8):
                        mm(bank(bo_ + hf)[:, :], mT[:, kc, cs], Wout[:, kc, hf * 512:(hf + 1) * 512], kc == 0, kc == 7,
                           [r_mT[t // 4], r_Wout], [pb[bo_ + hf]])
                yield
                for hf in range(2):
                    act(tmpf[:, hf * 512:(hf + 1) * 512], bank(bo_ + hf)[:, :], AF.Square, [pb[bo_ + hf]], [r_tmpf, r_st4], accum_out=st4[:, hf:hf + 1])
                yield
                tt("dve", st4[:, 2:3], st4[:, 0:1], st4[:, 1:2], ALU.add, [r_st4], [r_st4])
                yield
                ts("dve", st4[:, 3:4], st4[:, 2:3], 1.0 / D, 1e-6, ALU.mult, ALU.add, [r_st4], [r_st4])
                yield
                act(st4[:, 3:4], st4[:, 3:4], AF.Ln, [r_st4], [r_st4])
                yield
                act(st4[:, 4:5], st4[:, 3:4], AF.Exp, [r_st4], [r_st4], scale=-0.5)
                yield
                for hf in range(2):
                    hs_ = slice(hf * 512, (hf + 1) * 512)
                    stt(tmpf[:, hs_], bank(bo_ + hf)[:, :], st4[:, 4:5], G1[:, b, hs_], ALU.mult, ALU.mult, [pb[bo_ + hf], r_st4, r_m], [r_tmpf])
                    yield
                tt("dve", x1t[bi], tmpf, xt4[bi], ALU.add, [r_tmpf, r_xt4[bi]], [r_x1t[bi]])
                yield
                S.dma("sp", out_d[b, cs, :], x1t[bi], reads=[r_x1t[bi]], writes=[r_x1[t]])
                act(tmpf, x1t[bi], AF.Square, [r_x1t[bi]], [r_tmpf, r_st4], accum_out=st4[:, 5:6])
                yield
                ts("dve", st4[:, 6:7], st4[:, 5:6], 1.0 / D, 1e-6, ALU.mult, ALU.add, [r_st4], [r_st4])
                yield
                act(st4[:, 6:7], st4[:, 6:7], AF.Ln, [r_st4], [r_st4])
                yield
                act(st4[:, 7:8], st4[:, 6:7], AF.Exp, [r_st4], [r_st4], scale=-0.5)
                yield
                ts("dve", tmpf, x1t[bi], st4[:, 7:8], None, ALU.mult, None, [r_x1t[bi], r_st4], [r_tmpf])
                yield
                if SPARSE:
                    tt("dve", h2tf, tmpf, A2R, ALU.mult, [r_tmpf, r_AR], [r_h2tf])
                    tt("pool", h2tok[:, t, :], h2tf, B2R, ALU.add, [r_h2tf, r_AR], [r_h2tok[t]])
                    yield
                for kc in range(8):
                    bk = bo_ + 2 + kc // 4
                    tr(bank(bk)[:, (kc % 4) * 128:(kc % 4) * 128 + 128], tmpf[:, kc * 128:(kc + 1) * 128], ident[:],
                       [r_tmpf, r_c], [pb[bk]], inc=(kc % 4 == 3))
                yield
                pv = PS[bo_ // 2 + 1][:, :].rearrange("p (a b) -> p a b", a=8, b=128)
                tt("dve", h2f, pv, A2v.unsqueeze(2).to_broadcast([128, 8, 128]), ALU.mult, [pb[bo_ + 2], pb[bo_ + 3], r_m], [r_h2f])
                yield
                tt("dve", h2f, h2f, B2v.unsqueeze(2).to_broadcast([128, 8, 128]), ALU.add, [r_h2f, r_m], [r_h2f])
                yield
                cp("act", hT[:, :, cs], h2f, [r_h2f], [r_hT[t]])
                for kc in range(8):
                    mm(bank(rb_)[:, 0:64], h2f[:, kc, :], wrt[:, kc, :], kc == 0, kc == 7, [r_h2f, r_c], [pb[rb_]])
                yield

            def partB(t):
                rb_ = 0 if t % 2 == 0 else 4
                kb_ = 1 if t % 2 == 0 else 5
                sc_ = rt[:, 0, :]
                sel_ = rt[:, 1, :]
                act(sc_, bank(rb_)[:, 0:64], AF.Exp, [pb[rb_]], [r_rt], scale=-1.0)
                ts("dve", sc_, sc_, 1.0, None, ALU.add, None, [r_rt], [r_rt])
                recip(sc_, sc_, [r_rt], [r_rt])
                tt("dve", sel_, sc_, rbiasB[:, :], ALU.add, [r_rt, r_c], [r_rt])
                sel3v = sel_.rearrange("p (a b) -> p a b", a=8, b=8)
                red(rt8[:, 0, :], sel3v, ALU.max, [r_rt], [r_rt])
                tt("dve", rt[:, 2, :].rearrange("p (a b) -> p a b", a=8, b=8), sel3v,
                   rt8[:, 0, :].unsqueeze(2).to_broadcast([128, 8, 8]), ALU.is_equal, [r_rt], [r_rt])
                stt(rt[:, 2, :], rt[:, 2, :], -BIG, sel_, ALU.mult, ALU.add, [r_rt], [r_rt])
                red(rt8[:, 1, :], rt[:, 2, :].rearrange("p (a b) -> p a b", a=8, b=8), ALU.max, [r_rt], [r_rt])
                tt("dve", rt8[:, 2, :], rt8[:, 0, :], rt8[:, 1, :], ALU.add, [r_rt], [r_rt])
                S.op("dve", lambda e: e.max(rt8[:, 3, :], rt8[:, 2, :]), reads=[r_rt], writes=[r_rt])
                ts("dve", rt8[:, 4, :], rt8[:, 2, :], rt8[:, 3, 3:4], None, ALU.is_ge, None, [r_rt], [r_rt])
                ts("dve", rt8[:, 4, :], rt8[:, 4, :], -1.0, BIG, ALU.add, ALU.mult, [r_rt], [r_rt])
                tt("dve", rt[:, 3, :].rearrange("p (a b) -> p a b", a=8, b=8), sel3v,
                   rt8[:, 4, :].unsqueeze(2).to_broadcast([128, 8, 8]), ALU.add, [r_rt], [r_rt])
                S.op("dve", lambda e: e.max(rt8[:, 5, :], rt[:, 3, :]), reads=[r_rt], writes=[r_rt])
                ts("dve", rt[:, 4, :], rt[:, 3, :], rt8[:, 5, 7:8], None, ALU.is_ge, None, [r_rt], [r_rt])
                tt("dve", rt[:, 4, :], rt[:, 4, :], sc_, ALU.mult, [r_rt], [r_rt])
                red(sm[:, 8:9], rt[:, 4, :], ALU.add, [r_rt], [r_rt])
                recip(sm[:, 9:10], sm[:, 8:9], [r_rt], [r_rt])
                ts("dve", Wr_all[:, t, :], rt[:, 4, :], sm[:, 9:10], 2.5, ALU.mult, ALU.mult, [r_rt], [r_Wr[t]])
                if SPARSE:
                    ts("dve", maskb, Wr_all[:, t, :], 0.0, None, ALU.is_gt, None, [r_Wr[t]], [r_mk])
                    mm(bank(kb_)[:, 0:64], trisb[:], maskb, True, True, [r_c, r_mk], [pb[kb_]], inc=False)
                    mm(bank(kb_)[:, 64:128], onesbp[:], maskb, True, True, [r_c, r_mk], [pb[kb_]])
                    tt("dve", rank_all[:, t, :], bank(kb_)[:, 0:64], csum[:], ALU.add, [pb[kb_], r_rank], [r_rank])
                    tt("dve", csum[:], csum[:], bank(kb_)[:, 64:128], ALU.add, [pb[kb_], r_rank], [r_rank])
            act_ = []
            nt_ = 0
            while nt_ < NT or act_:
                while len(act_) < 2 and nt_ < NT:
                    act_.append((nt_, partA(nt_)))
                    nt_ += 1
                nw_ = []
                for (t_, g_) in act_:
                    try:
                        next(g_)
                        nw_.append((t_, g_))
                    except StopIteration:
                        partB(t_)
                act_ = nw_
            h2T = hT
            r_h2T = r_hT
            if dbg and "Wr" in dbg and b == 0:
                S.dma("sp", dbg_d["Wr"].rearrange("(t p) e -> p t e", p=128), Wr_all[:], reads=r_Wr)
            if dbg and "h2T" in dbg and b == 0:
                S.barrier()
                arena.off = a1
                dtmp = arena.alloc((2, S_LEN), F32)
                r_d = Res()
                for kk in range(4):
                    cp("dve", dtmp, h2T[:, kk * 2:kk * 2 + 2, :], r_h2T, [r_d])
                    S.dma("sp", dbg_d["h2T"][kk * 256:(kk + 1) * 256, :].rearrange("(k p) t -> p k t", p=128), dtmp, reads=[r_d])
            if stop == "p4":
                break
            if SPARSE:
                r_sp = Res()
                r_d8 = Res()
                r_w8 = Res()
                r_xbuf = Res()
                r_ybuf = Res()
                r_eb = Res()
                ones64 = cst["ones"][:, 0:64]
                S.barrier()
                arena.off = a1
                ts("dve", sp64[:, 0, :], csum[:], 255.0, None, ALU.add, None, [r_rank], [r_sp])
                cp("dve", sp64i[:], sp64[:, 0, :], [r_sp], [r_sp])
                ts("dve", sp64i[:], sp64i[:], 8, None, ALU.arith_shift_right, None, [r_sp], [r_sp])
                ts("dve", sp64i[:], sp64i[:], 8, None, ALU.logical_shift_left, None, [r_sp], [r_sp])
                cp("dve", sp64[:, 1, :], sp64i[:], [r_sp], [r_sp])
                S.op("dve", lambda e: e.tensor_tensor_scan(sp64[:, 2, :], ones64, sp64[:, 1, :], 0.0, ALU.mult, ALU.add),
                     reads=[r_sp, r_c], writes=[r_sp])
                tt("dve", sp64[:, 3, :], sp64[:, 2, :], sp64[:, 1, :], ALU.subtract, [r_sp], [r_sp])
                cmpb = arena.alloc((32, 64), F32)
                ebf = arena.alloc((NBLK,), F32)
                for ch in range(NBLK // 32):
                    tt("dve", cmpb, sp64[:, 2, :].unsqueeze(1).to_broadcast([128, 32, 64]),
                       cst["jtab"][:, ch * 32:(ch + 1) * 32].unsqueeze(2).to_broadcast([128, 32, 64]), ALU.is_le, [r_sp, r_c], [r_sp])
                    red(ebf[:, ch * 32:(ch + 1) * 32], cmpb, ALU.add, [r_sp], [r_sp])
                ts("dve", ebf, ebf, 63.0, None, ALU.min, None, [r_sp], [r_sp])
                ts("dve", ebf, ebf, 128.0, cst["pidx"][:, 0:1], ALU.mult, ALU.add, [r_sp, r_c], [r_sp])
                cp("dve", eb_i[:], ebf, [r_sp], [r_eb])
                dm = arena.alloc((NT, 64), F32)
                mk = arena.alloc((NT, 64), F32)
                d8a = arena.alloc((NT, 8), F32)
                tt("dve", dm, rank_all, sp64[:, 3, :].unsqueeze(1).to_broadcast([128, NT, 64]), ALU.add, [r_rank, r_sp], [r_sp])
                ts("dve", mk, Wr_all[:], 0.0, None, ALU.is_gt, None, r_Wr, [r_sp])
                stt(dm.rearrange("p a b -> p (a b)"), dm.rearrange("p a b -> p (a b)"), 1.0, mk.rearrange("p a b -> p (a b)"), ALU.add, ALU.mult, [r_sp], [r_sp])
                ts("dve", dm, dm, -1.0, None, ALU.add, None, [r_sp], [r_sp])
                for t in range(NT):
                    S.op("dve", lambda e, t=t: e.max(d8a[:, t, :], dm[:, t, :]), reads=[r_sp], writes=[r_sp])
                cp("dve", dest8u[:], d8a, [r_sp], [r_d8])
                for t in range(NT):
                    for j in range(8):
                        S.dma_fn("pool", (lambda e, t=t, j=j: e.indirect_dma_start(
                            out=xbuf, out_offset=bass.IndirectOffsetOnAxis(ap=dest8u[:, t, j:j + 1], axis=0),
                            in_=h2tok[:, t, :], in_offset=None)), reads=[r_d8, r_h2tok[t]], writes=[Res()])
                for j in range(8):
                    tt("dve", mk, dm, d8a[:, :, j:j + 1].to_broadcast([128, NT, 64]), ALU.is_equal, [r_sp], [r_sp])
                    tt("dve", mk, mk, Wr_all[:], ALU.mult, [r_sp] + r_Wr, [r_sp])
                    red(w8[:, :, j:j + 1].rearrange("p a b -> p (a b)"), mk, ALU.add, [r_sp], [r_w8])
                conv_some(1000)
                S.barrier()
                arena.off = a1 - 8 * S_LEN * 2

                NWS = 6
                Wblk = [(arena.alloc((8, 256), BF16), arena.alloc((8, 256), BF16), arena.alloc((2, D), BF16)) for _ in range(NWS)]
                r_Wb = [RL(3) for _ in range(NWS)]
                xtok = [arena.alloc((2, D), BF16) for _ in range(NWS)]
                r_xtok = RL(NWS)
                xTb = [arena.alloc((8, 256), BF16) for _ in range(2)]
                r_xTb = RL(2)
                sgb = arena.alloc((512,), F32)
                r_sgb = Res()
                hidb = [arena.alloc((2, 256), BF16) for _ in range(2)]
                r_hidb = RL(2)
                ysb = [arena.alloc((2, D), BF16) for _ in range(2)]
                r_ysb = RL(2)
                def load_blk(j, slot):
                    for wi_, src_t in enumerate((wgb, wub, wdb)):
                        dst = Wblk[slot][wi_]
                        dst2 = dst.rearrange("p a b -> p (a b)")
                        S.dma_fn("pool", (lambda e, dst2=dst2, src_t=src_t, j=j: e.indirect_dma_start(
                            out=dst2, out_offset=None, in_=src_t,
                            in_offset=bass.IndirectOffsetOnAxis(ap=eb_i[:, j:j + 1], axis=0))), reads=[r_eb, r_wconv], writes=[r_Wb[slot][wi_]])
                    S.dma("sp", xtok[slot], xbuf[j * BLK:(j + 1) * BLK, :].rearrange("(s p) n -> p s n", p=128), reads=[r_xbuf], writes=[r_xtok[slot]])

                def emit_T(j):
                    slot = j % 2
                    ws = j % NWS
                    for s_ in range(2):
                        for kc in range(8):
                            bk = kc // 4
                            o_ = (kc % 4) * 256 + s_ * 128
                            tr(bankb(bk)[:, o_:o_ + 128], xtok[ws][:, s_, kc * 128:(kc + 1) * 128], identb[:], [r_xtok[ws], r_c], [pb[bk]],
                               inc=(s_ == 1 and kc % 4 == 3))
                    cp("act", xTb[slot][:, 0:4, :].rearrange("p a b -> p (a b)"), bankb(0)[:, 0:1024], [pb[0]], [r_xTb[slot]])
                    cp("dve", xTb[slot][:, 4:8, :].rearrange("p a b -> p (a b)"), bankb(1)[:, 0:1024], [pb[1]], [r_xTb[slot]])

                def emit_GU(j):
                    slot = j % 2
                    ws = j % NWS
                    Wg_, Wu_, _ = Wblk[ws]
                    for (Wx, bk, wi_) in ((Wg_, 2, 0), (Wu_, 3, 1)):
                        for ffc in range(2):
                            for kc in range(8):
                                mm(bank(bk)[:, ffc * 256:(ffc + 1) * 256], Wx[:, kc, ffc * 128:(ffc + 1) * 128], xTb[slot][:, kc, :], kc == 0, kc == 7,
                                   [r_Wb[ws][wi_], r_xTb[slot]], [pb[bk]])
                    act(sgb, bank(2)[:, :], AF.Silu, [pb[2]], [r_sgb])
                    tt("dve", hidb[slot].rearrange("p a b -> p (a b)"), bank(3)[:, :], sgb, ALU.mult, [pb[3], r_sgb], [r_hidb[slot]])

                def emit_D(j):
                    slot = j % 2
                    ws = j % NWS
                    Wdn = Wblk[ws][2]
                    for s_ in range(2):
                        for hf in range(2):
                            bk = 4 + s_ * 2 + hf
                            for ffc in range(2):
                                mm(bank(bk)[:, :], hidb[slot][:, ffc, s_ * 128:(s_ + 1) * 128], Wdn[:, ffc, hf * 512:(hf + 1) * 512], ffc == 0, ffc == 1,
                                   [r_hidb[slot], r_Wb[ws][2]], [pb[bk]])
                            cp("act" if hf == 0 else "dve", ysb[slot][:, s_, hf * 512:(hf + 1) * 512], bank(bk)[:, :], [pb[bk]], [r_ysb[slot]])
                    S.dma("sp", ybuf[j * BLK:(j + 1) * BLK, :].rearrange("(s p) n -> p s n", p=128), ysb[slot], reads=[r_ysb[slot]], writes=[Res()])

                for j0_ in range(NWS - 1):
                    load_blk(j0_, j0_)
                emit_T(0)
                for j in range(NBLK):
                    if j + NWS - 1 < NBLK:
                        load_blk(j + NWS - 1, (j + NWS - 1) % NWS)
                    emit_GU(j)
                    if j + 1 < NBLK:
                        emit_T(j + 1)
                    emit_D(j)
                S.barrier()
                arena.off = a1

                Wsh = (arena.alloc((8, 256), BF16), arena.alloc((8, 256), BF16), arena.alloc((2, D), BF16))
                r_Wsh = Res()
                ldw(Wsh[0], Wd["w_sh_gate"].rearrange("(k p) f -> p k f", p=128), r_Wsh)
                ldw(Wsh[1], Wd["w_sh_up"].rearrange("(k p) f -> p k f", p=128), r_Wsh)
                ldw(Wsh[2], Wd["w_sh_down"].rearrange("(c p) n -> p c n", p=128), r_Wsh)
                sgs = arena.alloc((512,), F32)
                r_sgs = Res()
                hsh = [arena.alloc((512,), BF16) for _ in range(4)]
                r_hsh = RL(4)
                NYG = 16
                NDG = 16
                dgs = [arena.alloc((128,), BF16) for _ in range(NDG)]
                r_dg = RL(NDG)
                dgi = [0]
                yg = [arena.alloc((D,), BF16) for _ in range(NYG)]
                r_yg = RL(NYG)
                facc = [arena.alloc((D,), F32) for _ in range(2)]
                r_facc = RL(2)
                xo = [arena.alloc((D,), F32) for _ in range(2)]
                r_xo = RL(2)
                x1r = [arena.alloc((D,), F32) for _ in range(2)]
                r_x1r = RL(2)
                junk6 = arena.alloc((D,), F32)
                r_j6 = Res()
                r_st6 = Res()
                gi = 0
                for t4 in range(4):
                    cols = slice(t4 * 512, (t4 + 1) * 512)
                    rh = r_hT[t4 * 4:t4 * 4 + 4]
                    hh = []
                    for ffc in range(2):
                        for kc in range(8):
                            mm(bank(ffc)[:, :], Wsh[0][:, kc, ffc * 128:(ffc + 1) * 128], hT[:, kc, cols], kc == 0, kc == 7, [r_Wsh] + rh, [pb[ffc]])
                        for kc in range(8):
                            mm(bank(2 + ffc)[:, :], Wsh[1][:, kc, ffc * 128:(ffc + 1) * 128], hT[:, kc, cols], kc == 0, kc == 7, [r_Wsh] + rh, [pb[2 + ffc]])
                    for ffc in range(2):
                        hx = (t4 * 2 + ffc) % 4
                        act(sgs, bank(ffc)[:, :], AF.Silu, [pb[ffc]], [r_sgs])
                        tt("dve", hsh[hx], bank(2 + ffc)[:, :], sgs, ALU.mult, [pb[2 + ffc], r_sgs], [r_hsh[hx]])
                        hh.append(hx)
                    for sub in range(4):
                        t = t4 * 4 + sub
                        bi = t % 2
                        cs = slice(t * 128, (t + 1) * 128)
                        S.dma("sp", x1r[bi], out_d[b, cs, :], reads=[r_x1[t]], writes=[r_x1r[bi]])
                        gl_ = []
                        for j in range(8):
                            g_ = gi % NYG
                            gi += 1
                            S.dma_fn("pool", (lambda e, t=t, j=j, g_=g_: e.indirect_dma_start(
                                out=yg[g_], out_offset=None, in_=ybuf,
                                in_offset=bass.IndirectOffsetOnAxis(ap=dest8u[:, t, j:j + 1], axis=0))), reads=[r_d8], writes=[r_yg[g_]])
                            dj = dgi[0] % NDG
                            dgi[0] += 1
                            ts("dve", dgs[dj], identb[:], w8[:, t, j:j + 1], None, ALU.mult, None, [r_c, r_d8, r_w8], [r_dg[dj]])
                            gl_.append((g_, dj))
                        for hf in range(2):
                            bk = 4 + 2 * bi + hf
                            for ffc in range(2):
                                mm(bank(bk)[:, :], hsh[hh[ffc]][:, sub * 128:(sub + 1) * 128], Wsh[2][:, ffc, hf * 512:(hf + 1) * 512],
                                   ffc == 0, False, [r_hsh[hh[ffc]], r_Wsh], [pb[bk]], inc=False)
                            for j, (g_, dj) in enumerate(gl_):
                                mm(bank(bk)[:, :], dgs[dj], yg[g_][:, hf * 512:(hf + 1) * 512], False, j == 7, [r_dg[dj], r_yg[g_]], [pb[bk]])
                            cp("act", facc[bi][:, hf * 512:(hf + 1) * 512], bank(bk)[:, :], [pb[bk]], [r_facc[bi]])
                        if dbg and "acc" in dbg and b == 0:
                            S.dma("sp", dbg_d["acc"][cs, :], facc[bi], reads=[r_facc[bi]])
                        act(junk6, facc[bi], AF.Square, [r_facc[bi]], [r_j6, r_st6], accum_out=st6[:, 0:1])
                        rstd_from_ssq(st6[:, 2:3], st6[:, 0:1], D, st6[:, 1:2], r_st6)
                        stt(xo[bi], facc[bi], st6[:, 2:3], G2[:, b, :], ALU.mult, ALU.mult, [r_facc[bi], r_st6, r_m], [r_xo[bi]])
                        tt("dve", xo[bi], xo[bi], x1r[bi], ALU.add, [r_xo[bi], r_x1r[bi]], [r_xo[bi]])
                        S.dma("sp", out_d[b, cs, :], xo[bi], reads=[r_xo[bi]], writes=[r_x1[t]])
                S.barrier()
                continue
            S.barrier()
            arena.off = a1

            acc = arena.alloc((NT, D), F32)
            r_acc = RL(NT)
            a5 = arena.off
            We = [(arena.alloc((8, 256), BF16), arena.alloc((8, 256), BF16), arena.alloc((2, D), BF16)) for _ in range(2)]
            r_We = RL(2)
            sgm = [arena.alloc((512,), F32) for _ in range(2)]
            r_sgm = RL(2)
            hid = [arena.alloc((512,), BF16) for _ in range(4)]
            r_hid = RL(4)
            ones1 = cst["ones"][:, 0:1]

            def load_e_w(e_, slot):
                if e_ < NEXP:
                    g_, u_, d__ = Wd["w_e_gate"][e_], Wd["w_e_up"][e_], Wd["w_e_down"][e_]
                else:
                    g_, u_, d__ = Wd["w_sh_gate"], Wd["w_sh_up"], Wd["w_sh_down"]
                ldw(We[slot][0], g_.rearrange("(k p) f -> p k f", p=128), r_We[slot])
                ldw(We[slot][1], u_.rearrange("(k p) f -> p k f", p=128), r_We[slot])
                ldw(We[slot][2], d__.rearrange("(c p) n -> p c n", p=128), r_We[slot])

            load_e_w(0, 0)
            hi_ = 0
            for e_ in range(NEXP + 1):
                slot = e_ % 2
                if e_ + 1 <= NEXP:
                    load_e_w(e_ + 1, 1 - slot)
                Wg_, Wu_, Wdn = We[slot]
                for t4 in range(4):
                    cols = slice(t4 * 512, (t4 + 1) * 512)
                    rh = r_h2T[t4 * 4:t4 * 4 + 4]
                    for ffc in range(2):
                        for kc in range(8):
                            mm(bank(ffc)[:, :], Wg_[:, kc, ffc * 128:(ffc + 1) * 128], h2T[:, kc, cols], kc == 0, kc == 7, [r_We[slot]] + rh, [pb[ffc]])
                        for kc in range(8):
                            mm(bank(2 + ffc)[:, :], Wu_[:, kc, ffc * 128:(ffc + 1) * 128], h2T[:, kc, cols], kc == 0, kc == 7, [r_We[slot]] + rh, [pb[2 + ffc]])
                    hh = []
                    for ffc in range(2):
                        act(sgm[ffc], bank(ffc)[:, :], AF.Silu, [pb[ffc]], [r_sgm[ffc]])
                        hx = hi_ % 4
                        hi_ += 1
                        tt("dve", hid[hx], bank(2 + ffc)[:, :], sgm[ffc], ALU.mult, [pb[2 + ffc], r_sgm[ffc]], [r_hid[hx]])
                        hh.append(hx)
                    for sub in range(4):
                        t = t4 * 4 + sub
                        for hf in range(2):
                            bk = 4 + (sub * 2 + hf) % 4
                            for ffc in range(2):
                                mm(bank(bk)[:, :], hid[hh[ffc]][:, sub * 128:(sub + 1) * 128], Wdn[:, ffc, hf * 512:(hf + 1) * 512],
                                   ffc == 0, ffc == 1, [r_hid[hh[ffc]], r_We[slot]], [pb[bk]])
                            wcol = Wr_all[:, t, e_:e_ + 1] if e_ < NEXP else ones1
                            a_ = acc[:, t, hf * 512:(hf + 1) * 512]
                            if e_ == 0:
                                ts("dve", a_, bank(bk)[:, :], wcol, None, ALU.mult, None, [pb[bk], r_Wr[t]], [r_acc[t]])
                            else:
                                stt(a_, bank(bk)[:, :], wcol, a_, ALU.mult, ALU.add, [pb[bk], r_Wr[t], r_acc[t]], [r_acc[t]])
            if dbg and "acc" in dbg and b == 0:
                S.dma("sp", dbg_d["acc"].rearrange("(t p) n -> p t n", p=128), acc, reads=r_acc)

            S.barrier()
            arena.off = a5
            xo = [arena.alloc((D,), F32) for _ in range(2)]
            r_xo = RL(2)
            x1r = [arena.alloc((D,), F32) for _ in range(2)]
            r_x1r = RL(2)
            junk6 = arena.alloc((D,), F32)
            r_j6 = Res()
            r_st6 = Res()
            for t in range(NT):
                bi = t % 2
                cs = slice(t * 128, (t + 1) * 128)
                S.dma("sp", x1r[bi], out_d[b, cs, :], reads=[r_x1[t]], writes=[r_x1r[bi]])
                act(junk6, acc[:, t, :], AF.Square, [r_acc[t]], [r_j6, r_st6], accum_out=st6[:, 0:1])
                rstd_from_ssq(st6[:, 2:3], st6[:, 0:1], D, st6[:, 1:2], r_st6)
                stt(xo[bi], acc[:, t, :], st6[:, 2:3], G2[:, b, :], ALU.mult, ALU.mult, [r_acc[t], r_st6, r_m], [r_xo[bi]])
                tt("dve", xo[bi], xo[bi], x1r[bi], ALU.add, [r_xo[bi], r_x1r[bi]], [r_xo[bi]])
                S.dma("sp", out_d[b, cs, :], xo[bi], reads=[r_xo[bi]], writes=[r_x1[t]])
            S.barrier()


        S.emit()
    return nc


def make_in_maps(inputs, n_cores=8):
    consts = host_consts()
    shared = {}
    for k, shp in WSHAPES.items():
        shared[k] = np.ascontiguousarray(np.asarray(inputs[k], dtype=np.float32).reshape(shp))
    for k, v in consts.items():
        shared["k_" + k] = np.ascontiguousarray(v.astype(np.float32))
    x = np.asarray(inputs["x"], dtype=np.float32)
    c = np.asarray(inputs["c"], dtype=np.float32)
    ctx = np.asarray(inputs["ctx"], dtype=np.float32)
    c_ctx = np.asarray(inputs["c_ctx"], dtype=np.float32)
    maps = []
    for i in range(n_cores):
        m = dict(shared)
        m["x"] = np.ascontiguousarray(x[i * NB:(i + 1) * NB])
        m["ctx"] = np.ascontiguousarray(ctx[i * NB:(i + 1) * NB])
        m["c3"] = np.ascontiguousarray(np.concatenate([c[i * NB:(i + 1) * NB], c_ctx[None, :]], axis=0))
        maps.append(m)
    return maps


def kernel(**inputs):
    nc = build()
    maps = make_in_maps(inputs)
    res = run_bass_kernel_spmd(nc, maps, core_ids=list(range(8)))
    return np.concatenate([r["out"] for r in res.results], axis=0).astype(np.float32)
```
